# Optimizing a Trainium2 kernel written in Bass

```python
import jax, jax.numpy as jnp
from jax import lax
import numpy as np

D_MODEL = 2048
BATCH = 4
SEQ = 4096
DEPTH = 4

GRID_W = 64
CTX_LEN = 256
N_MIXERS = 2
N_MOD = 6
NORM_EPS = 1e-6

MLA_HEADS = 16
MLA_Q_RANK = 768
MLA_KV_RANK = 256
MLA_NOPE_DIM = 128
MLA_ROPE_DIM = 64
MLA_V_DIM = 128
MLA_QK_DIM = MLA_NOPE_DIM + MLA_ROPE_DIM
Q_BLOCK = 128
ROPE_THETA = 10000.0

GLA_HEADS = 4
GLA_KEY_DIM = D_MODEL // 2
GLA_VALUE_DIM = D_MODEL
GLA_DK = GLA_KEY_DIM // GLA_HEADS
GLA_DV = GLA_VALUE_DIM // GLA_HEADS
GLA_GATE_RANK = 16
GLA_TAU = 16.0
GLA_CHUNK = 64

MOE_GROUPS = 4
MOE_PER_GROUP = 8
MOE_EXPERTS = MOE_GROUPS * MOE_PER_GROUP
MOE_TOP_K = 2
MOE_D_FF = 512
MOE_BLOCK = 256

N_MLA_LAYERS = (DEPTH + N_MIXERS - 1) // N_MIXERS
N_GLA_LAYERS = DEPTH // N_MIXERS

kernel_name = 'hybrid_mla_gla_hmoe_dit'


def layer_norm(x, g, b):
    xf = x.astype(jnp.float32)
    mu = jnp.mean(xf, axis=-1, keepdims=True)
    var = jnp.mean(jnp.square(xf - mu), axis=-1, keepdims=True)
    return ((xf - mu) * lax.rsqrt(var + NORM_EPS) * g + b).astype(x.dtype)


def rms_norm(x, g):
    xf = x.astype(jnp.float32)
    return (xf * lax.rsqrt(jnp.mean(jnp.square(xf), axis=-1, keepdims=True) + NORM_EPS) * g).astype(x.dtype)


def modulate(x, shift, scale):
    return x * (1 + scale) + shift


def adaln(cond, w_mod, b_mod):
    m = jax.nn.silu(cond) @ w_mod + b_mod
    return jnp.split(m[..., None, :], N_MOD, axis=-1)


def split_last(t, sizes):
    return jnp.split(t, np.cumsum(sizes)[:-1].tolist(), axis=-1)


def axial_rope_tables(n_tokens):
    n_rows = n_tokens // GRID_W
    row = jnp.repeat(jnp.arange(n_rows, dtype=jnp.float32), GRID_W)
    col = jnp.tile(jnp.arange(GRID_W, dtype=jnp.float32), n_rows)
    n_freq = MLA_ROPE_DIM // 4
    inv_freq = jnp.power(ROPE_THETA, -jnp.arange(n_freq, dtype=jnp.float32) / n_freq)
    ang = jnp.concatenate([row[:, None] * inv_freq, col[:, None] * inv_freq], axis=-1)
    return jnp.cos(ang), jnp.sin(ang)


def apply_rope(t, cos, sin):
    t1, t2 = jnp.split(t.astype(jnp.float32), 2, axis=-1)
    return jnp.concatenate([t1 * cos - t2 * sin, t1 * sin + t2 * cos], axis=-1).astype(t.dtype)


def mla_project(h, rope, w_in, q_norm_g, w_uq, kv_norm_g, w_ukv):
    B, L, _ = h.shape
    cq, ckv, k_pe = split_last(h @ w_in, [MLA_Q_RANK, MLA_KV_RANK, MLA_ROPE_DIM])
    q = (rms_norm(cq, q_norm_g) @ w_uq).reshape(B, L, MLA_HEADS, MLA_QK_DIM)
    kv = (rms_norm(ckv, kv_norm_g) @ w_ukv).reshape(B, L, MLA_HEADS, MLA_NOPE_DIM + MLA_V_DIM)
    q_nope, q_pe = split_last(q, [MLA_NOPE_DIM, MLA_ROPE_DIM])
    k_nope, v = split_last(kv, [MLA_NOPE_DIM, MLA_V_DIM])
    if rope is not None:
        cos, sin = rope
        q_pe = apply_rope(q_pe, cos[None, :, None], sin[None, :, None])
        k_pe = apply_rope(k_pe, cos[None], sin[None])
    k_pe = jnp.broadcast_to(k_pe[:, :, None, :], (B, L, MLA_HEADS, MLA_ROPE_DIM))
    return (jnp.concatenate([q_nope, q_pe], axis=-1),
            jnp.concatenate([k_nope, k_pe], axis=-1), v)


def block_attention(q, k, v):
    B, Lq, H, dqk = q.shape
    dv = v.shape[-1]
    nb = Lq // Q_BLOCK
    qb = jnp.moveaxis(q.reshape(B, nb, Q_BLOCK, H, dqk), 1, 0)
    scale = dqk ** -0.5

    def one_block(q_blk):
        s = jnp.einsum('bqhd,bkhd->bhqk', q_blk, k, preferred_element_type=jnp.float32) * scale
        p = jax.nn.softmax(s, axis=-1).astype(v.dtype)
        return jnp.einsum('bhqk,bkhd->bqhd', p, v)

    o = lax.map(one_block, qb)
    return jnp.moveaxis(o, 0, 1).reshape(B, Lq, H * dv)


def mla_mixer(h_ctx, h_lat, rope, ctx_out, w_in, q_norm_g, w_uq, kv_norm_g, w_ukv, w_o):
    q_c, k_c, v_c = mla_project(h_ctx, None, w_in, q_norm_g, w_uq, kv_norm_g, w_ukv)
    q_l, k_l, v_l = mla_project(h_lat, rope, w_in, q_norm_g, w_uq, kv_norm_g, w_ukv)
    o_l = block_attention(q_l, jnp.concatenate([k_c, k_l], axis=1), jnp.concatenate([v_c, v_l], axis=1))
    y_ctx = block_attention(q_c, k_c, v_c) @ w_o if ctx_out else None
    return y_ctx, o_l @ w_o


def gla_chunk_scan(q, k, v, g, s0):
    B, H, L, DK = q.shape
    DV = v.shape[-1]
    n = L // GLA_CHUNK

    def chunks(t):
        return jnp.moveaxis(t.reshape(B, H, n, GLA_CHUNK, t.shape[-1]), 2, 0)

    mask = jnp.tril(jnp.ones((GLA_CHUNK, GLA_CHUNK), dtype=bool))[:, :, None]

    def step(S, inp):
        qc, kc, vc, gc = inp
        qf, kf, vf = qc.astype(jnp.float32), kc.astype(jnp.float32), vc.astype(jnp.float32)
        b = jnp.cumsum(gc.astype(jnp.float32), axis=2)
        o_inter = jnp.einsum('bhcd,bhde->bhce', qf * jnp.exp(b), S)
        rel = jnp.where(mask, b[:, :, :, None, :] - b[:, :, None, :, :], -jnp.inf)
        A = jnp.einsum('bhid,bhjd,bhijd->bhij', qf, kf, jnp.exp(rel))
        o_intra = jnp.einsum('bhij,bhje->bhie', A, vf)
        b_last = b[:, :, -1:, :]
        S_new = jnp.exp(b_last[:, :, 0, :])[..., None] * S + jnp.einsum('bhcd,bhce->bhde', kf * jnp.exp(b_last - b), vf)
        return S_new, o_inter + o_intra

    S_fin, o = lax.scan(step, s0, (chunks(q), chunks(k), chunks(v), chunks(g)))
    return jnp.moveaxis(o, 0, 2).reshape(B, H, L, DV), S_fin


def gla_project(h, w_in, gate_a, gate_b, gate_bias):
    B, L, _ = h.shape
    q, k, v, r = split_last(h @ w_in, [GLA_KEY_DIM, GLA_KEY_DIM, GLA_VALUE_DIM, GLA_VALUE_DIM])

    def heads(t):
        return jnp.transpose(t.reshape(B, L, GLA_HEADS, -1), (0, 2, 1, 3))

    g_fwd, g_bwd = [heads(jax.nn.log_sigmoid(((h @ gate_a[d]) @ gate_b[d] + gate_bias[d]).astype(jnp.float32)) / GLA_TAU)
                    for d in range(2)]
    return heads(q) * GLA_DK ** -0.5, heads(k), heads(v), r, g_fwd, g_bwd


def gla_bidir(q, k, v, g_fwd, g_bwd, s_fwd, s_bwd):
    flip = lambda t: jnp.flip(t, axis=2)
    o_f, s_f = gla_chunk_scan(q, k, v, g_fwd, s_fwd)
    o_b, s_b = gla_chunk_scan(flip(q), flip(k), flip(v), flip(g_bwd), s_bwd)
    return o_f + flip(o_b), s_f, s_b


def gla_mixer(h_ctx, h_lat, ctx_out, w_in, gate_a, gate_b, gate_bias, norm_g, w_o):
    B = h_lat.shape[0]
    q_c, k_c, v_c, r_c, gf_c, gb_c = gla_project(h_ctx, w_in, gate_a, gate_b, gate_bias)
    q_l, k_l, v_l, r_l, gf_l, gb_l = gla_project(h_lat, w_in, gate_a, gate_b, gate_bias)
    s0 = jnp.zeros((B, GLA_HEADS, GLA_DK, GLA_DV), jnp.float32)
    o_c, s_f, s_b = gla_bidir(q_c, k_c, v_c, gf_c, gb_c, s0, s0)
    o_l, _, _ = gla_bidir(q_l, k_l, v_l, gf_l, gb_l, s_f, s_b)

    def out(o, r):
        L = o.shape[2]
        o = jnp.transpose(rms_norm(o, norm_g), (0, 2, 1, 3)).reshape(B, L, GLA_VALUE_DIM).astype(r.dtype)
        return (o * jax.nn.silu(r)) @ w_o

    return (out(o_c, r_c) if ctx_out else None), out(o_l, r_l)


def hier_moe(h, w_grp, b_grp, w_exp, b_exp, w1, w3, w2):
    N, D = h.shape
    grp_logits = (h @ w_grp).astype(jnp.float32) + b_grp
    grp_p = jax.nn.softmax(grp_logits, axis=-1)
    _, g_top = lax.top_k(grp_logits, 1)
    p_grp = jnp.take_along_axis(grp_p, g_top, axis=-1)
    exp_logits = ((h @ w_exp).astype(jnp.float32) + b_exp).reshape(N, MOE_GROUPS, MOE_PER_GROUP)
    in_grp = jnp.take_along_axis(exp_logits, g_top[:, :, None], axis=1)[:, 0]
    top_v, top_i = lax.top_k(jax.nn.softmax(in_grp, axis=-1), MOE_TOP_K)
    weight = p_grp * top_v / jnp.sum(top_v, axis=-1, keepdims=True)
    expert_id = g_top * MOE_PER_GROUP + top_i

    A = N * MOE_TOP_K
    e_flat = expert_id.reshape(A)
    tok_flat = jnp.repeat(jnp.arange(N, dtype=jnp.int32), MOE_TOP_K)
    w_flat = weight.reshape(A)
    order = jnp.argsort(e_flat)
    e_sorted = e_flat[order]
    counts = jnp.zeros((MOE_EXPERTS,), jnp.int32).at[e_flat].add(1)
    starts = jnp.cumsum(counts) - counts
    padded = (counts + MOE_BLOCK - 1) // MOE_BLOCK * MOE_BLOCK
    pad_end = jnp.cumsum(padded)
    pad_start = pad_end - padded
    dest = pad_start[e_sorted] + (jnp.arange(A, dtype=jnp.int32) - starts[e_sorted])
    n_blocks = -(-A // MOE_BLOCK) + MOE_EXPERTS
    R = n_blocks * MOE_BLOCK
    slot_tok = jnp.full((R,), N, jnp.int32).at[dest].set(tok_flat[order])
    slot_w = jnp.zeros((R,), jnp.float32).at[dest].set(w_flat[order])
    block_exp = jnp.minimum(jnp.searchsorted(pad_end, jnp.arange(n_blocks, dtype=jnp.int32) * MOE_BLOCK, side='right'),
                            MOE_EXPERTS - 1)
    h_pad = jnp.concatenate([h, jnp.zeros((1, D), h.dtype)], axis=0)

    def run_block(args):
        toks, ws, e = args
        xb = h_pad[toks]
        y = (jax.nn.silu(xb @ w1[e]) * (xb @ w3[e])) @ w2[e]
        return y * ws[:, None]

    out = lax.map(run_block, (slot_tok.reshape(n_blocks, MOE_BLOCK), slot_w.reshape(n_blocks, MOE_BLOCK), block_exp))
    y = jax.ops.segment_sum(out.reshape(R, D), slot_tok, num_segments=N + 1)[:N]
    return y.astype(h.dtype)


def setup_inputs(seed: int = 0) -> dict:
    key = jax.random.key(seed)
    keys = jax.random.split(key, 32)
    counter = [0]

    def nrm(shape, scale):
        k = keys[counter[0]]
        counter[0] += 1
        return jax.random.normal(k, shape, jnp.float32) * scale

    D = D_MODEL
    beta = (8.0 * DEPTH) ** -0.25
    return {
        'x': nrm((BATCH, SEQ, D), 1.0),
        'c': nrm((BATCH, D), 1.0),
        'ctx': nrm((BATCH, CTX_LEN, D), 1.0),
        'c_ctx': nrm((D,), 1.0),
        'w_mod': nrm((DEPTH, D, N_MOD * D), 0.5 * D ** -0.5),
        'b_mod': nrm((DEPTH, N_MOD * D), 0.02),
        'ln1_g': 1.0 + nrm((DEPTH, D), 0.02),
        'ln1_b': nrm((DEPTH, D), 0.02),
        'ln2_g': 1.0 + nrm((DEPTH, D), 0.02),
        'ln2_b': nrm((DEPTH, D), 0.02),
        'mla_w_in': nrm((N_MLA_LAYERS, D, MLA_Q_RANK + MLA_KV_RANK + MLA_ROPE_DIM), D ** -0.5),
        'mla_q_norm': 1.0 + nrm((N_MLA_LAYERS, MLA_Q_RANK), 0.02),
        'mla_w_uq': nrm((N_MLA_LAYERS, MLA_Q_RANK, MLA_HEADS * MLA_QK_DIM), MLA_Q_RANK ** -0.5),
        'mla_kv_norm': 1.0 + nrm((N_MLA_LAYERS, MLA_KV_RANK), 0.02),
        'mla_w_ukv': nrm((N_MLA_LAYERS, MLA_KV_RANK, MLA_HEADS * (MLA_NOPE_DIM + MLA_V_DIM)), MLA_KV_RANK ** -0.5),
        'mla_w_o': nrm((N_MLA_LAYERS, MLA_HEADS * MLA_V_DIM, D), beta * (MLA_HEADS * MLA_V_DIM) ** -0.5),
        'gla_w_in': nrm((N_GLA_LAYERS, D, 2 * GLA_KEY_DIM + 2 * GLA_VALUE_DIM), D ** -0.5),
        'gla_gate_a': nrm((N_GLA_LAYERS, 2, D, GLA_GATE_RANK), D ** -0.5),
        'gla_gate_b': nrm((N_GLA_LAYERS, 2, GLA_GATE_RANK, GLA_KEY_DIM), GLA_GATE_RANK ** -0.5),
        'gla_gate_bias': nrm((N_GLA_LAYERS, 2, GLA_KEY_DIM), 0.1),
        'gla_norm': 1.0 + nrm((N_GLA_LAYERS, GLA_DV), 0.02),
        'gla_w_o': nrm((N_GLA_LAYERS, GLA_VALUE_DIM, D), beta * GLA_VALUE_DIM ** -0.5),
        'moe_w_grp': nrm((DEPTH, D, MOE_GROUPS), D ** -0.5),
        'moe_b_grp': nrm((DEPTH, MOE_GROUPS), 0.01),
        'moe_w_exp': nrm((DEPTH, D, MOE_EXPERTS), D ** -0.5),
        'moe_b_exp': nrm((DEPTH, MOE_EXPERTS), 0.01),
        'moe_w1': nrm((DEPTH, MOE_EXPERTS, D, MOE_D_FF), D ** -0.5),
        'moe_w3': nrm((DEPTH, MOE_EXPERTS, D, MOE_D_FF), D ** -0.5),
        'moe_w2': nrm((DEPTH, MOE_EXPERTS, MOE_D_FF, D), beta * MOE_D_FF ** -0.5),
    }


def reference(x, c, ctx, c_ctx, w_mod, b_mod, ln1_g, ln1_b, ln2_g, ln2_b,
              mla_w_in, mla_q_norm, mla_w_uq, mla_kv_norm, mla_w_ukv, mla_w_o,
              gla_w_in, gla_gate_a, gla_gate_b, gla_gate_bias, gla_norm, gla_w_o,
              moe_w_grp, moe_b_grp, moe_w_exp, moe_b_exp, moe_w1, moe_w3, moe_w2):
    B, L, D = x.shape
    Lc = ctx.shape[1]
    alpha = (2.0 * DEPTH) ** 0.25
    rope = axial_rope_tables(L)
    x_lat, x_ctx = x, ctx
    for i in range(DEPTH):
        last = i == DEPTH - 1
        sh_l1, sc_l1, g_l1, sh_l2, sc_l2, g_l2 = adaln(c, w_mod[i], b_mod[i])
        sh_c1, sc_c1, g_c1, sh_c2, sc_c2, g_c2 = adaln(c_ctx, w_mod[i], b_mod[i])
        h_ctx = modulate(x_ctx, sh_c1, sc_c1)
        h_lat = modulate(x_lat, sh_l1, sc_l1)
        j = i // N_MIXERS
        if i % N_MIXERS == 0:
            y_ctx, y_lat = mla_mixer(h_ctx, h_lat, rope, not last, mla_w_in[j], mla_q_norm[j], mla_w_uq[j],
                                     mla_kv_norm[j], mla_w_ukv[j], mla_w_o[j])
        else:
            y_ctx, y_lat = gla_mixer(h_ctx, h_lat, not last, gla_w_in[j], gla_gate_a[j], gla_gate_b[j],
                                     gla_gate_bias[j], gla_norm[j], gla_w_o[j])
        x_lat = layer_norm(alpha * x_lat + g_l1 * y_lat, ln1_g[i], ln1_b[i])
        h_lat = modulate(x_lat, sh_l2, sc_l2)
        moe_args = (moe_w_grp[i], moe_b_grp[i], moe_w_exp[i], moe_b_exp[i], moe_w1[i], moe_w3[i], moe_w2[i])
        if last:
            y_lat = hier_moe(h_lat.reshape(B * L, D), *moe_args).reshape(B, L, D)
        else:
            x_ctx = layer_norm(alpha * x_ctx + g_c1 * y_ctx, ln1_g[i], ln1_b[i])
            h_ctx = modulate(x_ctx, sh_c2, sc_c2)
            y = hier_moe(jnp.concatenate([h_ctx.reshape(B * Lc, D), h_lat.reshape(B * L, D)], axis=0), *moe_args)
            y_ctx = y[:B * Lc].reshape(B, Lc, D)
            y_lat = y[B * Lc:].reshape(B, L, D)
            x_ctx = layer_norm(alpha * x_ctx + g_c2 * y_ctx, ln2_g[i], ln2_b[i])
        x_lat = layer_norm(alpha * x_lat + g_l2 * y_lat, ln2_g[i], ln2_b[i])
    return x_lat
```

```python
import numpy as np
import ml_dtypes
from contextlib import ExitStack
import concourse.bass as bass
import concourse.mybir as mybir
from concourse.bass_utils import run_bass_kernel_spmd

F32 = mybir.dt.float32
BF16 = mybir.dt.bfloat16
I32 = mybir.dt.int32
U32 = mybir.dt.uint32
AF = mybir.ActivationFunctionType
ALU = mybir.AluOpType
AX = mybir.AxisListType

import os
SKIP = os.environ.get("SKIP", "")
STOPAT = float(os.environ.get("STOPAT", "99"))
D = 2048
DEPTH = 4
NOWN = 2176
NB = 4352
ALPHA = (2.0 * DEPTH) ** 0.25
EPS = 1e-6


class Buf:
    __slots__ = ("name", "t", "last_w", "reads", "dsem", "dcount", "psum", "nowaw")

    def __init__(self, name, t=None):
        self.psum = False
        self.nowaw = False
        self.name = name
        self.t = t
        self.last_w = None
        self.reads = []
        self.dsem = None
        self.dcount = 0

    def __getitem__(self, idx):
        return self.t[idx]


class Prog:
    def __init__(self, nc):
        self.nc = nc
        self.es = ExitStack()
        self.engs = {"pe": nc.tensor, "act": nc.scalar, "dve": nc.vector, "pool": nc.gpsimd, "sp": nc.sync}
        self.esem = {}
        self.ecount = {k: 0 for k in self.engs}
        self.waited = {k: {} for k in self.engs}
        self.sems = {}
        self.epoch = 0
        for k in self.engs:
            s = self.es.enter_context(nc.semaphore("es_" + k))
            self.esem[k] = s
            self.sems[("e", k, 0)] = s
        self.sem_pool = []
        self.scopes = []
        self.nsem = 0
        self.dbufs = []
        self.nwaits = 0
        self.ninst = 0
        self.uid = 0

    def sb(self, stack, name, shape, dt):
        self.uid += 1
        nm = "%s_%d" % (name, self.uid)
        t = stack.enter_context(self.nc.sbuf_tensor(nm, list(shape), dt))
        b = Buf(nm, t)
        if self.scopes:
            self.scopes[-1].append(b)
        return b

    def ps(self, stack, name, shape, dt):
        self.uid += 1
        nm = "%s_%d" % (name, self.uid)
        t = stack.enter_context(self.nc.psum_tensor(nm, list(shape), dt))
        b = Buf(nm, t)
        b.psum = True
        return b

    def dram(self, name, shape, dt, kind="Internal"):
        t = self.nc.dram_tensor(name, list(shape), dt, kind=kind)
        b = Buf(name, t.ap())
        b.nowaw = True
        return b

    def _dsem(self, buf):
        if buf.dsem is None:
            if self.sem_pool:
                buf.dsem, buf.dcount = self.sem_pool.pop()
            else:
                self.nsem += 1
                s = self.es.enter_context(self.nc.semaphore("ds%d" % self.nsem))
                buf.dsem = ("d", self.nsem)
                self.sems[buf.dsem] = s
            self.dbufs.append(buf)
        return buf.dsem

    def new_epoch(self):
        self.barrier()
        self.epoch += 1
        for k in self.engs:
            s = self.es.enter_context(self.nc.semaphore("es_%s_%d" % (k, self.epoch)))
            self.esem[k] = s
            self.sems[("e", k, self.epoch)] = s
            self.ecount[k] = 0

    def release_scope(self, bufs):
        for b in bufs:
            if b.dsem is not None:
                self.sem_pool.append((b.dsem, b.dcount))
                self.dbufs.remove(b)
                b.dsem = None

    def _wait(self, ek, ev):
        if ev is None:
            return
        key, val = ev
        if key == ("e", ek, self.epoch) and ek == "pe":
            return
        if self.waited[ek].get(key, 0) >= val:
            return
        self.waited[ek][key] = val
        self.engs[ek].wait_ge(self.sems[key], val)
        self.nwaits += 1

    def _deps(self, ek, reads, writes):
        for b in reads:
            self._wait(ek, b.last_w)
        for b in writes:
            if not (b.nowaw and b.last_w is not None and b.last_w[0] == b.dsem):
                self._wait(ek, b.last_w)
            for r in b.reads:
                self._wait(ek, r)

    def _commit(self, ev, reads, writes):
        for b in reads:
            b.reads.append(ev)
            if len(b.reads) > 24:
                d = {}
                for k, v in b.reads:
                    d[k] = max(d.get(k, 0), v)
                b.reads = list(d.items())
        for b in writes:
            b.last_w = ev
            b.reads = []

    def op(self, ek, fn, reads=(), writes=()):
        pr = [b for b in reads if b.psum]
        if pr:
            reads = [b for b in reads if not b.psum]
            writes = list(writes) + pr
        self._deps(ek, reads, writes)
        ins = fn(self.engs[ek])
        self.ecount[ek] += 1
        ins.then_inc(self.esem[ek], 1)
        self._commit((("e", ek, self.epoch), self.ecount[ek]), reads, writes)
        self.ninst += 1
        return ins

    def dma(self, q, out_ap, in_ap, reads=(), writes=(), sem_buf=None, **kw):
        self._deps(q, reads, writes)
        sb_ = sem_buf if sem_buf is not None else writes[0]
        key = self._dsem(sb_)
        ins = self.engs[q].dma_start(out=out_ap, in_=in_ap, **kw)
        sb_.dcount += 1
        ins.then_inc(self.sems[key], 16)
        self._commit((key, 16 * sb_.dcount), reads, writes)
        self.ninst += 1
        return ins

    def scope(self):
        return _Scope(self)

    def barrier(self):
        for ek in self.engs:
            for e2 in self.engs:
                if e2 != ek and self.ecount[e2] > 0:
                    self._wait(ek, (("e", e2, self.epoch), self.ecount[e2]))
            for b in self.dbufs:
                self._wait(ek, (b.dsem, 16 * b.dcount))

    def idma(self, out_ap, in_ap, idx_ap, reads=(), writes=()):
        self._deps("pool", reads, writes)
        sb_ = writes[0]
        key = self._dsem(sb_)
        ins = self.nc.gpsimd.indirect_dma_start(out=out_ap, out_offset=None, in_=in_ap,
                                                in_offset=bass.IndirectOffsetOnAxis(ap=idx_ap, axis=0))
        sb_.dcount += 1
        ins.then_inc(self.sems[key], 16)
        self._commit((key, 16 * sb_.dcount), reads, writes)
        self.ninst += 1
        return ins

    def iscatter(self, out_ap, in_ap, idx_ap, reads=(), writes=()):
        self._deps("pool", reads, writes)
        sb_ = writes[0]
        self._wait("pool", sb_.last_w)
        key = self._dsem(sb_)
        ins = self.nc.gpsimd.indirect_dma_start(out=out_ap, out_offset=bass.IndirectOffsetOnAxis(ap=idx_ap, axis=0),
                                                in_=in_ap, in_offset=None)
        sb_.dcount += 1
        ins.then_inc(self.sems[key], 16)
        self._commit((key, 16 * sb_.dcount), reads, writes)
        self.ninst += 1
        return ins

    def finish(self, bufs, ek="sp"):
        for b in bufs:
            self._wait(ek, b.last_w)

    def close(self):
        self.es.close()


class _Scope:
    def __init__(self, P):
        self.P = P
        self.st = ExitStack()

    def __enter__(self):
        self.st.__enter__()
        self.bufs = []
        self.P.scopes.append(self.bufs)
        return self.st

    def __exit__(self, *a):
        if a[0] is None:
            self.P.barrier()
            self.P.release_scope(self.bufs)
        self.P.scopes.pop()
        return self.st.__exit__(*a)


def blocks(n, bs):
    out = []
    t = 0
    while t < n:
        out.append((t, min(n, t + bs)))
        t += bs
    return out


def segs_own(t0, t1, ctxn=128):
    out = []
    if t0 < ctxn:
        out.append((t0, min(t1, ctxn), 1))
    if t1 > ctxn:
        out.append((max(t0, ctxn), t1, 0))
    return out


def consts(P, st):
    C = {}
    C["ones_f"] = P.sb(st, "ones_f", [128, 128], F32)
    P.op("dve", lambda e: e.memset(C["ones_f"][:], 1.0), writes=[C["ones_f"]])
    C["ones_b"] = P.sb(st, "ones_b", [128, 128], BF16)
    P.op("dve", lambda e: e.memset(C["ones_b"][:], 1.0), writes=[C["ones_b"]])
    C["one"] = P.sb(st, "one", [128, 1], F32)
    P.op("dve", lambda e: e.memset(C["one"][:], 1.0), writes=[C["one"]])
    C["eps"] = P.sb(st, "eps", [128, 1], F32)
    P.op("dve", lambda e: e.memset(C["eps"][:], EPS), writes=[C["eps"]])
    C["epsa"] = P.sb(st, "epsa", [128, 1], F32)
    P.op("dve", lambda e: e.memset(C["epsa"][:], EPS / (ALPHA * ALPHA)), writes=[C["epsa"]])
    return C


def identity(P, st, C, dt, name):
    idf = P.sb(st, name, [128, 128], dt)
    P.op("pool", lambda e: e.memset(idf[:], 1.0), writes=[idf])
    P.op("pool", lambda e: e.affine_select(out=idf[:], in_=idf[:], pattern=[[-1, 128]], compare_op=ALU.is_equal,
                                           fill=0.0, base=0, channel_multiplier=1), reads=[idf], writes=[idf])
    return idf


def tri_mask(P, st, name, dt, val, sgn, strict=False):
    t = P.sb(st, name, [128, 128], dt)
    P.op("pool", lambda e: e.memset(t[:], val), writes=[t])
    P.op("pool", lambda e: e.affine_select(out=t[:], in_=t[:], pattern=[[sgn, 128]],
                                           compare_op=(ALU.is_gt if strict else ALU.is_ge),
                                           fill=0.0, base=0, channel_multiplier=-sgn), reads=[t], writes=[t])
    return t


def phase_a(P, C, cc, wmod, bmod, xT, mods_o, hT, N=NOWN, ctxn=128):
    with P.scope() as st:
        cs = P.sb(st, "cs", [128, 16, 2], F32)
        bm = P.sb(st, "bm", [128, 96], F32)
        mods = P.sb(st, "mods", [128, 96, 2], F32)
        slabs = [P.sb(st, "slab%d" % i, [128, 16, 512], F32) for i in range(2)]
        pm = P.ps(st, "pm", [128, 192], F32)
        P.dma("sp", cs[:], cc[:], reads=[cc], writes=[cs])
        P.dma("sp", bm[:], bmod[:], reads=[bmod], writes=[bm])
        P.op("act", lambda e: e.activation(out=cs[:], in_=cs[:], func=AF.Silu), reads=[cs], writes=[cs])
        wv = wmod.t.rearrange("(kc p) n -> p kc n", p=128)
        for s in range(24):
            sl = slabs[s % 2]
            P.dma("sp", sl[:], wv[:, :, s * 512:(s + 1) * 512], reads=[wmod], writes=[sl])
            for j in range(4):
                n = s * 4 + j
                for kc in range(16):
                    P.op("pe", lambda e: e.matmul(out=pm[:, 2 * n:2 * n + 2], lhsT=sl[:, kc, j * 128:(j + 1) * 128],
                                                  rhs=cs[:, kc, :], start=(kc == 0), stop=(kc == 15)),
                         reads=[sl, cs], writes=[pm])
        for j in range(2):
            P.op("dve", lambda e: e.tensor_tensor(out=mods[:, :, j],
                                                  in0=pm[:].rearrange("p (n j) -> p n j", j=2)[:, :, j],
                                                  in1=bm[:], op=ALU.add), reads=[pm, bm], writes=[mods])
        P.dma("sp", mods_o[:], mods[:], reads=[mods], writes=[mods_o])
        sc1p = P.sb(st, "sc1p", [128, 16, 2], F32)
        P.op("dve", lambda e: e.tensor_scalar(out=sc1p[:], in0=mods[:, 16:32, :], scalar1=1.0, scalar2=None,
                                              op0=ALU.add), reads=[mods], writes=[sc1p])
        xb = [P.sb(st, "xb%d" % i, [128, N], F32) for i in range(2)]
        hb = [P.sb(st, "hb%d" % i, [128, N], BF16) for i in range(2)]
        for c in range(16):
            x_ = xb[c % 2]
            h_ = hb[c % 2]
            P.dma("sp", x_[:], xT[c * 128:(c + 1) * 128, :], reads=[xT], writes=[x_])
            for (a, b, j) in segs_own(0, N, ctxn):
                P.op("act", lambda e: e.activation(out=h_[:, a:b], in_=x_[:, a:b], func=AF.Identity,
                                                   scale=sc1p[:, c, j:j + 1], bias=mods[:, c, j:j + 1]),
                     reads=[x_, sc1p, mods], writes=[h_])
            P.dma("sp", hT[c * 128:(c + 1) * 128, :], h_[:], reads=[h_], writes=[hT])


def phase_mla(P, C, hT, w_in, qng, w_uq, kvng, w_ukv, cos2, sin2, oT, dbg=None, NHT=8):
    NH = 8
    scale = 192.0 ** -0.5
    with P.scope() as st0:
        cqn = P.sb(st0, "cqn", [128, 6, NB], BF16)
        ckvn = P.sb(st0, "ckvn", [128, 2, NB], BF16)
        kr = P.sb(st0, "kr", [64, NB], BF16)
        with P.scope() as st:
            win = P.sb(st, "win", [128, 16, 1088], BF16)
            P.dma("pool", win[:], w_in.t.rearrange("(kc p) n -> p kc n", p=128), reads=[w_in], writes=[win])
            wrot = P.sb(st, "wrot", [128, 16, 64], BF16)
            P.op("dve", lambda e: e.tensor_scalar(out=wrot[:, :, 0:32], in0=win[:, :, 1056:1088], scalar1=-1.0,
                                                  scalar2=None, op0=ALU.mult), reads=[win], writes=[wrot])
            P.op("dve", lambda e: e.tensor_copy(out=wrot[:, :, 32:64], in_=win[:, :, 1024:1056]), reads=[win],
                 writes=[wrot])
            qg = P.sb(st, "qg", [128, 6], F32)
            kg = P.sb(st, "kg", [128, 2], F32)
            P.dma("sp", qg[:], qng[:], reads=[qng], writes=[qg])
            P.dma("sp", kg[:], kvng[:], reads=[kvng], writes=[kg])
            hbs = [P.sb(st, "hblk%d" % i, [128, 16, 512], BF16) for i in range(2)]
            cf = P.sb(st, "cf", [128, 8, 512], F32)
            sq = [P.sb(st, "sq%d" % i, [128, 512], F32) for i in range(2)]
            rs = [P.sb(st, "rs%d" % i, [128, 512], F32) for i in range(2)]
            tb = [P.sb(st, "tb%d" % i, [64, 512], F32) for i in range(4)]
            pj = [P.ps(st, "pj%d" % i, [128, 512], F32) for i in range(3)]
            pss = [P.ps(st, "pss%d" % i, [128, 512], F32) for i in range(2)]
            hv = hT.t.rearrange("(kc p) t -> p kc t", p=128)
            npj = 0
            for bi, (t0, t1) in enumerate(blocks(NB, 512)):
                nt = t1 - t0
                hb_ = hbs[bi % 2]
                P.dma("sp", hb_[:, :, 0:nt], hv[:, :, t0:t1], reads=[hT], writes=[hb_])
                for oc in range(8):
                    pb = pj[npj % 3]
                    npj += 1
                    for kc in range(16):
                        P.op("pe", lambda e: e.matmul(out=pb[:, 0:nt], lhsT=win[:, kc, oc * 128:(oc + 1) * 128],
                                                      rhs=hb_[:, kc, 0:nt], start=(kc == 0), stop=(kc == 15)),
                             reads=[win, hb_], writes=[pb])
                    P.op("act", lambda e: e.activation(out=cf[:, oc, 0:nt], in_=pb[:, 0:nt], func=AF.Copy),
                         reads=[pb], writes=[cf])
                if "rope" not in SKIP:
                    pk = pj[npj % 3]
                    npj += 1
                    pr = pj[npj % 3]
                    npj += 1
                    for kc in range(16):
                        P.op("pe", lambda e: e.matmul(out=pk[0:64, 0:nt], lhsT=win[:, kc, 1024:1088], rhs=hb_[:, kc, 0:nt],
                                                      start=(kc == 0), stop=(kc == 15)), reads=[win, hb_], writes=[pk])
                    for kc in range(16):
                        P.op("pe", lambda e: e.matmul(out=pr[0:64, 0:nt], lhsT=wrot[:, kc, :], rhs=hb_[:, kc, 0:nt],
                                                      start=(kc == 0), stop=(kc == 15)), reads=[wrot, hb_], writes=[pr])
                    cb, sb_ = tb[0], tb[1]
                    P.dma("sp", cb[:, 0:nt], cos2[:, t0:t1], reads=[cos2], writes=[cb])
                    P.dma("sp", sb_[:, 0:nt], sin2[:, t0:t1], reads=[sin2], writes=[sb_])
                    P.op("dve", lambda e: e.tensor_tensor(out=tb[2][:, 0:nt], in0=pk[0:64, 0:nt], in1=cb[:, 0:nt], op=ALU.mult),
                         reads=[pk, cb], writes=[tb[2]])
                    P.op("dve", lambda e: e.tensor_tensor(out=tb[3][:, 0:nt], in0=pr[0:64, 0:nt], in1=sb_[:, 0:nt], op=ALU.mult),
                         reads=[pr, sb_], writes=[tb[3]])
                    P.op("dve", lambda e: e.tensor_tensor(out=kr[:, t0:t1], in0=tb[2][:, 0:nt], in1=tb[3][:, 0:nt], op=ALU.add),
                         reads=[tb[2], tb[3]], writes=[kr])
                for gi, (c0, c1, nf, gt, dst) in enumerate([(0, 6, 768.0, qg, cqn), (6, 8, 256.0, kg, ckvn)]):
                    pssb = pss[gi]
                    for ci in range(c0, c1):
                        s_ = sq[ci % 2]
                        P.op("act", lambda e: e.activation(out=s_[:, 0:nt], in_=cf[:, ci, 0:nt], func=AF.Square),
                             reads=[cf], writes=[s_])
                        P.op("pe", lambda e: e.matmul(out=pssb[:, 0:nt], lhsT=C["ones_f"][:], rhs=s_[:, 0:nt],
                                                      start=(ci == c0), stop=(ci == c1 - 1)),
                             reads=[C["ones_f"], s_], writes=[pssb])
                    r_ = rs[gi]
                    P.op("act", lambda e: e.activation(out=r_[:, 0:nt], in_=pssb[:, 0:nt], func=AF.Sqrt,
                                                       scale=1.0 / nf, bias=C["eps"][:]), reads=[pssb, C["eps"]], writes=[r_])
                    P.op("dve", lambda e: e.reciprocal(out=r_[:, 0:nt], in_=r_[:, 0:nt]), reads=[r_], writes=[r_])
                    for ci in range(c0, c1):
                        P.op("dve", lambda e: e.scalar_tensor_tensor(out=dst[:, ci - c0, t0:t1], in0=cf[:, ci, 0:nt],
                                                                     scalar=gt[:, ci - c0:ci - c0 + 1], in1=r_[:, 0:nt],
                                                                     op0=ALU.mult, op1=ALU.mult),
                             reads=[cf, gt, r_], writes=[dst])
        if dbg is not None:
            P.dma("sp", dbg["cqn"][:], cqn[:], reads=[cqn], writes=[dbg["cqn"]])
            P.dma("sp", dbg["ckvn"][:], ckvn[:], reads=[ckvn], writes=[dbg["ckvn"]])
            P.dma("sp", dbg["kr"][:], kr[:], reads=[kr], writes=[dbg["kr"]])
        for hg in range(NHT // 8):
            with P.scope() as st:
                wuq = P.sb(st, "wuq", [128, 6, NH * 192], BF16)
                P.dma("pool", wuq[:], w_uq.t.rearrange("(kc p) n -> p kc n", p=128)[:, :, hg * 1536:(hg + 1) * 1536], reads=[w_uq], writes=[wuq])
                wuqr = P.sb(st, "wuqr", [128, 6, NH * 64], BF16)
                wv4 = wuq[:].rearrange("p k (h d) -> p k h d", d=192)
                wr4 = wuqr[:].rearrange("p k (h d) -> p k h d", d=64)
                for kc in range(6):
                    P.op("dve", lambda e: e.tensor_scalar(out=wr4[:, kc, :, 0:32], in0=wv4[:, kc, :, 160:192], scalar1=-1.0,
                                                          scalar2=None, op0=ALU.mult), reads=[wuq], writes=[wuqr])
                    P.op("dve", lambda e: e.tensor_copy(out=wr4[:, kc, :, 32:64], in_=wv4[:, kc, :, 128:160]), reads=[wuq],
                         writes=[wuqr])
                wukv = P.sb(st, "wukv", [128, 2, NH * 256], BF16)
                P.dma("pool", wukv[:], w_ukv.t.rearrange("(kc p) n -> p kc n", p=128)[:, :, hg * 2048:(hg + 1) * 2048], reads=[w_ukv], writes=[wukv])
                Kh = [P.sb(st, "Kh%d" % i, [128, NB], BF16) for i in range(2)]
                Vh = [P.sb(st, "Vh%d" % i, [128, 34, 128], BF16) for i in range(2)]
                Qn = [P.sb(st, "Qn%d" % i, [128, 512], BF16) for i in range(2)]
                Qp = [P.sb(st, "Qp%d" % i, [64, 512], BF16) for i in range(2)]
                tq = [P.sb(st, "tq%d" % i, [64, 512], F32) for i in range(4)]
                Pt = [P.sb(st, "Pt%d" % i, [128, 512], BF16) for i in range(3)]
                rl = [P.sb(st, "rl%d" % i, [128, 512], F32) for i in range(2)]
                Lacc = [P.sb(st, "Lacc%d" % i, [128, 512], F32) for i in range(2)]
                ob = [P.sb(st, "ob%d" % i, [128, 512], BF16) for i in range(2)]
                pS = [P.ps(st, "pS%d" % i, [128, 512], F32) for i in range(3)]
                pO = [P.ps(st, "pO%d" % i, [128, 512], F32) for i in range(2)]
                pL = [P.ps(st, "pL%d" % i, [128, 512], F32) for i in range(2)]
                pX = P.ps(st, "pX", [128, 512], F32)
                nS = 0
                nq = 0
                qblocks = [(0, 256, 0, 2)] + [(256 + 512 * i, 256 + 512 * (i + 1), 0, 34) for i in range(8)]
                for h in range(NH):
                    K_ = Kh[h % 2]
                    V_ = Vh[h % 2]
                    for (t0, t1) in blocks(NB, 512):
                        nt = t1 - t0
                        for kc in range(2):
                            P.op("pe", lambda e: e.matmul(out=pX[:, 0:nt], lhsT=wukv[:, kc, h * 256:h * 256 + 128],
                                                          rhs=ckvn[:, kc, t0:t1], start=(kc == 0), stop=(kc == 1)),
                                 reads=[wukv, ckvn], writes=[pX])
                        P.op("act", lambda e: e.activation(out=K_[:, t0:t1], in_=pX[:, 0:nt], func=AF.Copy),
                             reads=[pX], writes=[K_])
                    for g in range(0, 34, 4):
                        ng = min(4, 34 - g)
                        for j in range(ng):
                            tt = g + j
                            for kc in range(2):
                                P.op("pe", lambda e: e.matmul(out=pX[:, j * 128:(j + 1) * 128],
                                                              lhsT=ckvn[:, kc, tt * 128:(tt + 1) * 128],
                                                              rhs=wukv[:, kc, h * 256 + 128:h * 256 + 256],
                                                              start=(kc == 0), stop=(kc == 1)),
                                     reads=[wukv, ckvn], writes=[pX])
                        P.op("dve", lambda e: e.tensor_copy(out=V_[:, g:g + ng, :],
                                                            in_=pX[:, 0:ng * 128].rearrange("p (g d) -> p g d", d=128)),
                             reads=[pX], writes=[V_])
                    for (q0, q1, k0, k1) in qblocks:
                        nt = q1 - q0
                        Qn_, Qp_ = Qn[nq % 2], Qp[nq % 2]
                        pO_, pL_ = pO[nq % 2], pL[nq % 2]
                        rl_, ob_ = rl[nq % 2], ob[nq % 2]
                        La_ = Lacc[nq % 2]
                        nq += 1
                        pa = pS[nS % 3]; nS += 1
                        for kc in range(6):
                            P.op("pe", lambda e: e.matmul(out=pa[:, 0:nt], lhsT=wuq[:, kc, h * 192:h * 192 + 128],
                                                          rhs=cqn[:, kc, q0:q1], start=(kc == 0), stop=(kc == 5)),
                                 reads=[wuq, cqn], writes=[pa])
                        P.op("act", lambda e: e.activation(out=Qn_[:, 0:nt], in_=pa[:, 0:nt], func=AF.Copy),
                             reads=[pa], writes=[Qn_])
                        pb = pS[nS % 3]; nS += 1
                        for kc in range(6):
                            P.op("pe", lambda e: e.matmul(out=pb[0:64, 0:nt], lhsT=wuq[:, kc, h * 192 + 128:h * 192 + 192],
                                                          rhs=cqn[:, kc, q0:q1], start=(kc == 0), stop=(kc == 5)),
                                 reads=[wuq, cqn], writes=[pb])
                        pc = pS[nS % 3]; nS += 1
                        for kc in range(6):
                            P.op("pe", lambda e: e.matmul(out=pc[0:64, 0:nt], lhsT=wuqr[:, kc, h * 64:(h + 1) * 64],
                                                          rhs=cqn[:, kc, q0:q1], start=(kc == 0), stop=(kc == 5)),
                                 reads=[wuqr, cqn], writes=[pc])
                        cb, sb_ = tq[0], tq[1]
                        P.dma("sp", cb[:, 0:nt], cos2[:, q0:q1], reads=[cos2], writes=[cb])
                        P.dma("sp", sb_[:, 0:nt], sin2[:, q0:q1], reads=[sin2], writes=[sb_])
                        P.op("dve", lambda e: e.tensor_tensor(out=tq[2][:, 0:nt], in0=pb[0:64, 0:nt], in1=cb[:, 0:nt], op=ALU.mult),
                             reads=[pb, cb], writes=[tq[2]])
                        P.op("dve", lambda e: e.tensor_tensor(out=tq[3][:, 0:nt], in0=pc[0:64, 0:nt], in1=sb_[:, 0:nt], op=ALU.mult),
                             reads=[pc, sb_], writes=[tq[3]])
                        P.op("dve", lambda e: e.tensor_tensor(out=Qp_[:, 0:nt], in0=tq[2][:, 0:nt], in1=tq[3][:, 0:nt], op=ALU.add),
                             reads=[tq[2], tq[3]], writes=[Qp_])
                        AHEAD = 2
                        slots_ = {}

                        def issue_s(kt):
                            nonlocal nS
                            pS_ = pS[nS % 3]
                            Pt_ = Pt[nS % 3]
                            nS += 1
                            P.op("pe", lambda e: e.matmul(out=pS_[:, 0:nt], lhsT=K_[:, kt * 128:(kt + 1) * 128], rhs=Qn_[:, 0:nt],
                                                          start=True, stop=False), reads=[K_, Qn_], writes=[pS_])
                            P.op("pe", lambda e: e.matmul(out=pS_[:, 0:nt], lhsT=kr[:, kt * 128:(kt + 1) * 128], rhs=Qp_[:, 0:nt],
                                                          start=False, stop=True), reads=[kr, Qp_], writes=[pS_])
                            slots_[kt] = (pS_, Pt_)

                        for kt in range(k0, min(k1, k0 + AHEAD)):
                            issue_s(kt)
                        for kt in range(k0, k1):
                            if kt + AHEAD < k1:
                                issue_s(kt + AHEAD)
                            pS_, Pt_ = slots_.pop(kt)
                            P.op("act", lambda e: e.activation(out=Pt_[:, 0:nt], in_=pS_[:, 0:nt], func=AF.Exp, scale=scale),
                                 reads=[pS_], writes=[Pt_])
                            P.op("pe", lambda e: e.matmul(out=pO_[:, 0:nt], lhsT=V_[:, kt, :], rhs=Pt_[:, 0:nt],
                                                          start=(kt == k0), stop=(kt == k1 - 1)), reads=[V_, Pt_], writes=[pO_])
                            if kt == k0:
                                P.op("dve", lambda e: e.tensor_copy(out=La_[:, 0:nt], in_=Pt_[:, 0:nt]), reads=[Pt_], writes=[La_])
                            else:
                                P.op("dve", lambda e: e.tensor_tensor(out=La_[:, 0:nt], in0=La_[:, 0:nt], in1=Pt_[:, 0:nt],
                                                                      op=ALU.add), reads=[La_, Pt_], writes=[La_])
                        P.op("pe", lambda e: e.matmul(out=pL_[:, 0:nt], lhsT=C["ones_f"][:], rhs=La_[:, 0:nt], start=True,
                                                      stop=True), reads=[C["ones_f"], La_], writes=[pL_])
                        P.op("dve", lambda e: e.reciprocal(out=rl_[:, 0:nt], in_=pL_[:, 0:nt]), reads=[pL_], writes=[rl_])
                        P.op("dve", lambda e: e.tensor_tensor(out=ob_[:, 0:nt], in0=pO_[:, 0:nt], in1=rl_[:, 0:nt], op=ALU.mult),
                             reads=[pO_, rl_], writes=[ob_])
                        P.dma("sp", oT[(hg * 8 + h) * 128:(hg * 8 + h + 1) * 128, q0:q1], ob_[:, 0:nt], reads=[ob_], writes=[oT])


def phase_gla(P, C, hT, w_in, ga, gb, gbias, ng, oT, oscr, NHL=2):
    NT = 34
    with P.scope() as st0:
        ident = identity(P, st0, C, BF16, "identb")
        triF = tri_mask(P, st0, "triF", F32, -1.0 / 16.0, 1)
        triB = tri_mask(P, st0, "triB", F32, -1.0 / 16.0, -1)
        mskF = tri_mask(P, st0, "mskF", F32, 1.0, 1)
        mskB = tri_mask(P, st0, "mskB", F32, 1.0, -1)
        uT = [P.sb(st0, "uT%d" % d, [17, NB], BF16) for d in range(2)]
        gbw = [P.sb(st0, "gbw%d" % d, [17, NHL * 256], BF16) for d in range(2)]
        ngb = P.sb(st0, "ngb", [128, 512], F32)
        hv = hT.t.rearrange("(kc p) t -> p kc t", p=128)
        with P.scope() as st:
            gaw = P.sb(st, "gaw", [128, 16, 32], BF16)
            P.dma("pool", gaw[:], ga.t.rearrange("(kc p) n -> p kc n", p=128), reads=[ga], writes=[gaw])
            for d in range(2):
                P.dma("pool", gbw[d][0:16, :], gb[d], reads=[gb], writes=[gbw[d]])
                P.dma("pool", gbw[d][16:17, :], gbias[d], reads=[gbias], writes=[gbw[d]])
                P.op("dve", lambda e: e.memset(uT[d][:], 1.0), writes=[uT[d]])
            ngr = P.sb(st, "ngr", [1, 512], F32)
            P.dma("sp", ngr[:], ng[:], reads=[ng], writes=[ngr])
            pu = [P.ps(st, "pu%d" % i, [128, 512], F32) for i in range(2)]
            P.op("pe", lambda e: e.matmul(out=pu[0][:, :], lhsT=C["ones_f"][0:1, :], rhs=ngr[:], start=True, stop=True),
                 reads=[C["ones_f"], ngr], writes=[pu[0]])
            P.op("act", lambda e: e.activation(out=ngb[:], in_=pu[0][:, :], func=AF.Copy), reads=[pu[0]], writes=[ngb])
            hbs = [P.sb(st, "hblk%d" % i, [128, 16, 512], BF16) for i in range(2)]
            for bi, (t0, t1) in enumerate(blocks(NB, 512)):
                nt = t1 - t0
                hb_ = hbs[bi % 2]
                P.dma("sp", hb_[:, :, 0:nt], hv[:, :, t0:t1], reads=[hT], writes=[hb_])
                for d in range(2):
                    for kc in range(16):
                        P.op("pe", lambda e: e.matmul(out=pu[d][0:16, 0:nt], lhsT=gaw[:, kc, d * 16:(d + 1) * 16],
                                                      rhs=hb_[:, kc, 0:nt], start=(kc == 0), stop=(kc == 15)),
                             reads=[gaw, hb_], writes=[pu[d]])
                    P.op("act", lambda e: e.activation(out=uT[d][0:16, t0:t1], in_=pu[d][0:16, 0:nt], func=AF.Copy),
                         reads=[pu[d]], writes=[uT[d]])
        for hl in range(NHL):
            with P.scope() as st1:
                qT = P.sb(st1, "qT", [128, 2, NB], BF16)
                kT = P.sb(st1, "kT", [128, 2, NB], BF16)
                vv = P.sb(st1, "vv", [128, NT, 512], BF16)
                rr = P.sb(st1, "rr", [128, NT, 512], BF16)
                for sub in range(2):
                    if "proj" in SKIP:
                        continue
                    with P.scope() as st:
                        ncols = 512 if sub == 0 else 1024
                        c_off = hl * 1536 + (0 if sub == 0 else 512)
                        wh = P.sb(st, "wh", [128, 16, ncols], BF16)
                        P.dma("pool", wh[:], w_in.t.rearrange("(kc p) n -> p kc n", p=128)[:, :, c_off:c_off + ncols],
                              reads=[w_in], writes=[wh])
                        hbs = [P.sb(st, "hblk%d" % i, [128, 16, 256], BF16) for i in range(2)]
                        pp = [P.ps(st, "pp%d" % i, [128, 512], F32) for i in range(4)]
                        npp = 0
                        for bi, (t0, t1) in enumerate(blocks(NB, 256)):
                            nt = t1 - t0
                            hb_ = hbs[bi % 2]
                            P.dma("sp", hb_[:, :, 0:nt], hv[:, :, t0:t1], reads=[hT], writes=[hb_])
                            if sub == 0:
                                for oc in range(4):
                                    pb = pp[npp % 4]; npp += 1
                                    for kc in range(16):
                                        P.op("pe", lambda e: e.matmul(out=pb[:, 0:nt], lhsT=wh[:, kc, oc * 128:(oc + 1) * 128],
                                                                      rhs=hb_[:, kc, 0:nt], start=(kc == 0), stop=(kc == 15)),
                                             reads=[wh, hb_], writes=[pb])
                                    if oc < 2:
                                        P.op("act", lambda e: e.activation(out=qT[:, oc, t0:t1], in_=pb[:, 0:nt], func=AF.Copy,
                                                                           scale=1.0 / 16.0), reads=[pb], writes=[qT])
                                    else:
                                        P.op("dve", lambda e: e.tensor_copy(out=kT[:, oc - 2, t0:t1], in_=pb[:, 0:nt]),
                                             reads=[pb], writes=[kT])
                            else:
                                for j in range(nt // 128):
                                    tt = t0 // 128 + j
                                    pb = pp[npp % 4]; npp += 1
                                    for kc in range(16):
                                        P.op("pe", lambda e: e.matmul(out=pb[:, :], lhsT=hb_[:, kc, j * 128:(j + 1) * 128],
                                                                      rhs=wh[:, kc, 0:512], start=(kc == 0), stop=(kc == 15)),
                                             reads=[wh, hb_], writes=[pb])
                                    P.op("dve", lambda e: e.tensor_copy(out=vv[:, tt, :], in_=pb[:, :]), reads=[pb], writes=[vv])
                                    pb = pp[npp % 4]; npp += 1
                                    for kc in range(16):
                                        P.op("pe", lambda e: e.matmul(out=pb[:, :], lhsT=hb_[:, kc, j * 128:(j + 1) * 128],
                                                                      rhs=wh[:, kc, 512:1024], start=(kc == 0), stop=(kc == 15)),
                                             reads=[wh, hb_], writes=[pb])
                                    P.op("act", lambda e: e.activation(out=rr[:, tt, :], in_=pb[:, :], func=AF.Silu),
                                         reads=[pb], writes=[rr])
                with P.scope() as st:
                    S = P.sb(st, "S", [128, 2, 512], F32)
                    Sb = P.sb(st, "Sb", [128, 2, 512], BF16)
                    e1 = [P.sb(st, "e1_%d" % i, [128, 256], F32) for i in range(2)]
                    bl = [P.sb(st, "bl%d" % i, [128, 2], F32) for i in range(2)]
                    el = [P.sb(st, "el%d" % i, [128, 2], F32) for i in range(2)]
                    E1 = [P.sb(st, "E1_%d" % i, [128, 2, 128], F32) for i in range(2)]
                    E2 = [P.sb(st, "E2_%d" % i, [128, 2, 128], F32) for i in range(2)]
                    E3 = [P.sb(st, "E3_%d" % i, [128, 2, 128], F32) for i in range(2)]
                    qt = [P.sb(st, "qt%d" % i, [128, 2, 128], BF16) for i in range(2)]
                    kt_ = [P.sb(st, "kt%d" % i, [128, 2, 128], BF16) for i in range(2)]
                    kh = [P.sb(st, "kh%d" % i, [128, 2, 128], BF16) for i in range(2)]
                    khT = [P.sb(st, "khT%d" % i, [128, 256], BF16) for i in range(2)]
                    At = [P.sb(st, "At%d" % i, [128, 128], BF16) for i in range(2)]
                    of_ = [P.sb(st, "of%d" % i, [128, 512], F32) for i in range(2)]
                    osum = [P.sb(st, "osum%d" % i, [128, 512], F32) for i in range(2)]
                    junk = P.sb(st, "junk", [128, 512], F32)
                    ss = [P.sb(st, "ss%d" % i, [128, 1], F32) for i in range(2)]
                    on = [P.sb(st, "on%d" % i, [128, 512], BF16) for i in range(2)]
                    onT = [P.sb(st, "onT%d" % i, [128, 4, 128], BF16) for i in range(2)]
                    pg = P.ps(st, "pg", [128, 512], F32)
                    pbT = P.ps(st, "pbT", [128, 2, 128], F32)
                    pA = P.ps(st, "pA", [128, 512], F32)
                    po = [P.ps(st, "po%d" % i, [128, 512], F32) for i in range(2)]
                    pdS = [P.ps(st, "pdS%d" % i, [128, 512], F32) for i in range(2)]
                    ptr = P.ps(st, "ptr", [128, 1024], BF16)
                    for d in range(2):
                        if "scan" in SKIP:
                            continue
                        tri = triF if d == 0 else triB
                        msk = mskF if d == 0 else mskB
                        lastcol = 127 if d == 0 else 0
                        order = list(range(NT)) if d == 0 else [1, 0] + list(range(NT - 1, 1, -1))
                        P.op("dve", lambda e: e.memset(S[:], 0.0), writes=[S])
                        P.op("dve", lambda e: e.memset(Sb[:], 0.0), writes=[Sb])
                        def part1(ci, tt):
                                i2 = ci % 2
                                ts = slice(tt * 128, (tt + 1) * 128)
                                P.op("pe", lambda e: e.matmul(out=pg[:, 0:256], lhsT=uT[d][0:17, ts],
                                                              rhs=gbw[d][0:17, hl * 256:(hl + 1) * 256], start=True, stop=True),
                                     reads=[uT[d], gbw[d]], writes=[pg])
                                P.op("act", lambda e: e.activation(out=e1[i2][:], in_=pg[:, 0:256], func=AF.Exp, scale=-1.0),
                                     reads=[pg], writes=[e1[i2]])
                                P.op("act", lambda e: e.activation(out=e1[i2][:], in_=e1[i2][:], func=AF.Ln, bias=C["one"][:]),
                                     reads=[e1[i2], C["one"]], writes=[e1[i2]])
                                for dk in range(2):
                                    P.op("pe", lambda e: e.matmul(out=pbT[:, dk, :], lhsT=e1[i2][:, dk * 128:(dk + 1) * 128],
                                                                  rhs=tri[:], start=True, stop=True),
                                         reads=[e1[i2], tri], writes=[pbT])
                                P.op("dve", lambda e: e.tensor_copy(out=bl[i2][:], in_=pbT[:, :, lastcol]), reads=[pbT],
                                     writes=[bl[i2]])
                                for dk in range(2):
                                    P.op("act", lambda e: e.activation(out=E1[i2][:, dk, :], in_=pbT[:, dk, :], func=AF.Exp),
                                         reads=[pbT], writes=[E1[i2]])
                                    P.op("act", lambda e: e.activation(out=E2[i2][:, dk, :], in_=pbT[:, dk, :], func=AF.Exp,
                                                                       scale=-1.0), reads=[pbT], writes=[E2[i2]])
                                for dk in range(2):
                                    P.op("act", lambda e: e.activation(out=E3[i2][:, dk, :], in_=pbT[:, dk, :], func=AF.Exp,
                                                                       scale=-1.0, bias=bl[i2][:, dk:dk + 1]),
                                         reads=[pbT, bl[i2]], writes=[E3[i2]])
                                P.op("act", lambda e: e.activation(out=el[i2][:], in_=bl[i2][:], func=AF.Exp), reads=[bl[i2]],
                                     writes=[el[i2]])
                                P.op("dve", lambda e: e.tensor_tensor(out=qt[i2][:], in0=qT[:, :, ts], in1=E1[i2][:], op=ALU.mult),
                                     reads=[qT, E1[i2]], writes=[qt[i2]])
                                P.op("pool", lambda e: e.tensor_tensor(out=kt_[i2][:], in0=kT[:, :, ts], in1=E2[i2][:], op=ALU.mult),
                                     reads=[kT, E2[i2]], writes=[kt_[i2]])
                                P.op("pool", lambda e: e.tensor_tensor(out=kh[i2][:], in0=kT[:, :, ts], in1=E3[i2][:], op=ALU.mult),
                                     reads=[kT, E3[i2]], writes=[kh[i2]])
                                for dk in range(2):
                                    P.op("pe", lambda e: e.transpose(out=ptr[:, dk * 128:(dk + 1) * 128], in_=kh[i2][:, dk, :],
                                                                     identity=ident[:]), reads=[kh[i2], ident], writes=[ptr])
                                P.op("dve", lambda e: e.tensor_copy(out=khT[i2][:], in_=ptr[:, 0:256]), reads=[ptr],
                                     writes=[khT[i2]])
                                for dk in range(2):
                                    P.op("pe", lambda e: e.matmul(out=pA[:, 0:128], lhsT=kt_[i2][:, dk, :], rhs=qt[i2][:, dk, :],
                                                                  start=(dk == 0), stop=(dk == 1)),
                                         reads=[kt_[i2], qt[i2]], writes=[pA])
                                P.op("dve", lambda e: e.tensor_tensor(out=At[i2][:], in0=pA[:, 0:128], in1=msk[:], op=ALU.mult),
                                     reads=[pA, msk], writes=[At[i2]])

                        def part2(ci, tt):
                                i2 = ci % 2
                                ts = slice(tt * 128, (tt + 1) * 128)
                                po_ = po[i2]
                                for dk in range(2):
                                    P.op("pe", lambda e: e.matmul(out=po_[:, :], lhsT=qt[i2][:, dk, :], rhs=Sb[:, dk, :],
                                                                  start=(dk == 0), stop=False), reads=[qt[i2], Sb], writes=[po_])
                                P.op("pe", lambda e: e.matmul(out=po_[:, :], lhsT=At[i2][:], rhs=vv[:, tt, :], start=False,
                                                              stop=True), reads=[At[i2], vv], writes=[po_])
                                for dk in range(2):
                                    P.op("pe", lambda e: e.matmul(out=pdS[dk][:, :], lhsT=khT[i2][:, dk * 128:(dk + 1) * 128],
                                                                  rhs=vv[:, tt, :], start=True, stop=True),
                                         reads=[khT[i2], vv], writes=[pdS[dk]])
                                for dk in range(2):
                                    P.op("dve", lambda e: e.scalar_tensor_tensor(out=S[:, dk, :], in0=S[:, dk, :],
                                                                                 scalar=el[i2][:, dk:dk + 1], in1=pdS[dk][:, :],
                                                                                 op0=ALU.mult, op1=ALU.add),
                                         reads=[S, el[i2], pdS[dk]], writes=[S])
                                P.op("act", lambda e: e.activation(out=Sb[:], in_=S[:], func=AF.Copy), reads=[S], writes=[Sb])
                                if d == 0:
                                    P.op("act", lambda e: e.activation(out=of_[i2][:], in_=po_[:, :], func=AF.Copy),
                                         reads=[po_], writes=[of_[i2]])
                                    P.dma("sp", oscr[ts, :], of_[i2][:], reads=[of_[i2]], writes=[oscr])
                                else:
                                    P.dma("sp", of_[i2][:], oscr[ts, :], reads=[oscr], writes=[of_[i2]])
                                    P.op("dve", lambda e: e.tensor_tensor(out=osum[i2][:], in0=po_[:, :], in1=of_[i2][:],
                                                                          op=ALU.add), reads=[po_, of_[i2]], writes=[osum[i2]])
                                    P.op("dve", lambda e: e.tensor_tensor(out=junk[:], in0=osum[i2][:], in1=osum[i2][:], op=ALU.mult),
                                         reads=[osum[i2]], writes=[junk])
                                    P.op("dve", lambda e: e.tensor_reduce(out=ss[i2][:], in_=junk[:], axis=AX.X, op=ALU.add),
                                         reads=[junk], writes=[ss[i2]])
                                    P.op("act", lambda e: e.activation(out=ss[i2][:], in_=ss[i2][:], func=AF.Sqrt,
                                                                       scale=1.0 / 512.0, bias=C["eps"][:]),
                                         reads=[ss[i2], C["eps"]], writes=[ss[i2]])
                                    P.op("dve", lambda e: e.reciprocal(out=ss[i2][:], in_=ss[i2][:]), reads=[ss[i2]],
                                         writes=[ss[i2]])
                                    P.op("dve", lambda e: e.scalar_tensor_tensor(out=osum[i2][:], in0=osum[i2][:],
                                                                                 scalar=ss[i2][:, 0:1], in1=ngb[:],
                                                                                 op0=ALU.mult, op1=ALU.mult),
                                         reads=[osum[i2], ss[i2], ngb], writes=[osum[i2]])
                                    P.op("dve", lambda e: e.tensor_tensor(out=on[i2][:], in0=osum[i2][:], in1=rr[:, tt, :],
                                                                          op=ALU.mult), reads=[osum[i2], rr], writes=[on[i2]])
                                    for dv in range(4):
                                        P.op("pe", lambda e: e.transpose(out=ptr[:, 256 + dv * 128:256 + (dv + 1) * 128],
                                                                         in_=on[i2][:, dv * 128:(dv + 1) * 128],
                                                                         identity=ident[:]), reads=[on[i2], ident], writes=[ptr])
                                    P.op("act", lambda e: e.activation(out=onT[i2][:], in_=ptr[:, 256:768].rearrange(
                                        "p (a b) -> p a b", b=128), func=AF.Copy), reads=[ptr], writes=[onT[i2]])
                                    P.dma("sp", oT[hl * 512:(hl + 1) * 512, ts].rearrange("(a p) t -> p a t", p=128),
                                          onT[i2][:], reads=[onT[i2]], writes=[oT])


                        part1(0, order[0])
                        for ci, tt in enumerate(order):
                            if ci + 1 < len(order):
                                part1(ci + 1, order[ci + 1])
                            part2(ci, tt)


def ln_block(P, C, st_bufs, z, c0, nt, out_fn, pst):
    sq, mean, rstd, tmp = st_bufs
    ps_s, ps_q = pst
    for fc in range(16):
        s_ = sq[fc % 2]
        P.op("act", lambda e: e.activation(out=s_[:, 0:nt], in_=z[:, fc, c0:c0 + nt], func=AF.Square), reads=[z], writes=[s_])
        P.op("pe", lambda e: e.matmul(out=ps_s[:, 0:nt], lhsT=C["ones_f"][:], rhs=z[:, fc, c0:c0 + nt], start=(fc == 0),
                                      stop=(fc == 15)), reads=[C["ones_f"], z], writes=[ps_s])
        P.op("pe", lambda e: e.matmul(out=ps_q[:, 0:nt], lhsT=C["ones_f"][:], rhs=s_[:, 0:nt], start=(fc == 0),
                                      stop=(fc == 15)), reads=[C["ones_f"], s_], writes=[ps_q])
    P.op("act", lambda e: e.activation(out=mean[:, 0:nt], in_=ps_s[:, 0:nt], func=AF.Copy, scale=1.0 / D),
         reads=[ps_s], writes=[mean])
    P.op("dve", lambda e: e.tensor_tensor(out=rstd[:, 0:nt], in0=mean[:, 0:nt], in1=mean[:, 0:nt], op=ALU.mult),
         reads=[mean], writes=[rstd])
    P.op("dve", lambda e: e.scalar_tensor_tensor(out=rstd[:, 0:nt], in0=ps_q[:, 0:nt], scalar=1.0 / D, in1=rstd[:, 0:nt],
                                                 op0=ALU.mult, op1=ALU.subtract), reads=[ps_q, rstd], writes=[rstd])
    P.op("act", lambda e: e.activation(out=rstd[:, 0:nt], in_=rstd[:, 0:nt], func=AF.Sqrt, bias=C["epsa"][:]),
         reads=[rstd, C["epsa"]], writes=[rstd])
    P.op("dve", lambda e: e.reciprocal(out=rstd[:, 0:nt], in_=rstd[:, 0:nt]), reads=[rstd], writes=[rstd])
    for fc in range(16):
        t_ = tmp[fc % 2]
        P.op("dve", lambda e: e.tensor_tensor(out=t_[:, 0:nt], in0=z[:, fc, c0:c0 + nt], in1=mean[:, 0:nt], op=ALU.subtract),
             reads=[z, mean], writes=[t_])
        P.op("dve", lambda e: e.tensor_tensor(out=t_[:, 0:nt], in0=t_[:, 0:nt], in1=rstd[:, 0:nt], op=ALU.mult),
             reads=[t_, rstd], writes=[t_])
        out_fn(fc, t_)


def phase_c1(P, C, xT, mods_d, oTf, w_o, lng, lnb, wr, br, x1T, h2T, wgT, N=NOWN, ctxn=128, h2tok=None, rt=None):
    with P.scope() as st:
        wo = P.sb(st, "wo", [128, 16, D], BF16)
        P.dma("pool", wo[:], w_o.t.rearrange("(kc p) n -> p kc n", p=128), reads=[w_o], writes=[wo])
        mods = P.sb(st, "mods", [128, 96, 2], F32)
        P.dma("sp", mods[:], mods_d[:], reads=[mods_d], writes=[mods])
        g1a = P.sb(st, "g1a", [128, 16, 2], F32)
        P.op("dve", lambda e: e.tensor_scalar(out=g1a[:], in0=mods[:, 32:48, :], scalar1=1.0 / ALPHA, scalar2=None,
                                              op0=ALU.mult), reads=[mods], writes=[g1a])
        sc2p = P.sb(st, "sc2p", [128, 16, 2], F32)
        P.op("dve", lambda e: e.tensor_scalar(out=sc2p[:], in0=mods[:, 64:80, :], scalar1=1.0, scalar2=None, op0=ALU.add),
             reads=[mods], writes=[sc2p])
        g_ = P.sb(st, "lng", [128, 16], F32)
        b_ = P.sb(st, "lnb", [128, 16], F32)
        P.dma("sp", g_[:], lng[:], reads=[lng], writes=[g_])
        P.dma("sp", b_[:], lnb[:], reads=[lnb], writes=[b_])
        wrt = P.sb(st, "wrt", [128, 16, 36], F32)
        P.dma("sp", wrt[:], wr.t.rearrange("(kc p) n -> p kc n", p=128), reads=[wr], writes=[wrt])
        brr = P.sb(st, "brr", [1, 36], F32)
        P.dma("sp", brr[:], br[:], reads=[br], writes=[brr])
        brb = P.sb(st, "brb", [128, 36], F32)
        identf = identity(P, st, C, F32, "identf")
        if h2tok is not None:
            identb = identity(P, st, C, BF16, "identb1")
            ptb = P.ps(st, "ptb", [128, 1024], BF16)
            htk = [P.sb(st, "htk%d" % i, [128, D], BF16) for i in range(2)]
            rts = [P.sb(st, "rts%d" % i, [128, 68], F32) for i in range(2)]
            ntk = 0
        BS = 256
        ob = [P.sb(st, "ob%d" % i, [128, 16, BS], BF16) for i in range(2)]
        xb = [P.sb(st, "xb%d" % i, [128, 16, BS], F32) for i in range(2)]
        z = P.sb(st, "z", [128, 16, BS], F32)
        x1 = P.sb(st, "x1", [128, 16, BS], F32)
        h2f = P.sb(st, "h2f", [128, 16, BS], F32)
        h2b = P.sb(st, "h2b", [128, 16, BS], BF16)
        sq = [P.sb(st, "sq%d" % i, [128, BS], F32) for i in range(2)]
        tmp = [P.sb(st, "tmp%d" % i, [128, BS], F32) for i in range(2)]
        mean = P.sb(st, "mean", [128, BS], F32)
        rstd = P.sb(st, "rstd", [128, BS], F32)
        lg = P.sb(st, "lg", [128, 36], F32)
        sm = [P.sb(st, "sm%d" % i, [128, 40], F32) for i in range(6)]
        wg = P.sb(st, "wg", [128, 32], F32)
        wgt_sb = P.sb(st, "wgt_sb", [32, BS], F32)
        py = [P.ps(st, "py%d" % i, [128, 512], F32) for i in range(2)]
        ps_s = P.ps(st, "ps_s", [128, 512], F32)
        ps_q = P.ps(st, "ps_q", [128, 512], F32)
        plg = P.ps(st, "plg", [128, 512], F32)
        pwt = P.ps(st, "pwt", [128, 512], F32)
        P.op("pe", lambda e: e.matmul(out=plg[:, 0:36], lhsT=C["ones_f"][0:1, :], rhs=brr[:], start=True, stop=True),
             reads=[C["ones_f"], brr], writes=[plg])
        P.op("act", lambda e: e.activation(out=brb[:], in_=plg[:, 0:36], func=AF.Copy), reads=[plg], writes=[brb])
        ov = oTf.t.rearrange("(kc p) t -> p kc t", p=128)
        xv = xT.t.rearrange("(kc p) t -> p kc t", p=128)
        x1v = x1T.t.rearrange("(kc p) t -> p kc t", p=128)
        h2v = h2T.t.rearrange("(kc p) t -> p kc t", p=128)
        for bi, (t0, t1) in enumerate(blocks(N, BS)):
            nt = t1 - t0
            sg = [(a - t0, b - t0, j) for (a, b, j) in segs_own(t0, t1, ctxn)]
            ob_, xb_ = ob[bi % 2], xb[bi % 2]
            P.dma("sp", ob_[:, :, 0:nt], ov[:, :, t0:t1], reads=[oTf], writes=[ob_])
            P.dma("sp", xb_[:, :, 0:nt], xv[:, :, t0:t1], reads=[xT], writes=[xb_])
            for fc in range(16):
                pb = py[fc % 2]
                for kc in range(16):
                    P.op("pe", lambda e: e.matmul(out=pb[:, 0:nt], lhsT=wo[:, kc, fc * 128:(fc + 1) * 128], rhs=ob_[:, kc, 0:nt],
                                                  start=(kc == 0), stop=(kc == 15)), reads=[wo, ob_], writes=[pb])
                for (a, b, j) in sg:
                    P.op("dve", lambda e: e.scalar_tensor_tensor(out=z[:, fc, a:b], in0=pb[:, a:b], scalar=g1a[:, fc, j:j + 1],
                                                                 in1=xb_[:, fc, a:b], op0=ALU.mult, op1=ALU.add),
                         reads=[pb, g1a, xb_], writes=[z])

            def outf(fc, t_):
                P.op("act", lambda e: e.activation(out=x1[:, fc, 0:nt], in_=t_[:, 0:nt], func=AF.Identity,
                                                   scale=g_[:, fc:fc + 1], bias=b_[:, fc:fc + 1]), reads=[t_, g_, b_], writes=[x1])
                for (a, b, j) in sg:
                    P.op("act", lambda e: e.activation(out=h2f[:, fc, a:b], in_=x1[:, fc, a:b], func=AF.Identity,
                                                       scale=sc2p[:, fc, j:j + 1], bias=mods[:, 48 + fc, j:j + 1]),
                         reads=[x1, sc2p, mods], writes=[h2f])
                P.op("pool", lambda e: e.tensor_copy(out=h2b[:, fc, 0:nt], in_=h2f[:, fc, 0:nt]), reads=[h2f], writes=[h2b])

            ln_block(P, C, (sq, mean, rstd, tmp), z, 0, nt, outf, (ps_s, ps_q))
            P.dma("sp", x1v[:, :, t0:t1], x1[:, :, 0:nt], reads=[x1], writes=[x1T])
            P.dma("sp", h2v[:, :, t0:t1], h2b[:, :, 0:nt], reads=[h2b], writes=[h2T])
            for j in range(nt // 128):
                for kc in range(16):
                    P.op("pe", lambda e: e.matmul(out=plg[:, 0:36], lhsT=h2f[:, kc, j * 128:(j + 1) * 128], rhs=wrt[:, kc, :],
                                                  start=(kc == 0), stop=(kc == 15)), reads=[h2f, wrt], writes=[plg])
                P.op("dve", lambda e: e.tensor_tensor(out=lg[:], in0=plg[:, 0:36], in1=brb[:], op=ALU.add), reads=[plg, brb],
                     writes=[lg])
                gmax, gsum, goh, m1, m2, wk = sm
                P.op("dve", lambda e: e.tensor_reduce(out=gmax[:, 0:1], in_=lg[:, 0:4], axis=AX.X, op=ALU.max), reads=[lg],
                     writes=[gmax])
                P.op("dve", lambda e: e.tensor_scalar(out=goh[:, 0:4], in0=lg[:, 0:4], scalar1=gmax[:, 0:1], scalar2=None,
                                                      op0=ALU.is_ge), reads=[lg, gmax], writes=[goh])
                P.op("dve", lambda e: e.tensor_scalar(out=gsum[:, 0:4], in0=lg[:, 0:4], scalar1=gmax[:, 0:1], scalar2=None,
                                                      op0=ALU.subtract), reads=[lg, gmax], writes=[gsum])
                P.op("act", lambda e: e.activation(out=gsum[:, 0:4], in_=gsum[:, 0:4], func=AF.Exp), reads=[gsum], writes=[gsum])
                P.op("dve", lambda e: e.tensor_reduce(out=gsum[:, 4:5], in_=gsum[:, 0:4], axis=AX.X, op=ALU.add),
                     reads=[gsum], writes=[gsum])
                P.op("dve", lambda e: e.reciprocal(out=gsum[:, 5:6], in_=gsum[:, 4:5]), reads=[gsum], writes=[gsum])
                P.op("dve", lambda e: e.tensor_scalar(out=goh[:, 4:8], in0=goh[:, 0:4], scalar1=-1.0, scalar2=1e30,
                                                      op0=ALU.add, op1=ALU.mult), reads=[goh], writes=[goh])
                for g in range(4):
                    P.op("dve", lambda e: e.tensor_scalar(out=m1[:, g * 8:(g + 1) * 8], in0=lg[:, 4 + g * 8:12 + g * 8],
                                                          scalar1=goh[:, 4 + g:5 + g], scalar2=None, op0=ALU.add),
                         reads=[lg, goh], writes=[m1])
                P.op("dve", lambda e: e.tensor_reduce(out=m1[:, 32:33], in_=m1[:, 0:32], axis=AX.X, op=ALU.max), reads=[m1],
                     writes=[m1])
                P.op("dve", lambda e: e.tensor_scalar(out=m2[:, 0:32], in0=m1[:, 0:32], scalar1=m1[:, 32:33], scalar2=None,
                                                      op0=ALU.is_ge), reads=[m1], writes=[m2])
                P.op("dve", lambda e: e.scalar_tensor_tensor(out=wk[:, 0:32], in0=m2[:, 0:32], scalar=-1e30, in1=m1[:, 0:32],
                                                             op0=ALU.mult, op1=ALU.add), reads=[m2, m1], writes=[wk])
                P.op("dve", lambda e: e.tensor_reduce(out=wk[:, 32:33], in_=wk[:, 0:32], axis=AX.X, op=ALU.max), reads=[wk],
                     writes=[wk])
                P.op("dve", lambda e: e.tensor_scalar(out=wk[:, 0:32], in0=wk[:, 0:32], scalar1=wk[:, 32:33], scalar2=None,
                                                      op0=ALU.is_ge), reads=[wk], writes=[wk])
                P.op("dve", lambda e: e.tensor_tensor(out=wk[:, 33:34], in0=wk[:, 32:33], in1=m1[:, 32:33], op=ALU.subtract),
                     reads=[wk, m1], writes=[wk])
                P.op("act", lambda e: e.activation(out=wk[:, 33:34], in_=wk[:, 33:34], func=AF.Exp), reads=[wk], writes=[wk])
                P.op("dve", lambda e: e.tensor_scalar(out=wk[:, 33:34], in0=wk[:, 33:34], scalar1=1.0, scalar2=None,
                                                      op0=ALU.add), reads=[wk], writes=[wk])
                P.op("dve", lambda e: e.reciprocal(out=wk[:, 34:35], in_=wk[:, 33:34]), reads=[wk], writes=[wk])
                P.op("dve", lambda e: e.tensor_tensor(out=wk[:, 34:35], in0=wk[:, 34:35], in1=gsum[:, 5:6], op=ALU.mult),
                     reads=[wk, gsum], writes=[wk])
                P.op("dve", lambda e: e.tensor_tensor(out=wk[:, 35:36], in0=gsum[:, 5:6], in1=wk[:, 34:35], op=ALU.subtract),
                     reads=[wk, gsum], writes=[wk])
                P.op("dve", lambda e: e.tensor_scalar(out=wg[:], in0=m2[:, 0:32], scalar1=wk[:, 34:35], scalar2=None,
                                                      op0=ALU.mult), reads=[m2, wk], writes=[wg])
                P.op("dve", lambda e: e.scalar_tensor_tensor(out=wg[:], in0=wk[:, 0:32], scalar=wk[:, 35:36], in1=wg[:],
                                                             op0=ALU.mult, op1=ALU.add), reads=[wk, wg], writes=[wg])
                P.op("pe", lambda e: e.transpose(out=pwt[0:32, j * 128:(j + 1) * 128], in_=wg[:], identity=identf[:]),
                     reads=[wg, identf], writes=[pwt])
                if h2tok is not None:
                    tk0 = t0 + j * 128
                    rts_, htk_ = rts[ntk % 2], htk[ntk % 2]
                    ntk += 1
                    P.op("dve", lambda e: e.tensor_copy(out=rts_[:, 0:32], in_=m2[:, 0:32]), reads=[m2], writes=[rts_])
                    P.op("dve", lambda e: e.tensor_copy(out=rts_[:, 32:64], in_=wk[:, 0:32]), reads=[wk], writes=[rts_])
                    P.op("dve", lambda e: e.tensor_copy(out=rts_[:, 64:66], in_=wk[:, 34:36]), reads=[wk], writes=[rts_])
                    P.op("dve", lambda e: e.tensor_copy(out=rts_[:, 66:68], in_=wk[:, 34:36]), reads=[wk], writes=[rts_])
                    P.dma("sp", rt[tk0:tk0 + 128, :], rts_[:], reads=[rts_], writes=[rt])
                    for hh in range(2):
                        for q in range(8):
                            fc = hh * 8 + q
                            P.op("pe", lambda e: e.transpose(out=ptb[:, q * 128:(q + 1) * 128], in_=h2b[:, fc, j * 128:(j + 1) * 128],
                                                             identity=identb[:]), reads=[h2b, identb], writes=[ptb])
                        P.op("act", lambda e: e.activation(out=htk_[:, hh * 1024:(hh + 1) * 1024], in_=ptb[:, :], func=AF.Copy),
                             reads=[ptb], writes=[htk_])
                    P.dma("sp", h2tok[tk0:tk0 + 128, :], htk_[:], reads=[htk_], writes=[h2tok])
            P.op("act", lambda e: e.activation(out=wgt_sb[:, 0:nt], in_=pwt[0:32, 0:nt], func=AF.Copy), reads=[pwt],
                 writes=[wgt_sb])
            P.dma("sp", wgT[:, t0:t1], wgt_sb[:, 0:nt], reads=[wgt_sb], writes=[wgT])


def phase_c2(P, C, x1T, h2T, wgT, mods_d, w1, w3, w2, lng, lnb, xoT, passes=None, ctxn=128):
    if passes is None:
        passes = [(0, 512), (512, 1024), (1024, 1536), (1536, 2176)]
    MAXT = max(b - a for a, b in passes)
    with P.scope() as st:
        mods = P.sb(st, "mods", [128, 96, 2], F32)
        P.dma("sp", mods[:], mods_d[:], reads=[mods_d], writes=[mods])
        g2a = P.sb(st, "g2a", [128, 16, 2], F32)
        P.op("dve", lambda e: e.tensor_scalar(out=g2a[:], in0=mods[:, 80:96, :], scalar1=1.0 / ALPHA, scalar2=None,
                                              op0=ALU.mult), reads=[mods], writes=[g2a])
        g_ = P.sb(st, "lng", [128, 16], F32)
        b_ = P.sb(st, "lnb", [128, 16], F32)
        P.dma("sp", g_[:], lng[:], reads=[lng], writes=[g_])
        P.dma("sp", b_[:], lnb[:], reads=[lnb], writes=[b_])
        id32 = P.sb(st, "id32", [32, 32], F32)
        P.op("pool", lambda e: e.memset(id32[:], 1.0), writes=[id32])
        P.op("pool", lambda e: e.affine_select(out=id32[:], in_=id32[:], pattern=[[-1, 32]], compare_op=ALU.is_equal,
                                               fill=0.0, base=0, channel_multiplier=1), reads=[id32], writes=[id32])
        W1 = [P.sb(st, "W1_%d" % i, [128, 16, 512], BF16) for i in range(2)]
        W3 = [P.sb(st, "W3_%d" % i, [128, 16, 512], BF16) for i in range(2)]
        W2 = [P.sb(st, "W2_%d" % i, [128, 4, D], BF16) for i in range(2)]
        acc = P.sb(st, "acc", [128, 16, MAXT], F32)
        h2b = [P.sb(st, "h2b%d" % i, [128, 16, MAXT], BF16) for i in range(1)]
        wgt = P.sb(st, "wgt", [32, MAXT], F32)
        wm = [P.sb(st, "wm%d" % i, [32, 512], F32) for i in range(2)]
        Wb = [P.sb(st, "Wb%d" % i, [128, 512], F32) for i in range(2)]
        sl = [P.sb(st, "sl%d" % i, [128, 512], F32) for i in range(2)]
        G = [P.sb(st, "G%d" % i, [128, 4, 512], BF16) for i in range(2)]
        x1c = [P.sb(st, "x1c%d" % i, [128, MAXT], F32) for i in range(2)]
        sq = [P.sb(st, "sq%d" % i, [128, 512], F32) for i in range(2)]
        tmp = [P.sb(st, "tmp%d" % i, [128, 512], F32) for i in range(2)]
        mean = P.sb(st, "mean", [128, 512], F32)
        rstd = P.sb(st, "rstd", [128, 512], F32)
        xo = [P.sb(st, "xo%d" % i, [128, 512], F32) for i in range(2)]
        pH1 = [P.ps(st, "pH1_%d" % i, [128, 512], F32) for i in range(2)]
        pH3 = [P.ps(st, "pH3_%d" % i, [128, 512], F32) for i in range(2)]
        pY = [P.ps(st, "pY%d" % i, [128, 512], F32) for i in range(3)]
        pW = P.ps(st, "pW", [128, 512], F32)
        h2v = h2T.t.rearrange("(kc p) t -> p kc t", p=128)
        w1v = w1.t.rearrange("e (kc p) n -> e p kc n", p=128)
        w3v = w3.t.rearrange("e (kc p) n -> e p kc n", p=128)
        w2v = w2.t.rearrange("e (kc p) n -> e p kc n", p=128)
        seq = [(pi, ex) for pi in range(len(passes)) for ex in range(32)]

        def load_w(k):
            ex = seq[k][1]
            P.dma("pool", W1[k % 2][:], w1v[ex], reads=[w1], writes=[W1[k % 2]])
            P.dma("pool", W3[k % 2][:], w3v[ex], reads=[w3], writes=[W3[k % 2]])
            P.dma("pool", W2[k % 2][:], w2v[ex], reads=[w2], writes=[W2[k % 2]])

        load_w(0)
        nG = 0
        nY = 0
        nH = 0
        for k, (pi, ex) in enumerate(seq):
            t0, t1 = passes[pi]
            ntp = t1 - t0
            subs = blocks(ntp, 512)
            h2b_ = h2b[0]
            if ex == 0:
                P.dma("sp", h2b_[:, :, 0:ntp], h2v[:, :, t0:t1], reads=[h2T], writes=[h2b_])
                P.dma("sp", wgt[:, 0:ntp], wgT[:, t0:t1], reads=[wgT], writes=[wgt])
            if k + 1 < len(seq):
                load_w(k + 1)
            W1_, W3_, W2_ = W1[k % 2], W3[k % 2], W2[k % 2]
            for (s0, s1) in subs:
                ns = s1 - s0
                G_ = G[nG % 2]
                Wb_ = Wb[nG % 2]
                wm_ = wm[nG % 2]
                nG += 1
                P.op("dve", lambda e: e.tensor_scalar(out=wm_[:, 0:ns], in0=wgt[:, s0:s1], scalar1=id32[:, ex:ex + 1],
                                                      scalar2=None, op0=ALU.mult), reads=[wgt, id32], writes=[wm_])
                P.op("pe", lambda e: e.matmul(out=pW[:, 0:ns], lhsT=C["ones_f"][0:32, :], rhs=wm_[:, 0:ns], start=True,
                                              stop=True), reads=[C["ones_f"], wm_], writes=[pW])
                P.op("act", lambda e: e.activation(out=Wb_[:, 0:ns], in_=pW[:, 0:ns], func=AF.Copy), reads=[pW],
                     writes=[Wb_])
                for ffc in range(4):
                    p1, p3 = pH1[nH % 2], pH3[nH % 2]
                    sl_ = sl[nH % 2]
                    nH += 1
                    for kc in range(16):
                        P.op("pe", lambda e: e.matmul(out=p1[:, 0:ns], lhsT=W1_[:, kc, ffc * 128:(ffc + 1) * 128],
                                                      rhs=h2b_[:, kc, s0:s1], start=(kc == 0), stop=(kc == 15)),
                             reads=[W1_, h2b_], writes=[p1])
                    for kc in range(16):
                        P.op("pe", lambda e: e.matmul(out=p3[:, 0:ns], lhsT=W3_[:, kc, ffc * 128:(ffc + 1) * 128],
                                                      rhs=h2b_[:, kc, s0:s1], start=(kc == 0), stop=(kc == 15)),
                             reads=[W3_, h2b_], writes=[p3])
                    P.op("act", lambda e: e.activation(out=sl_[:, 0:ns], in_=p1[:, 0:ns], func=AF.Silu), reads=[p1],
                         writes=[sl_])
                    P.op("dve", lambda e: e.tensor_tensor(out=sl_[:, 0:ns], in0=sl_[:, 0:ns], in1=Wb_[:, 0:ns], op=ALU.mult),
                         reads=[sl_, Wb_], writes=[sl_])
                    P.op("dve", lambda e: e.tensor_tensor(out=G_[:, ffc, 0:ns], in0=p3[:, 0:ns], in1=sl_[:, 0:ns], op=ALU.mult),
                         reads=[p3, sl_], writes=[G_])
                for fc in range(16):
                    pY_ = pY[nY % 3]
                    nY += 1
                    for ffc in range(4):
                        P.op("pe", lambda e: e.matmul(out=pY_[:, 0:ns], lhsT=W2_[:, ffc, fc * 128:(fc + 1) * 128],
                                                      rhs=G_[:, ffc, 0:ns], start=(ffc == 0), stop=(ffc == 3)),
                             reads=[W2_, G_], writes=[pY_])
                    if ex == 0:
                        P.op("act", lambda e: e.activation(out=acc[:, fc, s0:s1], in_=pY_[:, 0:ns], func=AF.Copy),
                             reads=[pY_], writes=[acc])
                    else:
                        P.op("dve", lambda e: e.tensor_tensor(out=acc[:, fc, s0:s1], in0=acc[:, fc, s0:s1], in1=pY_[:, 0:ns],
                                                              op=ALU.add), reads=[acc, pY_], writes=[acc])
            if ex != 31:
                continue
            sg = [(a - t0, b - t0, j) for (a, b, j) in segs_own(t0, t1, ctxn)]
            for fc in range(16):
                x1_ = x1c[fc % 2]
                P.dma("sp", x1_[:, 0:ntp], x1T[fc * 128:(fc + 1) * 128, t0:t1], reads=[x1T], writes=[x1_])
                for (a, b, j) in sg:
                    P.op("dve", lambda e: e.scalar_tensor_tensor(out=acc[:, fc, a:b], in0=acc[:, fc, a:b],
                                                                 scalar=g2a[:, fc, j:j + 1], in1=x1_[:, a:b], op0=ALU.mult,
                                                                 op1=ALU.add), reads=[acc, g2a, x1_], writes=[acc])
            for (s0, s1) in subs:
                ns = s1 - s0

                def outf(fc, t_):
                    xo_ = xo[fc % 2]
                    P.op("act", lambda e: e.activation(out=xo_[:, 0:ns], in_=t_[:, 0:ns], func=AF.Identity,
                                                       scale=g_[:, fc:fc + 1], bias=b_[:, fc:fc + 1]), reads=[t_, g_, b_],
                         writes=[xo_])
                    P.dma("sp", xoT[fc * 128:(fc + 1) * 128, t0 + s0:t0 + s1], xo_[:, 0:ns], reads=[xo_], writes=[xoT])

                ln_block(P, C, (sq, mean, rstd, tmp), acc, s0, ns, outf, (pH1[0], pH3[0]))


def phase_c2_sparse(P, C, x1T, h2tok, rt, mods_d, w1, w3, w2, lng, lnb, xoT, ysl, slots, N, ctxn, SB=256, layer=0):
    NT = N // 128
    NBLK = (2 * N) // SB + 32
    SUBS = SB // 128
    w1r, w3r, w2r = w1.t, w3.t, w2.t
    off1 = float(layer * 32 * 128)
    with P.scope() as st0:
        R = P.sb(st0, "R", [128, NT, 68], F32)
        P.dma("sp", R[:], rt.t.rearrange("(T p) c -> p T c", p=128), reads=[rt], writes=[R])
        DI = [P.sb(st0, "DI%d" % k, [128, NT], I32) for k in range(2)]
        g_ = P.sb(st0, "lng", [128, 16], F32)
        b_ = P.sb(st0, "lnb", [128, 16], F32)
        P.dma("sp", g_[:], lng[:], reads=[lng], writes=[g_])
        P.dma("sp", b_[:], lnb[:], reads=[lnb], writes=[b_])
        mods = P.sb(st0, "mods", [128, 96, 2], F32)
        P.dma("sp", mods[:], mods_d[:], reads=[mods_d], writes=[mods])
        g2a = P.sb(st0, "g2a", [128, 16, 2], F32)
        P.op("dve", lambda e: e.tensor_scalar(out=g2a[:], in0=mods[:, 80:96, :], scalar1=1.0 / ALPHA, scalar2=None,
                                              op0=ALU.mult), reads=[mods], writes=[g2a])
        identf = identity(P, st0, C, F32, "identf2")
        identb = identity(P, st0, C, BF16, "identb2")
        with P.scope() as st:
            Mx = P.sb(st, "Mx", [128, NT, 32], F32)
            P.op("dve", lambda e: e.tensor_tensor(out=Mx[:], in0=R[:, :, 0:32], in1=R[:, :, 32:64], op=ALU.add), reads=[R],
                 writes=[Mx])
            U = tri_mask(P, st, "U", F32, 1.0, 1, strict=True)
            RK = P.sb(st, "RK", [128, NT, 32], F32)
            carry = P.sb(st, "carry", [128, 32], F32)
            P.op("dve", lambda e: e.memset(carry[:], 0.0), writes=[carry])
            pr = P.ps(st, "pr", [128, 512], F32)
            pc = P.ps(st, "pc", [128, 512], F32)
            for T in range(NT):
                P.op("pe", lambda e: e.matmul(out=pr[:, 0:32], lhsT=U[:], rhs=Mx[:, T, :], start=True, stop=True),
                     reads=[U, Mx], writes=[pr])
                P.op("dve", lambda e: e.tensor_tensor(out=RK[:, T, :], in0=pr[:, 0:32], in1=carry[:], op=ALU.add),
                     reads=[pr, carry], writes=[RK])
                P.op("pe", lambda e: e.matmul(out=pc[:, 0:32], lhsT=C["ones_f"][:], rhs=Mx[:, T, :], start=True, stop=True),
                     reads=[C["ones_f"], Mx], writes=[pc])
                P.op("dve", lambda e: e.tensor_tensor(out=carry[:], in0=carry[:], in1=pc[:, 0:32], op=ALU.add),
                     reads=[pc, carry], writes=[carry])
            nb = P.sb(st, "nb", [128, 32], F32)
            P.op("dve", lambda e: e.memset(nb[:], 0.0), writes=[nb])
            for jj in range(N // SB + 1):
                P.op("dve", lambda e: e.scalar_tensor_tensor(out=nb[:], in0=carry[:], scalar=float(SB * jj), in1=nb[:],
                                                             op0=ALU.is_gt, op1=ALU.add), reads=[carry, nb], writes=[nb])
            cs = [P.sb(st, "cs%d" % i, [128, 32], F32) for i in range(2)]
            P.op("dve", lambda e: e.tensor_copy(out=cs[0][:], in_=nb[:]), reads=[nb], writes=[cs[0]])
            cur = 0
            for sft in (1, 2, 4, 8, 16):
                a_, b2 = cs[cur], cs[1 - cur]
                P.op("dve", lambda e: e.tensor_copy(out=b2[:, 0:sft], in_=a_[:, 0:sft]), reads=[a_], writes=[b2])
                P.op("dve", lambda e: e.tensor_tensor(out=b2[:, sft:32], in0=a_[:, sft:32], in1=a_[:, 0:32 - sft], op=ALU.add),
                     reads=[a_], writes=[b2])
                cur = 1 - cur
            pend = P.sb(st, "pend", [128, 32], F32)
            pstart = P.sb(st, "pstart", [128, 32], F32)
            P.op("dve", lambda e: e.tensor_scalar(out=pend[:], in0=cs[cur][:], scalar1=float(SB), scalar2=None, op0=ALU.mult),
                 reads=[cs[cur]], writes=[pend])
            P.op("dve", lambda e: e.scalar_tensor_tensor(out=pstart[:], in0=nb[:], scalar=-float(SB), in1=pend[:],
                                                         op0=ALU.mult, op1=ALU.add), reads=[nb, pend], writes=[pstart])
            Dk = [P.sb(st, "Dk%d" % k, [128, NT], F32) for k in range(2)]
            t32 = [P.sb(st, "t32_%d" % i, [128, 32], F32) for i in range(2)]
            for T in range(NT):
                P.op("dve", lambda e: e.tensor_tensor(out=t32[0][:], in0=RK[:, T, :], in1=pstart[:], op=ALU.add),
                     reads=[RK, pstart], writes=[t32[0]])
                for k in range(2):
                    P.op("dve", lambda e: e.tensor_tensor(out=t32[1][:], in0=t32[0][:], in1=R[:, T, 32 * k:32 * k + 32],
                                                          op=ALU.mult), reads=[t32[0], R], writes=[t32[1]])
                    P.op("dve", lambda e: e.tensor_reduce(out=Dk[k][:, T:T + 1], in_=t32[1][:], axis=AX.X, op=ALU.add),
                         reads=[t32[1]], writes=[Dk[k]])
            for k in range(2):
                P.op("dve", lambda e: e.tensor_copy(out=DI[k][:], in_=Dk[k][:]), reads=[Dk[k]], writes=[DI[k]])
            BE = P.sb(st, "BE", [128, NBLK], F32)
            for b in range(NBLK):
                P.op("dve", lambda e: e.tensor_scalar(out=t32[0][:], in0=pend[:], scalar1=float(SB * b), scalar2=None,
                                                      op0=ALU.is_le), reads=[pend], writes=[t32[0]])
                P.op("dve", lambda e: e.tensor_reduce(out=BE[:, b:b + 1], in_=t32[0][:], axis=AX.X, op=ALU.add),
                     reads=[t32[0]], writes=[BE])
            P.op("dve", lambda e: e.tensor_scalar(out=BE[:], in0=BE[:], scalar1=31.0, scalar2=None, op0=ALU.min), reads=[BE],
                 writes=[BE])
            tki = P.sb(st, "tki", [128, NT], I32)
            P.op("pool", lambda e: e.iota(tki[:], pattern=[[128, NT]], base=0, channel_multiplier=1), writes=[tki])
            tkf = P.sb(st, "tkf", [128, NT], F32)
            P.op("dve", lambda e: e.tensor_copy(out=tkf[:], in_=tki[:]), reads=[tki], writes=[tkf])
            TW = [P.sb(st, "TW%d" % k, [128, NT, 2], F32) for k in range(2)]
            for k in range(2):
                P.op("dve", lambda e: e.tensor_copy(out=TW[k][:, :, 0], in_=tkf[:]), reads=[tkf], writes=[TW[k]])
                P.op("dve", lambda e: e.tensor_copy(out=TW[k][:, :, 1], in_=R[:, :, 64 + k]), reads=[R], writes=[TW[k]])
            NSUB = NBLK * SUBS
            zt = P.sb(st, "zt", [128, NSUB, 2], F32)
            P.op("dve", lambda e: e.memset(zt[:], 0.0), writes=[zt])
            sl_v = slots.t.rearrange("(a p) c -> p a c", p=128)
            P.dma("sp", sl_v, zt[:], reads=[zt], writes=[slots])
            for T in range(NT):
                for k in range(2):
                    P.iscatter(slots[:], TW[k][:, T, :], DI[k][:, T:T + 1].bitcast(U32), reads=[TW[k], DI[k]], writes=[slots])
            STA = P.sb(st, "STA", [128, NSUB, 2], F32)
            P.dma("sp", STA[:], sl_v, reads=[slots], writes=[STA])
            STI = P.sb(st, "STI", [128, NSUB], I32)
            P.op("dve", lambda e: e.tensor_copy(out=STI[:], in_=STA[:, :, 0]), reads=[STA], writes=[STI])
            W1 = [P.sb(st, "W1_%d" % i, [128, 16, 512], BF16) for i in range(2)]
            W3 = [P.sb(st, "W3_%d" % i, [128, 16, 512], BF16) for i in range(2)]
            W2 = [P.sb(st, "W2_%d" % i, [128, 4, D], BF16) for i in range(2)]
            ix1 = [P.sb(st, "ix1_%d" % i, [128, 16], I32) for i in range(2)]
            ix2 = [P.sb(st, "ix2_%d" % i, [128, 4], I32) for i in range(2)]
            X = [P.sb(st, "X%d" % i, [128, D], BF16) for i in range(2)]
            XT = [P.sb(st, "XT%d" % i, [128, 16, 128], BF16) for i in range(2)]
            sl = [P.sb(st, "sl%d" % i, [128, 512], F32) for i in range(2)]
            G = [P.sb(st, "G%d" % i, [128, 512], BF16) for i in range(2)]
            GT = [P.sb(st, "GT%d" % i, [128, 4, 128], BF16) for i in range(2)]
            Ys = [P.sb(st, "Ys%d" % i, [128, D], F32) for i in range(2)]
            ptx = P.ps(st, "ptx", [128, 1024], BF16)
            pH1 = P.ps(st, "pH1", [128, 512], F32)
            pH3 = P.ps(st, "pH3", [128, 512], F32)
            pYs = [P.ps(st, "pYs%d" % i, [128, 512], F32) for i in range(2)]

            eo = [P.sb(st, "eo%d" % i, [128, 2], F32) for i in range(2)]

            def load_wb(b):
                i1, eo_ = ix1[b % 2], eo[b % 2]
                P.op("dve", lambda e: e.tensor_scalar(out=eo_[:, 0:1], in0=BE[:, b:b + 1], scalar1=128.0, scalar2=off1,
                                                      op0=ALU.mult, op1=ALU.add), reads=[BE], writes=[eo_])
                P.op("dve", lambda e: e.tensor_scalar(out=i1[:, 0:1], in0=tkf[:, 0:1], scalar1=eo_[:, 0:1], scalar2=None,
                                                      op0=ALU.add), reads=[tkf, eo_], writes=[i1])
                P.idma(W1[b % 2][:].rearrange("p a b -> p (a b)"), w1r[:], i1[:, 0:1].bitcast(U32), reads=[i1, w1],
                       writes=[W1[b % 2]])
                P.idma(W3[b % 2][:].rearrange("p a b -> p (a b)"), w3r[:], i1[:, 0:1].bitcast(U32), reads=[i1, w3],
                       writes=[W3[b % 2]])
                P.idma(W2[b % 2][:].rearrange("p a b -> p (a b)"), w2r[:], i1[:, 0:1].bitcast(U32), reads=[i1, w2],
                       writes=[W2[b % 2]])

            load_wb(0)
            nsb = 0
            for b in range(NBLK):
                if b + 1 < NBLK:
                    load_wb(b + 1)
                W1_, W3_, W2_ = W1[b % 2], W3[b % 2], W2[b % 2]
                for sub in range(SUBS):
                    i2_ = nsb % 2
                    nsb += 1
                    base = b * SB + sub * 128
                    sbi = b * SUBS + sub
                    X_, XT_ = X[i2_], XT[i2_]
                    P.idma(X_[:], h2tok[:], STI[:, sbi:sbi + 1].bitcast(U32), reads=[STI, h2tok], writes=[X_])
                    for hh in range(2):
                        for q in range(8):
                            fc = hh * 8 + q
                            P.op("pe", lambda e: e.transpose(out=ptx[:, q * 128:(q + 1) * 128], in_=X_[:, fc * 128:(fc + 1) * 128],
                                                             identity=identb[:]), reads=[X_, identb], writes=[ptx])
                        P.op("act", lambda e: e.activation(out=XT_[:, hh * 8:(hh + 1) * 8, :],
                                                           in_=ptx[:, :].rearrange("p (a b) -> p a b", b=128), func=AF.Copy),
                             reads=[ptx], writes=[XT_])
                    for kc in range(16):
                        P.op("pe", lambda e: e.matmul(out=pH1[:, :], lhsT=XT_[:, kc, :], rhs=W1_[:, kc, :], start=(kc == 0),
                                                      stop=(kc == 15)), reads=[XT_, W1_], writes=[pH1])
                    for kc in range(16):
                        P.op("pe", lambda e: e.matmul(out=pH3[:, :], lhsT=XT_[:, kc, :], rhs=W3_[:, kc, :], start=(kc == 0),
                                                      stop=(kc == 15)), reads=[XT_, W3_], writes=[pH3])
                    sl_, G_, GT_ = sl[i2_], G[i2_], GT[i2_]
                    P.op("act", lambda e: e.activation(out=sl_[:], in_=pH1[:, :], func=AF.Silu), reads=[pH1], writes=[sl_])
                    P.op("dve", lambda e: e.tensor_tensor(out=G_[:], in0=pH3[:, :], in1=sl_[:], op=ALU.mult), reads=[pH3, sl_],
                         writes=[G_])
                    for q in range(4):
                        P.op("pe", lambda e: e.transpose(out=ptx[:, q * 128:(q + 1) * 128], in_=G_[:, q * 128:(q + 1) * 128],
                                                         identity=identb[:]), reads=[G_, identb], writes=[ptx])
                    P.op("act", lambda e: e.activation(out=GT_[:], in_=ptx[:, 0:512].rearrange("p (a b) -> p a b", b=128),
                                                       func=AF.Copy), reads=[ptx], writes=[GT_])
                    Ys_ = Ys[i2_]
                    for nb4 in range(4):
                        pY_ = pYs[nb4 % 2]
                        for q in range(4):
                            P.op("pe", lambda e: e.matmul(out=pY_[:, :], lhsT=GT_[:, q, :], rhs=W2_[:, q, nb4 * 512:(nb4 + 1) * 512],
                                                          start=(q == 0), stop=(q == 3)), reads=[GT_, W2_], writes=[pY_])
                        P.op("act" if nb4 % 2 else "dve",
                             (lambda e: e.activation(out=Ys_[:, nb4 * 512:(nb4 + 1) * 512], in_=pY_[:, :], func=AF.Copy,
                                                     scale=STA[:, sbi, 1:2])) if nb4 % 2 else
                             (lambda e: e.tensor_scalar(out=Ys_[:, nb4 * 512:(nb4 + 1) * 512], in0=pY_[:, :], scalar1=STA[:, sbi, 1:2],
                                                        scalar2=None, op0=ALU.mult)),
                             reads=[pY_, STA], writes=[Ys_])
                    P.dma("sp", ysl[base:base + 128, :], Ys_[:], reads=[Ys_], writes=[ysl])
        with P.scope() as st:
            ya = [P.sb(st, "ya%d" % i, [128, D], F32) for i in range(2)]
            yb = [P.sb(st, "yb%d" % i, [128, D], F32) for i in range(2)]
            acc = P.sb(st, "acc", [128, 16, 512], F32)
            x1c = [P.sb(st, "x1c%d" % i, [128, 512], F32) for i in range(2)]
            sq = [P.sb(st, "sq%d" % i, [128, 512], F32) for i in range(2)]
            tmp = [P.sb(st, "tmp%d" % i, [128, 512], F32) for i in range(2)]
            mean = P.sb(st, "mean", [128, 512], F32)
            rstd = P.sb(st, "rstd", [128, 512], F32)
            xo = [P.sb(st, "xo%d" % i, [128, 512], F32) for i in range(2)]
            ptf = [P.ps(st, "ptf%d" % i, [128, 512], F32) for i in range(2)]
            ps_s = P.ps(st, "ps_s", [128, 512], F32)
            ps_q = P.ps(st, "ps_q", [128, 512], F32)
            ng = 0
            for (t0, t1) in blocks(N, 512):
                ntp = t1 - t0
                for jt in range(ntp // 128):
                    T = t0 // 128 + jt
                    ya_, yb_ = ya[ng % 2], yb[ng % 2]
                    ng += 1
                    P.idma(ya_[:], ysl[:], DI[0][:, T:T + 1].bitcast(U32), reads=[DI[0], ysl], writes=[ya_])
                    P.idma(yb_[:], ysl[:], DI[1][:, T:T + 1].bitcast(U32), reads=[DI[1], ysl], writes=[yb_])
                    P.op("dve", lambda e: e.tensor_tensor(out=ya_[:], in0=ya_[:], in1=yb_[:], op=ALU.add), reads=[ya_, yb_],
                         writes=[ya_])
                    for g4 in range(4):
                        pt_ = ptf[g4 % 2]
                        for q in range(4):
                            fc = g4 * 4 + q
                            P.op("pe", lambda e: e.transpose(out=pt_[:, q * 128:(q + 1) * 128], in_=ya_[:, fc * 128:(fc + 1) * 128],
                                                             identity=identf[:]), reads=[ya_, identf], writes=[pt_])
                        P.op("act", lambda e: e.activation(out=acc[:, g4 * 4:(g4 + 1) * 4, jt * 128:(jt + 1) * 128],
                                                           in_=pt_[:, :].rearrange("p (a b) -> p a b", b=128), func=AF.Copy),
                             reads=[pt_], writes=[acc])
                sg = [(a - t0, b - t0, j) for (a, b, j) in segs_own(t0, t1, ctxn)]
                for fc in range(16):
                    x1_ = x1c[fc % 2]
                    P.dma("sp", x1_[:, 0:ntp], x1T[fc * 128:(fc + 1) * 128, t0:t1], reads=[x1T], writes=[x1_])
                    for (a, b, j) in sg:
                        P.op("dve", lambda e: e.scalar_tensor_tensor(out=acc[:, fc, a:b], in0=acc[:, fc, a:b],
                                                                     scalar=g2a[:, fc, j:j + 1], in1=x1_[:, a:b], op0=ALU.mult,
                                                                     op1=ALU.add), reads=[acc, g2a, x1_], writes=[acc])

                def outf(fc, t_):
                    xo_ = xo[fc % 2]
                    P.op("act", lambda e: e.activation(out=xo_[:, 0:ntp], in_=t_[:, 0:ntp], func=AF.Identity,
                                                       scale=g_[:, fc:fc + 1], bias=b_[:, fc:fc + 1]), reads=[t_, g_, b_],
                         writes=[xo_])
                    P.dma("sp", xoT[fc * 128:(fc + 1) * 128, t0:t1], xo_[:, 0:ntp], reads=[xo_], writes=[xoT])

                ln_block(P, C, (sq, mean, rstd, tmp), acc, 0, ntp, outf, (ps_s, ps_q))


_CACHE = {}


def _run(key, build_fn, in_maps):
    if key not in _CACHE:
        _CACHE[key] = build_fn()
    nc = _CACHE[key]
    res = run_bass_kernel_spmd(nc, in_maps, core_ids=list(range(8)))
    return res.results


def _prog():
    nc = bass.Bass("TRN2", target_bir_lowering=False)
    return nc, Prog(nc)


def build_a():
    nc, P = _prog()
    cc = P.dram("cc", [128, 16, 2], F32, kind="ExternalInput")
    wmod = P.dram("wmod", [D, 6 * D], F32, kind="ExternalInput")
    bmod = P.dram("bmod", [128, 96], F32, kind="ExternalInput")
    xT = P.dram("xT", [D, NOWN], F32, kind="ExternalInput")
    mods_o = P.dram("mods_o", [128, 96, 2], F32, kind="ExternalOutput")
    hT = P.dram("hT", [D, NOWN], BF16, kind="ExternalOutput")
    with ExitStack() as st:
        C = consts(P, st)
        phase_a(P, C, cc, wmod, bmod, xT, mods_o, hT)
        P.finish([mods_o, hT])
    P.close()
    return nc


def build_mla(debug=False):
    nc, P = _prog()
    dbg = None
    if debug:
        dbg = dict(cqn=P.dram("d_cqn", [128, 6, NB], BF16, kind="ExternalOutput"),
                   ckvn=P.dram("d_ckvn", [128, 2, NB], BF16, kind="ExternalOutput"),
                   kr=P.dram("d_kr", [64, NB], BF16, kind="ExternalOutput"))
    hT = P.dram("hT", [D, NB], BF16, kind="ExternalInput")
    w_in = P.dram("w_in", [D, 1088], F32, kind="ExternalInput")
    qng = P.dram("qng", [128, 6], F32, kind="ExternalInput")
    w_uq = P.dram("w_uq", [768, 8 * 192], F32, kind="ExternalInput")
    kvng = P.dram("kvng", [128, 2], F32, kind="ExternalInput")
    w_ukv = P.dram("w_ukv", [256, 8 * 256], F32, kind="ExternalInput")
    cos2 = P.dram("cos2", [64, NB], F32, kind="ExternalInput")
    sin2 = P.dram("sin2", [64, NB], F32, kind="ExternalInput")
    oT = P.dram("oT", [1024, NB], BF16, kind="ExternalOutput")
    with ExitStack() as st:
        C = consts(P, st)
        phase_mla(P, C, hT, w_in, qng, w_uq, kvng, w_ukv, cos2, sin2, oT, dbg)
        P.finish([oT] + (list(dbg.values()) if dbg else []))
    P.close()
    return nc


def build_gla():
    nc, P = _prog()
    hT = P.dram("hT", [D, NB], BF16, kind="ExternalInput")
    w_in = P.dram("w_in", [D, 2 * 1536], F32, kind="ExternalInput")
    ga = P.dram("ga", [D, 32], F32, kind="ExternalInput")
    gb = P.dram("gb", [2, 16, 512], F32, kind="ExternalInput")
    gbias = P.dram("gbias", [2, 1, 512], F32, kind="ExternalInput")
    ng = P.dram("ng", [1, 512], F32, kind="ExternalInput")
    oT = P.dram("oT", [1024, NB], BF16, kind="ExternalOutput")
    oscr = P.dram("oscr", [NB, 512], F32, kind="Internal")
    with ExitStack() as st:
        C = consts(P, st)
        phase_gla(P, C, hT, w_in, ga, gb, gbias, ng, oT, oscr)
        P.finish([oT])
    P.close()
    return nc


def build_c():
    nc, P = _prog()
    xT = P.dram("xT", [D, NOWN], F32, kind="ExternalInput")
    mods_d = P.dram("mods", [128, 96, 2], F32, kind="ExternalInput")
    oTf = P.dram("oTf", [D, NOWN], BF16, kind="ExternalInput")
    w_o = P.dram("w_o", [D, D], F32, kind="ExternalInput")
    l1g = P.dram("l1g", [128, 16], F32, kind="ExternalInput")
    l1b = P.dram("l1b", [128, 16], F32, kind="ExternalInput")
    l2g = P.dram("l2g", [128, 16], F32, kind="ExternalInput")
    l2b = P.dram("l2b", [128, 16], F32, kind="ExternalInput")
    wr = P.dram("wr", [D, 36], F32, kind="ExternalInput")
    br = P.dram("br", [1, 36], F32, kind="ExternalInput")
    w1 = P.dram("w1", [32 * 128, 8192], F32, kind="ExternalInput")
    w3 = P.dram("w3", [32 * 128, 8192], F32, kind="ExternalInput")
    w2 = P.dram("w2", [32 * 128, 8192], F32, kind="ExternalInput")
    x1T = P.dram("x1T", [D, NOWN], F32, kind="Internal")
    h2T = P.dram("h2T", [D, NOWN], BF16, kind="Internal")
    wgT = P.dram("wgT", [32, NOWN], F32, kind="Internal")
    xoT = P.dram("xoT", [D, NOWN], F32, kind="ExternalOutput")
    h2tok = P.dram("h2tok", [NOWN, D], BF16)
    rt = P.dram("rt", [NOWN, 68], F32)
    SB = 256
    ysl = P.dram("ysl", [((2 * NOWN) // SB + 32) * SB, D], F32)
    with ExitStack() as st:
        C = consts(P, st)
        phase_c1(P, C, xT, mods_d, oTf, w_o, l1g, l1b, wr, br, x1T, h2T, wgT, h2tok=h2tok, rt=rt)
        slots = P.dram("slots", [((2 * NOWN) // SB + 32) * SB, 2], F32)
        phase_c2_sparse(P, C, x1T, h2tok, rt, mods_d, w1, w3, w2, l2g, l2b, xoT, ysl, slots, NOWN, 128, SB=SB)
        P.finish([xoT])
    P.close()
    return nc


def _lv(b, i, name):
    v = Buf("%s_%d" % (name, i), b.t[i])
    return v


def build_fused():
    nc, P = _prog()
    N, CT = NB, 256
    I = "ExternalInput"
    xT0 = P.dram("xT0", [D, N], F32, kind=I)
    cc = P.dram("cc", [128, 16, 2], F32, kind=I)
    w_mod = P.dram("w_mod", [DEPTH, D, 6 * D], F32, kind=I)
    b_mod = P.dram("b_mod", [DEPTH, 128, 96], F32, kind=I)
    l1g = P.dram("l1g", [DEPTH, 128, 16], F32, kind=I)
    l1b = P.dram("l1b", [DEPTH, 128, 16], F32, kind=I)
    l2g = P.dram("l2g", [DEPTH, 128, 16], F32, kind=I)
    l2b = P.dram("l2b", [DEPTH, 128, 16], F32, kind=I)
    m_w_in = P.dram("m_w_in", [2, D, 1088], F32, kind=I)
    m_qn = P.dram("m_qn", [2, 128, 6], F32, kind=I)
    m_w_uq = P.dram("m_w_uq", [2, 768, 3072], F32, kind=I)
    m_kvn = P.dram("m_kvn", [2, 128, 2], F32, kind=I)
    m_w_ukv = P.dram("m_w_ukv", [2, 256, 4096], F32, kind=I)
    m_w_o = P.dram("m_w_o", [2, D, D], F32, kind=I)
    g_w_in = P.dram("g_w_in", [2, D, 6144], F32, kind=I)
    g_ga = P.dram("g_ga", [2, D, 32], F32, kind=I)
    g_gb = P.dram("g_gb", [2, 2, 16, 1024], F32, kind=I)
    g_gbias = P.dram("g_gbias", [2, 2, 1, 1024], F32, kind=I)
    g_ng = P.dram("g_ng", [2, 1, 512], F32, kind=I)
    g_w_o = P.dram("g_w_o", [2, D, D], F32, kind=I)
    wr = P.dram("wr", [DEPTH, D, 36], F32, kind=I)
    br = P.dram("br", [DEPTH, 1, 36], F32, kind=I)
    w1 = P.dram("w1", [DEPTH * 32 * 128, 8192], F32, kind=I)
    w3 = P.dram("w3", [DEPTH * 32 * 128, 8192], F32, kind=I)
    w2 = P.dram("w2", [DEPTH * 32 * 128, 8192], F32, kind=I)
    cos2 = P.dram("cos2", [64, N], F32, kind=I)
    sin2 = P.dram("sin2", [64, N], F32, kind=I)
    xs = [P.dram("xA", [D, N], F32), P.dram("xB", [D, N], F32)]
    hT = P.dram("hT", [D, N], BF16)
    oT = P.dram("oT", [D, N], BF16)
    mods = P.dram("mods", [128, 96, 2], F32)
    x1T = P.dram("x1T", [D, N], F32)
    h2T = P.dram("h2T", [D, N], BF16)
    wgT = P.dram("wgT", [32, N], F32)
    oscr = P.dram("oscr", [N, 512], F32)
    xoT = P.dram("xoT", [D, N], F32, kind="ExternalOutput")
    h2tok = P.dram("h2tok", [N, D], BF16)
    rt = P.dram("rt", [N, 68], F32)
    SB = 256
    ysl = P.dram("ysl", [((2 * N) // SB + 32) * SB, D], F32)
    slots = P.dram("slots", [((2 * N) // SB + 32) * SB, 2], F32)
    passes = blocks(N, 512)
    with ExitStack() as st:
        C = consts(P, st)
        xin = xT0
        for i in range(DEPTH):
            j = i // 2
            xout = xoT if i == DEPTH - 1 else xs[i % 2]
            phase_a(P, C, cc, _lv(w_mod, i, "wm"), _lv(b_mod, i, "bm"), xin, mods, hT, N=N, ctxn=CT)
            P.new_epoch()
            if i % 2 == 0:
                phase_mla(P, C, hT, _lv(m_w_in, j, "a"), _lv(m_qn, j, "b"), _lv(m_w_uq, j, "c"), _lv(m_kvn, j, "d"),
                          _lv(m_w_ukv, j, "e"), cos2, sin2, oT, NHT=16)
                w_o = _lv(m_w_o, j, "f")
            else:
                phase_gla(P, C, hT, _lv(g_w_in, j, "g"), _lv(g_ga, j, "h"), _lv(g_gb, j, "i"), _lv(g_gbias, j, "j"),
                          _lv(g_ng, j, "k"), oT, oscr, NHL=4)
                w_o = _lv(g_w_o, j, "l")
            P.new_epoch()
            phase_c1(P, C, xin, mods, oT, w_o, _lv(l1g, i, "m"), _lv(l1b, i, "n"), _lv(wr, i, "o"), _lv(br, i, "p"),
                     x1T, h2T, wgT, N=N, ctxn=CT, h2tok=h2tok, rt=rt)
            phase_c2_sparse(P, C, x1T, h2tok, rt, mods, w1, w3, w2, _lv(l2g, i, "t"),
                            _lv(l2b, i, "u"), xout, ysl, slots, N, CT, SB=SB, layer=i)
            P.new_epoch()
            xin = xout
        P.finish([xoT])
    P.close()
    print("fused program: ninst", P.ninst, "waits", P.nwaits, "nsem", P.nsem, "epochs", P.epoch)
    return nc


def relayout_w(w, kc):
    w = np.asarray(w, np.float32)
    L, E, K, n = w.shape
    return np.ascontiguousarray(w.reshape(L, E, kc, 128, n).transpose(0, 1, 3, 2, 4)).reshape(L * E * 128, kc * n)


def fm(v):
    return np.ascontiguousarray(np.asarray(v, np.float32).reshape(-1, 128).T)


def rope_tables():
    n_rows = 4096 // 64
    row = np.repeat(np.arange(n_rows, dtype=np.float32), 64)
    col = np.tile(np.arange(64, dtype=np.float32), n_rows)
    n_freq = 16
    inv_freq = np.power(np.float32(10000.0), -np.arange(n_freq, dtype=np.float32) / n_freq).astype(np.float32)
    ang = np.concatenate([row[:, None] * inv_freq, col[:, None] * inv_freq], axis=-1)
    cos, sin = np.cos(ang).astype(np.float32), np.sin(ang).astype(np.float32)
    cos2 = np.ones((64, NB), np.float32)
    sin2 = np.zeros((64, NB), np.float32)
    cos2[0:32, 256:] = cos.T
    cos2[32:64, 256:] = cos.T
    sin2[0:32, 256:] = sin.T
    sin2[32:64, 256:] = sin.T
    return cos2, sin2


def kernel(x, c, ctx, c_ctx, w_mod, b_mod, ln1_g, ln1_b, ln2_g, ln2_b,
           mla_w_in, mla_q_norm, mla_w_uq, mla_kv_norm, mla_w_ukv, mla_w_o,
           gla_w_in, gla_gate_a, gla_gate_b, gla_gate_bias, gla_norm, gla_w_o,
           moe_w_grp, moe_b_grp, moe_w_exp, moe_b_exp, moe_w1, moe_w3, moe_w2):
    f32 = np.float32
    A = lambda v: np.ascontiguousarray(np.asarray(v, f32))
    x = A(x); ctx = A(ctx); c = A(c); c_ctx = A(c_ctx)
    cos2, sin2 = rope_tables()
    gwi = np.asarray(gla_w_in, f32)
    cols = []
    for h in range(4):
        cols += [gwi[:, :, h * 256:(h + 1) * 256], gwi[:, :, 1024 + h * 256:1024 + (h + 1) * 256],
                 gwi[:, :, 2048 + h * 512:2048 + (h + 1) * 512], gwi[:, :, 4096 + h * 512:4096 + (h + 1) * 512]]
    shared = dict(
        w_mod=A(w_mod), b_mod=A(np.stack([fm(b_mod[i]) for i in range(DEPTH)])),
        l1g=A(np.stack([fm(ln1_g[i]) for i in range(DEPTH)])), l1b=A(np.stack([fm(ln1_b[i]) for i in range(DEPTH)])),
        l2g=A(np.stack([fm(ln2_g[i]) for i in range(DEPTH)])), l2b=A(np.stack([fm(ln2_b[i]) for i in range(DEPTH)])),
        m_w_in=A(mla_w_in), m_qn=A(np.stack([fm(mla_q_norm[j]) for j in range(2)])), m_w_uq=A(mla_w_uq),
        m_kvn=A(np.stack([fm(mla_kv_norm[j]) for j in range(2)])), m_w_ukv=A(mla_w_ukv), m_w_o=A(mla_w_o),
        g_w_in=A(np.concatenate(cols, axis=2)),
        g_ga=A(np.concatenate([np.asarray(gla_gate_a, f32)[:, 0], np.asarray(gla_gate_a, f32)[:, 1]], axis=2)),
        g_gb=A(gla_gate_b), g_gbias=A(np.asarray(gla_gate_bias, f32)[:, :, None, :]),
        g_ng=A(np.asarray(gla_norm, f32)[:, None, :]), g_w_o=A(gla_w_o),
        wr=A(np.concatenate([np.asarray(moe_w_grp, f32), np.asarray(moe_w_exp, f32)], axis=2)),
        br=A(np.concatenate([np.asarray(moe_b_grp, f32), np.asarray(moe_b_exp, f32)], axis=1)[:, None, :]),
        w1=relayout_w(moe_w1, 16), w3=relayout_w(moe_w3, 16), w2=relayout_w(moe_w2, 4), cos2=cos2, sin2=sin2)
    ins = []
    for core in range(8):
        b = core // 2
        d = dict(shared)
        d["xT0"] = np.ascontiguousarray(np.concatenate([ctx[b], x[b]], axis=0).T)
        d["cc"] = np.ascontiguousarray(np.stack([fm(c[b]), fm(c_ctx)], axis=-1))
        ins.append(d)
    res = _run("fused", build_fused, ins)
    out = np.empty((4, 4096, D), f32)
    for b in range(4):
        out[b] = res[2 * b]["xoT"][:, 256:].T
    return out
```

```python
import numpy as np
import ml_dtypes
from contextlib import ExitStack
import concourse.bass as bass
import concourse.mybir as mybir
from concourse.bass_utils import run_bass_kernel_spmd

F32 = mybir.dt.float32
BF16 = mybir.dt.bfloat16
I32 = mybir.dt.int32
U32 = mybir.dt.uint32
AF = mybir.ActivationFunctionType
ALU = mybir.AluOpType
AX = mybir.AxisListType

import os
SKIP = os.environ.get("SKIP", "")
STOPAT = float(os.environ.get("STOPAT", "99"))
D = 2048
DEPTH = 4
NOWN = 2176
NB = 4352
ALPHA = (2.0 * DEPTH) ** 0.25
EPS = 1e-6


class Buf:
    __slots__ = ("name", "t", "last_w", "reads", "dsem", "dcount", "psum", "nowaw")

    def __init__(self, name, t=None):
        self.psum = False
        self.nowaw = False
        self.name = name
        self.t = t
        self.last_w = None
        self.reads = []
        self.dsem = None
        self.dcount = 0

    def __getitem__(self, idx):
        return self.t[idx]


class Prog:
    def __init__(self, nc):
        self.nc = nc
        self.es = ExitStack()
        self.engs = {"pe": nc.tensor, "act": nc.scalar, "dve": nc.vector, "pool": nc.gpsimd, "sp": nc.sync}
        self.esem = {}
        self.ecount = {k: 0 for k in self.engs}
        self.waited = {k: {} for k in self.engs}
        self.sems = {}
        self.epoch = 0
        for k in self.engs:
            s = self.es.enter_context(nc.semaphore("es_" + k))
            self.esem[k] = s
            self.sems[("e", k, 0)] = s
        self.sem_pool = []
        self.scopes = []
        self.nsem = 0
        self.dbufs = []
        self.nwaits = 0
        self.ninst = 0
        self.uid = 0

    def sb(self, stack, name, shape, dt):
        self.uid += 1
        nm = "%s_%d" % (name, self.uid)
        t = stack.enter_context(self.nc.sbuf_tensor(nm, list(shape), dt))
        b = Buf(nm, t)
        if self.scopes:
            self.scopes[-1].append(b)
        return b

    def ps(self, stack, name, shape, dt):
        self.uid += 1
        nm = "%s_%d" % (name, self.uid)
        t = stack.enter_context(self.nc.psum_tensor(nm, list(shape), dt))
        b = Buf(nm, t)
        b.psum = True
        return b

    def dram(self, name, shape, dt, kind="Internal"):
        t = self.nc.dram_tensor(name, list(shape), dt, kind=kind)
        b = Buf(name, t.ap())
        b.nowaw = True
        return b

    def _dsem(self, buf):
        if buf.dsem is None:
            if self.sem_pool:
                buf.dsem, buf.dcount = self.sem_pool.pop()
            else:
                self.nsem += 1
                s = self.es.enter_context(self.nc.semaphore("ds%d" % self.nsem))
                buf.dsem = ("d", self.nsem)
                self.sems[buf.dsem] = s
            self.dbufs.append(buf)
        return buf.dsem

    def new_epoch(self):
        self.barrier()
        self.epoch += 1
        for k in self.engs:
            s = self.es.enter_context(self.nc.semaphore("es_%s_%d" % (k, self.epoch)))
            self.esem[k] = s
            self.sems[("e", k, self.epoch)] = s
            self.ecount[k] = 0

    def release_scope(self, bufs):
        for b in bufs:
            if b.dsem is not None:
                self.sem_pool.append((b.dsem, b.dcount))
                self.dbufs.remove(b)
                b.dsem = None

    def _wait(self, ek, ev):
        if ev is None:
            return
        key, val = ev
        if key == ("e", ek, self.epoch) and ek == "pe":
            return
        if self.waited[ek].get(key, 0) >= val:
            return
        self.waited[ek][key] = val
        self.engs[ek].wait_ge(self.sems[key], val)
        self.nwaits += 1

    def _deps(self, ek, reads, writes):
        for b in reads:
            self._wait(ek, b.last_w)
        for b in writes:
            if not (b.nowaw and b.last_w is not None and b.last_w[0] == b.dsem):
                self._wait(ek, b.last_w)
            for r in b.reads:
                self._wait(ek, r)

    def _commit(self, ev, reads, writes):
        for b in reads:
            b.reads.append(ev)
            if len(b.reads) > 24:
                d = {}
                for k, v in b.reads:
                    d[k] = max(d.get(k, 0), v)
                b.reads = list(d.items())
        for b in writes:
            b.last_w = ev
            b.reads = []

    def op(self, ek, fn, reads=(), writes=()):
        pr = [b for b in reads if b.psum]
        if pr:
            reads = [b for b in reads if not b.psum]
            writes = list(writes) + pr
        self._deps(ek, reads, writes)
        ins = fn(self.engs[ek])
        self.ecount[ek] += 1
        ins.then_inc(self.esem[ek], 1)
        self._commit((("e", ek, self.epoch), self.ecount[ek]), reads, writes)
        self.ninst += 1
        return ins

    def dma(self, q, out_ap, in_ap, reads=(), writes=(), sem_buf=None, **kw):
        self._deps(q, reads, writes)
        sb_ = sem_buf if sem_buf is not None else writes[0]
        key = self._dsem(sb_)
        ins = self.engs[q].dma_start(out=out_ap, in_=in_ap, **kw)
        sb_.dcount += 1
        ins.then_inc(self.sems[key], 16)
        self._commit((key, 16 * sb_.dcount), reads, writes)
        self.ninst += 1
        return ins

    def scope(self):
        return _Scope(self)

    def barrier(self):
        for ek in self.engs:
            for e2 in self.engs:
                if e2 != ek and self.ecount[e2] > 0:
                    self._wait(ek, (("e", e2, self.epoch), self.ecount[e2]))
            for b in self.dbufs:
                self._wait(ek, (b.dsem, 16 * b.dcount))

    def idma(self, out_ap, in_ap, idx_ap, reads=(), writes=()):
        self._deps("pool", reads, writes)
        sb_ = writes[0]
        key = self._dsem(sb_)
        ins = self.nc.gpsimd.indirect_dma_start(out=out_ap, out_offset=None, in_=in_ap,
                                                in_offset=bass.IndirectOffsetOnAxis(ap=idx_ap, axis=0))
        sb_.dcount += 1
        ins.then_inc(self.sems[key], 16)
        self._commit((key, 16 * sb_.dcount), reads, writes)
        self.ninst += 1
        return ins

    def iscatter(self, out_ap, in_ap, idx_ap, reads=(), writes=()):
        self._deps("pool", reads, writes)
        sb_ = writes[0]
        self._wait("pool", sb_.last_w)
        key = self._dsem(sb_)
        ins = self.nc.gpsimd.indirect_dma_start(out=out_ap, out_offset=bass.IndirectOffsetOnAxis(ap=idx_ap, axis=0),
                                                in_=in_ap, in_offset=None)
        sb_.dcount += 1
        ins.then_inc(self.sems[key], 16)
        self._commit((key, 16 * sb_.dcount), reads, writes)
        self.ninst += 1
        return ins

    def finish(self, bufs, ek="sp"):
        for b in bufs:
            self._wait(ek, b.last_w)

    def close(self):
        self.es.close()


class _Scope:
    def __init__(self, P):
        self.P = P
        self.st = ExitStack()

    def __enter__(self):
        self.st.__enter__()
        self.bufs = []
        self.P.scopes.append(self.bufs)
        return self.st

    def __exit__(self, *a):
        if a[0] is None:
            self.P.barrier()
            self.P.release_scope(self.bufs)
        self.P.scopes.pop()
        return self.st.__exit__(*a)


class Prefetcher:
    def __init__(self, P, stage, jobs):
        self.P, self.stage, self.jobs = P, stage, jobs
        self.n = 0
        self.m = 0

    def tick(self, k=1):
        P = self.P
        for _ in range(k):
            if self.m < self.n:
                src, dst, srcb, dstb = self.jobs[self.m]
                st_ = self.stage[self.m % len(self.stage)]
                P.dma("sp", dst, st_[:], reads=[st_], writes=[dstb])
                self.m += 1
            if self.n < len(self.jobs):
                src, dst, srcb, dstb = self.jobs[self.n]
                st_ = self.stage[self.n % len(self.stage)]
                P.dma("pool", st_[:], src, reads=[srcb], writes=[st_])
                self.n += 1

    def flush(self):
        while self.m < len(self.jobs):
            self.tick()


def blocks(n, bs):
    out = []
    t = 0
    while t < n:
        out.append((t, min(n, t + bs)))
        t += bs
    return out


def segs_own(t0, t1, ctxn=128):
    out = []
    if t0 < ctxn:
        out.append((t0, min(t1, ctxn), 1))
    if t1 > ctxn:
        out.append((max(t0, ctxn), t1, 0))
    return out


def consts(P, st):
    C = {}
    C["ones_f"] = P.sb(st, "ones_f", [128, 128], F32)
    P.op("dve", lambda e: e.memset(C["ones_f"][:], 1.0), writes=[C["ones_f"]])
    C["ones_b"] = P.sb(st, "ones_b", [128, 128], BF16)
    P.op("dve", lambda e: e.memset(C["ones_b"][:], 1.0), writes=[C["ones_b"]])
    C["one"] = P.sb(st, "one", [128, 1], F32)
    P.op("dve", lambda e: e.memset(C["one"][:], 1.0), writes=[C["one"]])
    C["eps"] = P.sb(st, "eps", [128, 1], F32)
    P.op("dve", lambda e: e.memset(C["eps"][:], EPS), writes=[C["eps"]])
    C["epsa"] = P.sb(st, "epsa", [128, 1], F32)
    P.op("dve", lambda e: e.memset(C["epsa"][:], EPS / (ALPHA * ALPHA)), writes=[C["epsa"]])
    return C


def identity(P, st, C, dt, name):
    idf = P.sb(st, name, [128, 128], dt)
    P.op("pool", lambda e: e.memset(idf[:], 1.0), writes=[idf])
    P.op("pool", lambda e: e.affine_select(out=idf[:], in_=idf[:], pattern=[[-1, 128]], compare_op=ALU.is_equal,
                                           fill=0.0, base=0, channel_multiplier=1), reads=[idf], writes=[idf])
    return idf


def tri_mask(P, st, name, dt, val, sgn, strict=False):
    t = P.sb(st, name, [128, 128], dt)
    P.op("pool", lambda e: e.memset(t[:], val), writes=[t])
    P.op("pool", lambda e: e.affine_select(out=t[:], in_=t[:], pattern=[[sgn, 128]],
                                           compare_op=(ALU.is_gt if strict else ALU.is_ge),
                                           fill=0.0, base=0, channel_multiplier=-sgn), reads=[t], writes=[t])
    return t


def phase_a(P, C, cc, wmod, bmod, xT, mods_o, hT, N=NOWN, ctxn=128):
    with P.scope() as st:
        cs = P.sb(st, "cs", [128, 16, 2], F32)
        bm = P.sb(st, "bm", [128, 96], F32)
        mods = P.sb(st, "mods", [128, 96, 2], F32)
        slabs = [P.sb(st, "slab%d" % i, [128, 16, 512], F32) for i in range(2)]
        pm = P.ps(st, "pm", [128, 192], F32)
        P.dma("sp", cs[:], cc[:], reads=[cc], writes=[cs])
        P.dma("sp", bm[:], bmod[:], reads=[bmod], writes=[bm])
        P.op("act", lambda e: e.activation(out=cs[:], in_=cs[:], func=AF.Silu), reads=[cs], writes=[cs])
        wv = wmod.t.rearrange("(kc p) n -> p kc n", p=128)
        for s in range(24):
            sl = slabs[s % 2]
            P.dma("sp", sl[:], wv[:, :, s * 512:(s + 1) * 512], reads=[wmod], writes=[sl])
            for j in range(4):
                n = s * 4 + j
                for kc in range(16):
                    P.op("pe", lambda e: e.matmul(out=pm[:, 2 * n:2 * n + 2], lhsT=sl[:, kc, j * 128:(j + 1) * 128],
                                                  rhs=cs[:, kc, :], start=(kc == 0), stop=(kc == 15)),
                         reads=[sl, cs], writes=[pm])
        for j in range(2):
            P.op("dve", lambda e: e.tensor_tensor(out=mods[:, :, j],
                                                  in0=pm[:].rearrange("p (n j) -> p n j", j=2)[:, :, j],
                                                  in1=bm[:], op=ALU.add), reads=[pm, bm], writes=[mods])
        P.dma("sp", mods_o[:], mods[:], reads=[mods], writes=[mods_o])
        sc1p = P.sb(st, "sc1p", [128, 16, 2], F32)
        P.op("dve", lambda e: e.tensor_scalar(out=sc1p[:], in0=mods[:, 16:32, :], scalar1=1.0, scalar2=None,
                                              op0=ALU.add), reads=[mods], writes=[sc1p])
        xb = [P.sb(st, "xb%d" % i, [128, N], F32) for i in range(2)]
        hb = [P.sb(st, "hb%d" % i, [128, N], BF16) for i in range(2)]
        for c in range(16):
            x_ = xb[c % 2]
            h_ = hb[c % 2]
            P.dma("sp", x_[:], xT[c * 128:(c + 1) * 128, :], reads=[xT], writes=[x_])
            for (a, b, j) in segs_own(0, N, ctxn):
                P.op("act", lambda e: e.activation(out=h_[:, a:b], in_=x_[:, a:b], func=AF.Identity,
                                                   scale=sc1p[:, c, j:j + 1], bias=mods[:, c, j:j + 1]),
                     reads=[x_, sc1p, mods], writes=[h_])
            P.dma("sp", hT[c * 128:(c + 1) * 128, :], h_[:], reads=[h_], writes=[hT])


def phase_mla(P, C, hT, w_in, qng, w_uq, kvng, w_ukv, cos2, sin2, oT, dbg=None, NHT=8, tick=None):
    NH = 8
    scale = 192.0 ** -0.5
    with P.scope() as st0:
        cqn = P.sb(st0, "cqn", [128, 6, NB], BF16)
        ckvn = P.sb(st0, "ckvn", [128, 2, NB], BF16)
        kr = P.sb(st0, "kr", [64, NB], BF16)
        with P.scope() as st:
            win = P.sb(st, "win", [128, 16, 1088], BF16)
            P.dma("pool", win[:], w_in.t.rearrange("(kc p) n -> p kc n", p=128), reads=[w_in], writes=[win])
            wrot = P.sb(st, "wrot", [128, 16, 64], BF16)
            P.op("dve", lambda e: e.tensor_scalar(out=wrot[:, :, 0:32], in0=win[:, :, 1056:1088], scalar1=-1.0,
                                                  scalar2=None, op0=ALU.mult), reads=[win], writes=[wrot])
            P.op("dve", lambda e: e.tensor_copy(out=wrot[:, :, 32:64], in_=win[:, :, 1024:1056]), reads=[win],
                 writes=[wrot])
            qg = P.sb(st, "qg", [128, 6], F32)
            kg = P.sb(st, "kg", [128, 2], F32)
            P.dma("sp", qg[:], qng[:], reads=[qng], writes=[qg])
            P.dma("sp", kg[:], kvng[:], reads=[kvng], writes=[kg])
            hbs = [P.sb(st, "hblk%d" % i, [128, 16, 512], BF16) for i in range(2)]
            cf = P.sb(st, "cf", [128, 8, 512], F32)
            sq = [P.sb(st, "sq%d" % i, [128, 512], F32) for i in range(2)]
            rs = [P.sb(st, "rs%d" % i, [128, 512], F32) for i in range(2)]
            tb = [P.sb(st, "tb%d" % i, [64, 512], F32) for i in range(4)]
            pj = [P.ps(st, "pj%d" % i, [128, 512], F32) for i in range(3)]
            pss = [P.ps(st, "pss%d" % i, [128, 512], F32) for i in range(2)]
            hv = hT.t.rearrange("(kc p) t -> p kc t", p=128)
            npj = 0
            for bi, (t0, t1) in enumerate(blocks(NB, 512)):
                nt = t1 - t0
                hb_ = hbs[bi % 2]
                P.dma("sp", hb_[:, :, 0:nt], hv[:, :, t0:t1], reads=[hT], writes=[hb_])
                for oc in range(8):
                    pb = pj[npj % 3]
                    npj += 1
                    for kc in range(16):
                        P.op("pe", lambda e: e.matmul(out=pb[:, 0:nt], lhsT=win[:, kc, oc * 128:(oc + 1) * 128],
                                                      rhs=hb_[:, kc, 0:nt], start=(kc == 0), stop=(kc == 15)),
                             reads=[win, hb_], writes=[pb])
                    P.op("act", lambda e: e.activation(out=cf[:, oc, 0:nt], in_=pb[:, 0:nt], func=AF.Copy),
                         reads=[pb], writes=[cf])
                if "rope" not in SKIP:
                    pk = pj[npj % 3]
                    npj += 1
                    pr = pj[npj % 3]
                    npj += 1
                    for kc in range(16):
                        P.op("pe", lambda e: e.matmul(out=pk[0:64, 0:nt], lhsT=win[:, kc, 1024:1088], rhs=hb_[:, kc, 0:nt],
                                                      start=(kc == 0), stop=(kc == 15)), reads=[win, hb_], writes=[pk])
                    for kc in range(16):
                        P.op("pe", lambda e: e.matmul(out=pr[0:64, 0:nt], lhsT=wrot[:, kc, :], rhs=hb_[:, kc, 0:nt],
                                                      start=(kc == 0), stop=(kc == 15)), reads=[wrot, hb_], writes=[pr])
                    cb, sb_ = tb[0], tb[1]
                    P.dma("sp", cb[:, 0:nt], cos2[:, t0:t1], reads=[cos2], writes=[cb])
                    P.dma("sp", sb_[:, 0:nt], sin2[:, t0:t1], reads=[sin2], writes=[sb_])
                    P.op("dve", lambda e: e.tensor_tensor(out=tb[2][:, 0:nt], in0=pk[0:64, 0:nt], in1=cb[:, 0:nt], op=ALU.mult),
                         reads=[pk, cb], writes=[tb[2]])
                    P.op("dve", lambda e: e.tensor_tensor(out=tb[3][:, 0:nt], in0=pr[0:64, 0:nt], in1=sb_[:, 0:nt], op=ALU.mult),
                         reads=[pr, sb_], writes=[tb[3]])
                    P.op("dve", lambda e: e.tensor_tensor(out=kr[:, t0:t1], in0=tb[2][:, 0:nt], in1=tb[3][:, 0:nt], op=ALU.add),
                         reads=[tb[2], tb[3]], writes=[kr])
                for gi, (c0, c1, nf, gt, dst) in enumerate([(0, 6, 768.0, qg, cqn), (6, 8, 256.0, kg, ckvn)]):
                    pssb = pss[gi]
                    for ci in range(c0, c1):
                        s_ = sq[ci % 2]
                        P.op("act", lambda e: e.activation(out=s_[:, 0:nt], in_=cf[:, ci, 0:nt], func=AF.Square),
                             reads=[cf], writes=[s_])
                        P.op("pe", lambda e: e.matmul(out=pssb[:, 0:nt], lhsT=C["ones_f"][:], rhs=s_[:, 0:nt],
                                                      start=(ci == c0), stop=(ci == c1 - 1)),
                             reads=[C["ones_f"], s_], writes=[pssb])
                    r_ = rs[gi]
                    P.op("act", lambda e: e.activation(out=r_[:, 0:nt], in_=pssb[:, 0:nt], func=AF.Sqrt,
                                                       scale=1.0 / nf, bias=C["eps"][:]), reads=[pssb, C["eps"]], writes=[r_])
                    P.op("dve", lambda e: e.reciprocal(out=r_[:, 0:nt], in_=r_[:, 0:nt]), reads=[r_], writes=[r_])
                    for ci in range(c0, c1):
                        P.op("dve", lambda e: e.scalar_tensor_tensor(out=dst[:, ci - c0, t0:t1], in0=cf[:, ci, 0:nt],
                                                                     scalar=gt[:, ci - c0:ci - c0 + 1], in1=r_[:, 0:nt],
                                                                     op0=ALU.mult, op1=ALU.mult),
                             reads=[cf, gt, r_], writes=[dst])
        if dbg is not None:
            P.dma("sp", dbg["cqn"][:], cqn[:], reads=[cqn], writes=[dbg["cqn"]])
            P.dma("sp", dbg["ckvn"][:], ckvn[:], reads=[ckvn], writes=[dbg["ckvn"]])
            P.dma("sp", dbg["kr"][:], kr[:], reads=[kr], writes=[dbg["kr"]])
        for hg in range(NHT // 8):
            with P.scope() as st:
                wuq = P.sb(st, "wuq", [128, 6, NH * 192], BF16)
                P.dma("pool", wuq[:], w_uq.t.rearrange("(kc p) n -> p kc n", p=128)[:, :, hg * 1536:(hg + 1) * 1536], reads=[w_uq], writes=[wuq])
                wuqr = P.sb(st, "wuqr", [128, 6, NH * 64], BF16)
                wv4 = wuq[:].rearrange("p k (h d) -> p k h d", d=192)
                wr4 = wuqr[:].rearrange("p k (h d) -> p k h d", d=64)
                for kc in range(6):
                    P.op("dve", lambda e: e.tensor_scalar(out=wr4[:, kc, :, 0:32], in0=wv4[:, kc, :, 160:192], scalar1=-1.0,
                                                          scalar2=None, op0=ALU.mult), reads=[wuq], writes=[wuqr])
                    P.op("dve", lambda e: e.tensor_copy(out=wr4[:, kc, :, 32:64], in_=wv4[:, kc, :, 128:160]), reads=[wuq],
                         writes=[wuqr])
                wukv = P.sb(st, "wukv", [128, 2, NH * 256], BF16)
                P.dma("pool", wukv[:], w_ukv.t.rearrange("(kc p) n -> p kc n", p=128)[:, :, hg * 2048:(hg + 1) * 2048], reads=[w_ukv], writes=[wukv])
                Kh = [P.sb(st, "Kh%d" % i, [128, NB], BF16) for i in range(2)]
                Vh = [P.sb(st, "Vh%d" % i, [128, 34, 128], BF16) for i in range(2)]
                Qn = [P.sb(st, "Qn%d" % i, [128, 512], BF16) for i in range(2)]
                Qp = [P.sb(st, "Qp%d" % i, [64, 512], BF16) for i in range(2)]
                tq = [P.sb(st, "tq%d" % i, [64, 512], F32) for i in range(4)]
                Pt = [P.sb(st, "Pt%d" % i, [128, 512], BF16) for i in range(3)]
                rl = [P.sb(st, "rl%d" % i, [128, 512], F32) for i in range(2)]
                Lacc = [P.sb(st, "Lacc%d" % i, [128, 512], F32) for i in range(2)]
                ob = [P.sb(st, "ob%d" % i, [128, 512], BF16) for i in range(2)]
                pS = [P.ps(st, "pS%d" % i, [128, 512], F32) for i in range(3)]
                pO = [P.ps(st, "pO%d" % i, [128, 512], F32) for i in range(2)]
                pL = [P.ps(st, "pL%d" % i, [128, 512], F32) for i in range(2)]
                pX = P.ps(st, "pX", [128, 512], F32)
                nS = 0
                nq = 0
                qblocks = [(0, 256, 0, 2)] + [(256 + 512 * i, 256 + 512 * (i + 1), 0, 34) for i in range(8)]
                for h in range(NH):
                    K_ = Kh[h % 2]
                    V_ = Vh[h % 2]
                    for (t0, t1) in blocks(NB, 512):
                        nt = t1 - t0
                        for kc in range(2):
                            P.op("pe", lambda e: e.matmul(out=pX[:, 0:nt], lhsT=wukv[:, kc, h * 256:h * 256 + 128],
                                                          rhs=ckvn[:, kc, t0:t1], start=(kc == 0), stop=(kc == 1)),
                                 reads=[wukv, ckvn], writes=[pX])
                        P.op("act", lambda e: e.activation(out=K_[:, t0:t1], in_=pX[:, 0:nt], func=AF.Copy),
                             reads=[pX], writes=[K_])
                    for g in range(0, 34, 4):
                        ng = min(4, 34 - g)
                        for j in range(ng):
                            tt = g + j
                            for kc in range(2):
                                P.op("pe", lambda e: e.matmul(out=pX[:, j * 128:(j + 1) * 128],
                                                              lhsT=ckvn[:, kc, tt * 128:(tt + 1) * 128],
                                                              rhs=wukv[:, kc, h * 256 + 128:h * 256 + 256],
                                                              start=(kc == 0), stop=(kc == 1)),
                                     reads=[wukv, ckvn], writes=[pX])
                        P.op("dve", lambda e: e.tensor_copy(out=V_[:, g:g + ng, :],
                                                            in_=pX[:, 0:ng * 128].rearrange("p (g d) -> p g d", d=128)),
                             reads=[pX], writes=[V_])
                    for (q0, q1, k0, k1) in qblocks:
                        if tick is not None:
                            tick(3)
                        nt = q1 - q0
                        Qn_, Qp_ = Qn[nq % 2], Qp[nq % 2]
                        pO_, pL_ = pO[nq % 2], pL[nq % 2]
                        rl_, ob_ = rl[nq % 2], ob[nq % 2]
                        La_ = Lacc[nq % 2]
                        nq += 1
                        pa = pS[nS % 3]; nS += 1
                        for kc in range(6):
                            P.op("pe", lambda e: e.matmul(out=pa[:, 0:nt], lhsT=wuq[:, kc, h * 192:h * 192 + 128],
                                                          rhs=cqn[:, kc, q0:q1], start=(kc == 0), stop=(kc == 5)),
                                 reads=[wuq, cqn], writes=[pa])
                        P.op("act", lambda e: e.activation(out=Qn_[:, 0:nt], in_=pa[:, 0:nt], func=AF.Copy),
                             reads=[pa], writes=[Qn_])
                        pb = pS[nS % 3]; nS += 1
                        for kc in range(6):
                            P.op("pe", lambda e: e.matmul(out=pb[0:64, 0:nt], lhsT=wuq[:, kc, h * 192 + 128:h * 192 + 192],
                                                          rhs=cqn[:, kc, q0:q1], start=(kc == 0), stop=(kc == 5)),
                                 reads=[wuq, cqn], writes=[pb])
                        pc = pS[nS % 3]; nS += 1
                        for kc in range(6):
                            P.op("pe", lambda e: e.matmul(out=pc[0:64, 0:nt], lhsT=wuqr[:, kc, h * 64:(h + 1) * 64],
                                                          rhs=cqn[:, kc, q0:q1], start=(kc == 0), stop=(kc == 5)),
                                 reads=[wuqr, cqn], writes=[pc])
                        cb, sb_ = tq[0], tq[1]
                        P.dma("sp", cb[:, 0:nt], cos2[:, q0:q1], reads=[cos2], writes=[cb])
                        P.dma("sp", sb_[:, 0:nt], sin2[:, q0:q1], reads=[sin2], writes=[sb_])
                        P.op("dve", lambda e: e.tensor_tensor(out=tq[2][:, 0:nt], in0=pb[0:64, 0:nt], in1=cb[:, 0:nt], op=ALU.mult),
                             reads=[pb, cb], writes=[tq[2]])
                        P.op("dve", lambda e: e.tensor_tensor(out=tq[3][:, 0:nt], in0=pc[0:64, 0:nt], in1=sb_[:, 0:nt], op=ALU.mult),
                             reads=[pc, sb_], writes=[tq[3]])
                        P.op("dve", lambda e: e.tensor_tensor(out=Qp_[:, 0:nt], in0=tq[2][:, 0:nt], in1=tq[3][:, 0:nt], op=ALU.add),
                             reads=[tq[2], tq[3]], writes=[Qp_])
                        AHEAD = 2
                        slots_ = {}

                        def issue_s(kt):
                            nonlocal nS
                            pS_ = pS[nS % 3]
                            Pt_ = Pt[nS % 3]
                            nS += 1
                            P.op("pe", lambda e: e.matmul(out=pS_[:, 0:nt], lhsT=K_[:, kt * 128:(kt + 1) * 128], rhs=Qn_[:, 0:nt],
                                                          start=True, stop=False), reads=[K_, Qn_], writes=[pS_])
                            P.op("pe", lambda e: e.matmul(out=pS_[:, 0:nt], lhsT=kr[:, kt * 128:(kt + 1) * 128], rhs=Qp_[:, 0:nt],
                                                          start=False, stop=True), reads=[kr, Qp_], writes=[pS_])
                            slots_[kt] = (pS_, Pt_)

                        for kt in range(k0, min(k1, k0 + AHEAD)):
                            issue_s(kt)
                        for kt in range(k0, k1):
                            if kt + AHEAD < k1:
                                issue_s(kt + AHEAD)
                            pS_, Pt_ = slots_.pop(kt)
                            P.op("act", lambda e: e.activation(out=Pt_[:, 0:nt], in_=pS_[:, 0:nt], func=AF.Exp, scale=scale),
                                 reads=[pS_], writes=[Pt_])
                            P.op("pe", lambda e: e.matmul(out=pO_[:, 0:nt], lhsT=V_[:, kt, :], rhs=Pt_[:, 0:nt],
                                                          start=(kt == k0), stop=(kt == k1 - 1)), reads=[V_, Pt_], writes=[pO_])
                            if kt == k0:
                                P.op("dve", lambda e: e.tensor_copy(out=La_[:, 0:nt], in_=Pt_[:, 0:nt]), reads=[Pt_], writes=[La_])
                            else:
                                P.op("dve", lambda e: e.tensor_tensor(out=La_[:, 0:nt], in0=La_[:, 0:nt], in1=Pt_[:, 0:nt],
                                                                      op=ALU.add), reads=[La_, Pt_], writes=[La_])
                        P.op("pe", lambda e: e.matmul(out=pL_[:, 0:nt], lhsT=C["ones_f"][:], rhs=La_[:, 0:nt], start=True,
                                                      stop=True), reads=[C["ones_f"], La_], writes=[pL_])
                        P.op("dve", lambda e: e.reciprocal(out=rl_[:, 0:nt], in_=pL_[:, 0:nt]), reads=[pL_], writes=[rl_])
                        P.op("dve", lambda e: e.tensor_tensor(out=ob_[:, 0:nt], in0=pO_[:, 0:nt], in1=rl_[:, 0:nt], op=ALU.mult),
                             reads=[pO_, rl_], writes=[ob_])
                        P.dma("sp", oT[(hg * 8 + h) * 128:(hg * 8 + h + 1) * 128, q0:q1], ob_[:, 0:nt], reads=[ob_], writes=[oT])


def phase_gla(P, C, hT, w_in, ga, gb, gbias, ng, oT, oscr, NHL=2, tick=None):
    NT = 34
    with P.scope() as st0:
        ident = identity(P, st0, C, BF16, "identb")
        triF = tri_mask(P, st0, "triF", F32, -1.0 / 16.0, 1)
        triB = tri_mask(P, st0, "triB", F32, -1.0 / 16.0, -1)
        mskF = tri_mask(P, st0, "mskF", F32, 1.0, 1)
        mskB = tri_mask(P, st0, "mskB", F32, 1.0, -1)
        uT = [P.sb(st0, "uT%d" % d, [17, NB], BF16) for d in range(2)]
        gbw = [P.sb(st0, "gbw%d" % d, [17, NHL * 256], BF16) for d in range(2)]
        ngb = P.sb(st0, "ngb", [128, 512], F32)
        hv = hT.t.rearrange("(kc p) t -> p kc t", p=128)
        with P.scope() as st:
            gaw = P.sb(st, "gaw", [128, 16, 32], BF16)
            P.dma("pool", gaw[:], ga.t.rearrange("(kc p) n -> p kc n", p=128), reads=[ga], writes=[gaw])
            for d in range(2):
                P.dma("pool", gbw[d][0:16, :], gb[d], reads=[gb], writes=[gbw[d]])
                P.dma("pool", gbw[d][16:17, :], gbias[d], reads=[gbias], writes=[gbw[d]])
                P.op("dve", lambda e: e.memset(uT[d][:], 1.0), writes=[uT[d]])
            ngr = P.sb(st, "ngr", [1, 512], F32)
            P.dma("sp", ngr[:], ng[:], reads=[ng], writes=[ngr])
            pu = [P.ps(st, "pu%d" % i, [128, 512], F32) for i in range(2)]
            P.op("pe", lambda e: e.matmul(out=pu[0][:, :], lhsT=C["ones_f"][0:1, :], rhs=ngr[:], start=True, stop=True),
                 reads=[C["ones_f"], ngr], writes=[pu[0]])
            P.op("act", lambda e: e.activation(out=ngb[:], in_=pu[0][:, :], func=AF.Copy), reads=[pu[0]], writes=[ngb])
            hbs = [P.sb(st, "hblk%d" % i, [128, 16, 512], BF16) for i in range(2)]
            for bi, (t0, t1) in enumerate(blocks(NB, 512)):
                nt = t1 - t0
                hb_ = hbs[bi % 2]
                P.dma("sp", hb_[:, :, 0:nt], hv[:, :, t0:t1], reads=[hT], writes=[hb_])
                for d in range(2):
                    for kc in range(16):
                        P.op("pe", lambda e: e.matmul(out=pu[d][0:16, 0:nt], lhsT=gaw[:, kc, d * 16:(d + 1) * 16],
                                                      rhs=hb_[:, kc, 0:nt], start=(kc == 0), stop=(kc == 15)),
                             reads=[gaw, hb_], writes=[pu[d]])
                    P.op("act", lambda e: e.activation(out=uT[d][0:16, t0:t1], in_=pu[d][0:16, 0:nt], func=AF.Copy),
                         reads=[pu[d]], writes=[uT[d]])
        for hl in range(NHL):
            with P.scope() as st1:
                qT = P.sb(st1, "qT", [128, 2, NB], BF16)
                kT = P.sb(st1, "kT", [128, 2, NB], BF16)
                vv = P.sb(st1, "vv", [128, NT, 512], BF16)
                rr = P.sb(st1, "rr", [128, NT, 512], BF16)
                for sub in range(2):
                    if "proj" in SKIP:
                        continue
                    with P.scope() as st:
                        ncols = 512 if sub == 0 else 1024
                        c_off = hl * 1536 + (0 if sub == 0 else 512)
                        wh = P.sb(st, "wh", [128, 16, ncols], BF16)
                        P.dma("pool", wh[:], w_in.t.rearrange("(kc p) n -> p kc n", p=128)[:, :, c_off:c_off + ncols],
                              reads=[w_in], writes=[wh])
                        hbs = [P.sb(st, "hblk%d" % i, [128, 16, 256], BF16) for i in range(2)]
                        pp = [P.ps(st, "pp%d" % i, [128, 512], F32) for i in range(4)]
                        npp = 0
                        for bi, (t0, t1) in enumerate(blocks(NB, 256)):
                            nt = t1 - t0
                            hb_ = hbs[bi % 2]
                            P.dma("sp", hb_[:, :, 0:nt], hv[:, :, t0:t1], reads=[hT], writes=[hb_])
                            if sub == 0:
                                for oc in range(4):
                                    pb = pp[npp % 4]; npp += 1
                                    for kc in range(16):
                                        P.op("pe", lambda e: e.matmul(out=pb[:, 0:nt], lhsT=wh[:, kc, oc * 128:(oc + 1) * 128],
                                                                      rhs=hb_[:, kc, 0:nt], start=(kc == 0), stop=(kc == 15)),
                                             reads=[wh, hb_], writes=[pb])
                                    if oc < 2:
                                        P.op("act", lambda e: e.activation(out=qT[:, oc, t0:t1], in_=pb[:, 0:nt], func=AF.Copy,
                                                                           scale=1.0 / 16.0), reads=[pb], writes=[qT])
                                    else:
                                        P.op("dve", lambda e: e.tensor_copy(out=kT[:, oc - 2, t0:t1], in_=pb[:, 0:nt]),
                                             reads=[pb], writes=[kT])
                            else:
                                for j in range(nt // 128):
                                    tt = t0 // 128 + j
                                    pb = pp[npp % 4]; npp += 1
                                    for kc in range(16):
                                        P.op("pe", lambda e: e.matmul(out=pb[:, :], lhsT=hb_[:, kc, j * 128:(j + 1) * 128],
                                                                      rhs=wh[:, kc, 0:512], start=(kc == 0), stop=(kc == 15)),
                                             reads=[wh, hb_], writes=[pb])
                                    P.op("dve", lambda e: e.tensor_copy(out=vv[:, tt, :], in_=pb[:, :]), reads=[pb], writes=[vv])
                                    pb = pp[npp % 4]; npp += 1
                                    for kc in range(16):
                                        P.op("pe", lambda e: e.matmul(out=pb[:, :], lhsT=hb_[:, kc, j * 128:(j + 1) * 128],
                                                                      rhs=wh[:, kc, 512:1024], start=(kc == 0), stop=(kc == 15)),
                                             reads=[wh, hb_], writes=[pb])
                                    P.op("act", lambda e: e.activation(out=rr[:, tt, :], in_=pb[:, :], func=AF.Silu),
                                         reads=[pb], writes=[rr])
                with P.scope() as st:
                    S = P.sb(st, "S", [128, 2, 512], F32)
                    Sb = P.sb(st, "Sb", [128, 2, 512], BF16)
                    e1 = [P.sb(st, "e1_%d" % i, [128, 256], F32) for i in range(2)]
                    bl = [P.sb(st, "bl%d" % i, [128, 2], F32) for i in range(2)]
                    el = [P.sb(st, "el%d" % i, [128, 2], F32) for i in range(2)]
                    E1 = [P.sb(st, "E1_%d" % i, [128, 2, 128], F32) for i in range(2)]
                    E2 = [P.sb(st, "E2_%d" % i, [128, 2, 128], F32) for i in range(2)]
                    E3 = [P.sb(st, "E3_%d" % i, [128, 2, 128], F32) for i in range(2)]
                    qt = [P.sb(st, "qt%d" % i, [128, 2, 128], BF16) for i in range(2)]
                    kt_ = [P.sb(st, "kt%d" % i, [128, 2, 128], BF16) for i in range(2)]
                    kh = [P.sb(st, "kh%d" % i, [128, 2, 128], BF16) for i in range(2)]
                    khT = [P.sb(st, "khT%d" % i, [128, 256], BF16) for i in range(2)]
                    At = [P.sb(st, "At%d" % i, [128, 128], BF16) for i in range(2)]
                    of_ = [P.sb(st, "of%d" % i, [128, 512], F32) for i in range(2)]
                    osum = [P.sb(st, "osum%d" % i, [128, 512], F32) for i in range(2)]
                    junk = P.sb(st, "junk", [128, 512], F32)
                    ss = [P.sb(st, "ss%d" % i, [128, 1], F32) for i in range(2)]
                    on = [P.sb(st, "on%d" % i, [128, 512], BF16) for i in range(2)]
                    onT = [P.sb(st, "onT%d" % i, [128, 4, 128], BF16) for i in range(2)]
                    pg = P.ps(st, "pg", [128, 512], F32)
                    pbT = P.ps(st, "pbT", [128, 2, 128], F32)
                    pA = P.ps(st, "pA", [128, 512], F32)
                    po = [P.ps(st, "po%d" % i, [128, 512], F32) for i in range(2)]
                    pdS = [P.ps(st, "pdS%d" % i, [128, 512], F32) for i in range(2)]
                    ptr = P.ps(st, "ptr", [128, 1024], BF16)
                    for d in range(2):
                        if "scan" in SKIP:
                            continue
                        tri = triF if d == 0 else triB
                        msk = mskF if d == 0 else mskB
                        lastcol = 127 if d == 0 else 0
                        order = list(range(NT)) if d == 0 else [1, 0] + list(range(NT - 1, 1, -1))
                        P.op("dve", lambda e: e.memset(S[:], 0.0), writes=[S])
                        P.op("dve", lambda e: e.memset(Sb[:], 0.0), writes=[Sb])
                        def part1(ci, tt):
                                i2 = ci % 2
                                ts = slice(tt * 128, (tt + 1) * 128)
                                P.op("pe", lambda e: e.matmul(out=pg[:, 0:256], lhsT=uT[d][0:17, ts],
                                                              rhs=gbw[d][0:17, hl * 256:(hl + 1) * 256], start=True, stop=True),
                                     reads=[uT[d], gbw[d]], writes=[pg])
                                P.op("act", lambda e: e.activation(out=e1[i2][:], in_=pg[:, 0:256], func=AF.Exp, scale=-1.0),
                                     reads=[pg], writes=[e1[i2]])
                                P.op("act", lambda e: e.activation(out=e1[i2][:], in_=e1[i2][:], func=AF.Ln, bias=C["one"][:]),
                                     reads=[e1[i2], C["one"]], writes=[e1[i2]])
                                for dk in range(2):
                                    P.op("pe", lambda e: e.matmul(out=pbT[:, dk, :], lhsT=e1[i2][:, dk * 128:(dk + 1) * 128],
                                                                  rhs=tri[:], start=True, stop=True),
                                         reads=[e1[i2], tri], writes=[pbT])
                                P.op("dve", lambda e: e.tensor_copy(out=bl[i2][:], in_=pbT[:, :, lastcol]), reads=[pbT],
                                     writes=[bl[i2]])
                                for dk in range(2):
                                    P.op("act", lambda e: e.activation(out=E1[i2][:, dk, :], in_=pbT[:, dk, :], func=AF.Exp),
                                         reads=[pbT], writes=[E1[i2]])
                                    P.op("act", lambda e: e.activation(out=E2[i2][:, dk, :], in_=pbT[:, dk, :], func=AF.Exp,
                                                                       scale=-1.0), reads=[pbT], writes=[E2[i2]])
                                for dk in range(2):
                                    P.op("act", lambda e: e.activation(out=E3[i2][:, dk, :], in_=pbT[:, dk, :], func=AF.Exp,
                                                                       scale=-1.0, bias=bl[i2][:, dk:dk + 1]),
                                         reads=[pbT, bl[i2]], writes=[E3[i2]])
                                P.op("act", lambda e: e.activation(out=el[i2][:], in_=bl[i2][:], func=AF.Exp), reads=[bl[i2]],
                                     writes=[el[i2]])
                                P.op("dve", lambda e: e.tensor_tensor(out=qt[i2][:], in0=qT[:, :, ts], in1=E1[i2][:], op=ALU.mult),
                                     reads=[qT, E1[i2]], writes=[qt[i2]])
                                P.op("pool", lambda e: e.tensor_tensor(out=kt_[i2][:], in0=kT[:, :, ts], in1=E2[i2][:], op=ALU.mult),
                                     reads=[kT, E2[i2]], writes=[kt_[i2]])
                                P.op("pool", lambda e: e.tensor_tensor(out=kh[i2][:], in0=kT[:, :, ts], in1=E3[i2][:], op=ALU.mult),
                                     reads=[kT, E3[i2]], writes=[kh[i2]])
                                for dk in range(2):
                                    P.op("pe", lambda e: e.transpose(out=ptr[:, dk * 128:(dk + 1) * 128], in_=kh[i2][:, dk, :],
                                                                     identity=ident[:]), reads=[kh[i2], ident], writes=[ptr])
                                P.op("dve", lambda e: e.tensor_copy(out=khT[i2][:], in_=ptr[:, 0:256]), reads=[ptr],
                                     writes=[khT[i2]])
                                for dk in range(2):
                                    P.op("pe", lambda e: e.matmul(out=pA[:, 0:128], lhsT=kt_[i2][:, dk, :], rhs=qt[i2][:, dk, :],
                                                                  start=(dk == 0), stop=(dk == 1)),
                                         reads=[kt_[i2], qt[i2]], writes=[pA])
                                P.op("dve", lambda e: e.tensor_tensor(out=At[i2][:], in0=pA[:, 0:128], in1=msk[:], op=ALU.mult),
                                     reads=[pA, msk], writes=[At[i2]])

                        def part2(ci, tt):
                                i2 = ci % 2
                                ts = slice(tt * 128, (tt + 1) * 128)
                                po_ = po[i2]
                                for dk in range(2):
                                    P.op("pe", lambda e: e.matmul(out=po_[:, :], lhsT=qt[i2][:, dk, :], rhs=Sb[:, dk, :],
                                                                  start=(dk == 0), stop=False), reads=[qt[i2], Sb], writes=[po_])
                                P.op("pe", lambda e: e.matmul(out=po_[:, :], lhsT=At[i2][:], rhs=vv[:, tt, :], start=False,
                                                              stop=True), reads=[At[i2], vv], writes=[po_])
                                for dk in range(2):
                                    P.op("pe", lambda e: e.matmul(out=pdS[dk][:, :], lhsT=khT[i2][:, dk * 128:(dk + 1) * 128],
                                                                  rhs=vv[:, tt, :], start=True, stop=True),
                                         reads=[khT[i2], vv], writes=[pdS[dk]])
                                for dk in range(2):
                                    P.op("dve", lambda e: e.scalar_tensor_tensor(out=S[:, dk, :], in0=S[:, dk, :],
                                                                                 scalar=el[i2][:, dk:dk + 1], in1=pdS[dk][:, :],
                                                                                 op0=ALU.mult, op1=ALU.add),
                                         reads=[S, el[i2], pdS[dk]], writes=[S])
                                P.op("act", lambda e: e.activation(out=Sb[:], in_=S[:], func=AF.Copy), reads=[S], writes=[Sb])
                                if d == 0:
                                    P.op("act", lambda e: e.activation(out=of_[i2][:], in_=po_[:, :], func=AF.Copy),
                                         reads=[po_], writes=[of_[i2]])
                                    P.dma("sp", oscr[ts, :], of_[i2][:], reads=[of_[i2]], writes=[oscr])
                                else:
                                    P.dma("sp", of_[i2][:], oscr[ts, :], reads=[oscr], writes=[of_[i2]])
                                    P.op("dve", lambda e: e.tensor_tensor(out=osum[i2][:], in0=po_[:, :], in1=of_[i2][:],
                                                                          op=ALU.add), reads=[po_, of_[i2]], writes=[osum[i2]])
                                    P.op("dve", lambda e: e.tensor_tensor(out=junk[:], in0=osum[i2][:], in1=osum[i2][:], op=ALU.mult),
                                         reads=[osum[i2]], writes=[junk])
                                    P.op("dve", lambda e: e.tensor_reduce(out=ss[i2][:], in_=junk[:], axis=AX.X, op=ALU.add),
                                         reads=[junk], writes=[ss[i2]])
                                    P.op("act", lambda e: e.activation(out=ss[i2][:], in_=ss[i2][:], func=AF.Sqrt,
                                                                       scale=1.0 / 512.0, bias=C["eps"][:]),
                                         reads=[ss[i2], C["eps"]], writes=[ss[i2]])
                                    P.op("dve", lambda e: e.reciprocal(out=ss[i2][:], in_=ss[i2][:]), reads=[ss[i2]],
                                         writes=[ss[i2]])
                                    P.op("dve", lambda e: e.scalar_tensor_tensor(out=osum[i2][:], in0=osum[i2][:],
                                                                                 scalar=ss[i2][:, 0:1], in1=ngb[:],
                                                                                 op0=ALU.mult, op1=ALU.mult),
                                         reads=[osum[i2], ss[i2], ngb], writes=[osum[i2]])
                                    P.op("dve", lambda e: e.tensor_tensor(out=on[i2][:], in0=osum[i2][:], in1=rr[:, tt, :],
                                                                          op=ALU.mult), reads=[osum[i2], rr], writes=[on[i2]])
                                    for dv in range(4):
                                        P.op("pe", lambda e: e.transpose(out=ptr[:, 256 + dv * 128:256 + (dv + 1) * 128],
                                                                         in_=on[i2][:, dv * 128:(dv + 1) * 128],
                                                                         identity=ident[:]), reads=[on[i2], ident], writes=[ptr])
                                    P.op("act", lambda e: e.activation(out=onT[i2][:], in_=ptr[:, 256:768].rearrange(
                                        "p (a b) -> p a b", b=128), func=AF.Copy), reads=[ptr], writes=[onT[i2]])
                                    P.dma("sp", oT[hl * 512:(hl + 1) * 512, ts].rearrange("(a p) t -> p a t", p=128),
                                          onT[i2][:], reads=[onT[i2]], writes=[oT])


                        part1(0, order[0])
                        for ci, tt in enumerate(order):
                            if tick is not None:
                                tick(2)
                            if ci + 1 < len(order):
                                part1(ci + 1, order[ci + 1])
                            part2(ci, tt)


def ln_block(P, C, st_bufs, z, c0, nt, out_fn, pst):
    sq, mean, rstd, tmp = st_bufs
    ps_s, ps_q = pst
    for fc in range(16):
        s_ = sq[fc % 2]
        P.op("act", lambda e: e.activation(out=s_[:, 0:nt], in_=z[:, fc, c0:c0 + nt], func=AF.Square), reads=[z], writes=[s_])
        P.op("pe", lambda e: e.matmul(out=ps_s[:, 0:nt], lhsT=C["ones_f"][:], rhs=z[:, fc, c0:c0 + nt], start=(fc == 0),
                                      stop=(fc == 15)), reads=[C["ones_f"], z], writes=[ps_s])
        P.op("pe", lambda e: e.matmul(out=ps_q[:, 0:nt], lhsT=C["ones_f"][:], rhs=s_[:, 0:nt], start=(fc == 0),
                                      stop=(fc == 15)), reads=[C["ones_f"], s_], writes=[ps_q])
    P.op("act", lambda e: e.activation(out=mean[:, 0:nt], in_=ps_s[:, 0:nt], func=AF.Copy, scale=1.0 / D),
         reads=[ps_s], writes=[mean])
    P.op("dve", lambda e: e.tensor_tensor(out=rstd[:, 0:nt], in0=mean[:, 0:nt], in1=mean[:, 0:nt], op=ALU.mult),
         reads=[mean], writes=[rstd])
    P.op("dve", lambda e: e.scalar_tensor_tensor(out=rstd[:, 0:nt], in0=ps_q[:, 0:nt], scalar=1.0 / D, in1=rstd[:, 0:nt],
                                                 op0=ALU.mult, op1=ALU.subtract), reads=[ps_q, rstd], writes=[rstd])
    P.op("act", lambda e: e.activation(out=rstd[:, 0:nt], in_=rstd[:, 0:nt], func=AF.Sqrt, bias=C["epsa"][:]),
         reads=[rstd, C["epsa"]], writes=[rstd])
    P.op("dve", lambda e: e.reciprocal(out=rstd[:, 0:nt], in_=rstd[:, 0:nt]), reads=[rstd], writes=[rstd])
    for fc in range(16):
        t_ = tmp[fc % 2]
        P.op("dve", lambda e: e.tensor_tensor(out=t_[:, 0:nt], in0=z[:, fc, c0:c0 + nt], in1=mean[:, 0:nt], op=ALU.subtract),
             reads=[z, mean], writes=[t_])
        P.op("dve", lambda e: e.tensor_tensor(out=t_[:, 0:nt], in0=t_[:, 0:nt], in1=rstd[:, 0:nt], op=ALU.mult),
             reads=[t_, rstd], writes=[t_])
        out_fn(fc, t_)


def phase_c1(P, C, xT, mods_d, oTf, w_o, lng, lnb, wr, br, x1T, h2T, wgT, N=NOWN, ctxn=128, h2tok=None, rt=None):
    with P.scope() as st:
        wo = P.sb(st, "wo", [128, 16, D], BF16)
        P.dma("pool", wo[:], w_o.t.rearrange("(kc p) n -> p kc n", p=128), reads=[w_o], writes=[wo])
        mods = P.sb(st, "mods", [128, 96, 2], F32)
        P.dma("sp", mods[:], mods_d[:], reads=[mods_d], writes=[mods])
        g1a = P.sb(st, "g1a", [128, 16, 2], F32)
        P.op("dve", lambda e: e.tensor_scalar(out=g1a[:], in0=mods[:, 32:48, :], scalar1=1.0 / ALPHA, scalar2=None,
                                              op0=ALU.mult), reads=[mods], writes=[g1a])
        sc2p = P.sb(st, "sc2p", [128, 16, 2], F32)
        P.op("dve", lambda e: e.tensor_scalar(out=sc2p[:], in0=mods[:, 64:80, :], scalar1=1.0, scalar2=None, op0=ALU.add),
             reads=[mods], writes=[sc2p])
        g_ = P.sb(st, "lng", [128, 16], F32)
        b_ = P.sb(st, "lnb", [128, 16], F32)
        P.dma("sp", g_[:], lng[:], reads=[lng], writes=[g_])
        P.dma("sp", b_[:], lnb[:], reads=[lnb], writes=[b_])
        wrt = P.sb(st, "wrt", [128, 16, 36], F32)
        P.dma("sp", wrt[:], wr.t.rearrange("(kc p) n -> p kc n", p=128), reads=[wr], writes=[wrt])
        brr = P.sb(st, "brr", [1, 36], F32)
        P.dma("sp", brr[:], br[:], reads=[br], writes=[brr])
        brb = P.sb(st, "brb", [128, 36], F32)
        identf = identity(P, st, C, F32, "identf")
        if h2tok is not None:
            identb = identity(P, st, C, BF16, "identb1")
            ptb = P.ps(st, "ptb", [128, 1024], BF16)
            htk = [P.sb(st, "htk%d" % i, [128, D], BF16) for i in range(2)]
            rts = [P.sb(st, "rts%d" % i, [128, 68], F32) for i in range(2)]
            ntk = 0
        BS = 256
        ob = [P.sb(st, "ob%d" % i, [128, 16, BS], BF16) for i in range(2)]
        xb = [P.sb(st, "xb%d" % i, [128, 16, BS], F32) for i in range(2)]
        z = P.sb(st, "z", [128, 16, BS], F32)
        x1 = P.sb(st, "x1", [128, 16, BS], F32)
        h2f = P.sb(st, "h2f", [128, 16, BS], F32)
        h2b = P.sb(st, "h2b", [128, 16, BS], BF16)
        sq = [P.sb(st, "sq%d" % i, [128, BS], F32) for i in range(2)]
        tmp = [P.sb(st, "tmp%d" % i, [128, BS], F32) for i in range(2)]
        mean = P.sb(st, "mean", [128, BS], F32)
        rstd = P.sb(st, "rstd", [128, BS], F32)
        lg = P.sb(st, "lg", [128, 36], F32)
        sm = [P.sb(st, "sm%d" % i, [128, 40], F32) for i in range(6)]
        wg = P.sb(st, "wg", [128, 32], F32)
        wgt_sb = P.sb(st, "wgt_sb", [32, BS], F32)
        py = [P.ps(st, "py%d" % i, [128, 512], F32) for i in range(2)]
        ps_s = P.ps(st, "ps_s", [128, 512], F32)
        ps_q = P.ps(st, "ps_q", [128, 512], F32)
        plg = P.ps(st, "plg", [128, 512], F32)
        pwt = P.ps(st, "pwt", [128, 512], F32)
        P.op("pe", lambda e: e.matmul(out=plg[:, 0:36], lhsT=C["ones_f"][0:1, :], rhs=brr[:], start=True, stop=True),
             reads=[C["ones_f"], brr], writes=[plg])
        P.op("act", lambda e: e.activation(out=brb[:], in_=plg[:, 0:36], func=AF.Copy), reads=[plg], writes=[brb])
        ov = oTf.t.rearrange("(kc p) t -> p kc t", p=128)
        xv = xT.t.rearrange("(kc p) t -> p kc t", p=128)
        x1v = x1T.t.rearrange("(kc p) t -> p kc t", p=128)
        h2v = h2T.t.rearrange("(kc p) t -> p kc t", p=128)
        for bi, (t0, t1) in enumerate(blocks(N, BS)):
            nt = t1 - t0
            sg = [(a - t0, b - t0, j) for (a, b, j) in segs_own(t0, t1, ctxn)]
            ob_, xb_ = ob[bi % 2], xb[bi % 2]
            P.dma("sp", ob_[:, :, 0:nt], ov[:, :, t0:t1], reads=[oTf], writes=[ob_])
            P.dma("sp", xb_[:, :, 0:nt], xv[:, :, t0:t1], reads=[xT], writes=[xb_])
            for fc in range(16):
                pb = py[fc % 2]
                for kc in range(16):
                    P.op("pe", lambda e: e.matmul(out=pb[:, 0:nt], lhsT=wo[:, kc, fc * 128:(fc + 1) * 128], rhs=ob_[:, kc, 0:nt],
                                                  start=(kc == 0), stop=(kc == 15)), reads=[wo, ob_], writes=[pb])
                for (a, b, j) in sg:
                    P.op("dve", lambda e: e.scalar_tensor_tensor(out=z[:, fc, a:b], in0=pb[:, a:b], scalar=g1a[:, fc, j:j + 1],
                                                                 in1=xb_[:, fc, a:b], op0=ALU.mult, op1=ALU.add),
                         reads=[pb, g1a, xb_], writes=[z])

            def outf(fc, t_):
                P.op("act", lambda e: e.activation(out=x1[:, fc, 0:nt], in_=t_[:, 0:nt], func=AF.Identity,
                                                   scale=g_[:, fc:fc + 1], bias=b_[:, fc:fc + 1]), reads=[t_, g_, b_], writes=[x1])
                for (a, b, j) in sg:
                    P.op("act", lambda e: e.activation(out=h2f[:, fc, a:b], in_=x1[:, fc, a:b], func=AF.Identity,
                                                       scale=sc2p[:, fc, j:j + 1], bias=mods[:, 48 + fc, j:j + 1]),
                         reads=[x1, sc2p, mods], writes=[h2f])
                P.op("pool", lambda e: e.tensor_copy(out=h2b[:, fc, 0:nt], in_=h2f[:, fc, 0:nt]), reads=[h2f], writes=[h2b])

            ln_block(P, C, (sq, mean, rstd, tmp), z, 0, nt, outf, (ps_s, ps_q))
            P.dma("sp", x1v[:, :, t0:t1], x1[:, :, 0:nt], reads=[x1], writes=[x1T])
            P.dma("sp", h2v[:, :, t0:t1], h2b[:, :, 0:nt], reads=[h2b], writes=[h2T])
            for j in range(nt // 128):
                for kc in range(16):
                    P.op("pe", lambda e: e.matmul(out=plg[:, 0:36], lhsT=h2f[:, kc, j * 128:(j + 1) * 128], rhs=wrt[:, kc, :],
                                                  start=(kc == 0), stop=(kc == 15)), reads=[h2f, wrt], writes=[plg])
                P.op("dve", lambda e: e.tensor_tensor(out=lg[:], in0=plg[:, 0:36], in1=brb[:], op=ALU.add), reads=[plg, brb],
                     writes=[lg])
                gmax, gsum, goh, m1, m2, wk = sm
                P.op("dve", lambda e: e.tensor_reduce(out=gmax[:, 0:1], in_=lg[:, 0:4], axis=AX.X, op=ALU.max), reads=[lg],
                     writes=[gmax])
                P.op("dve", lambda e: e.tensor_scalar(out=goh[:, 0:4], in0=lg[:, 0:4], scalar1=gmax[:, 0:1], scalar2=None,
                                                      op0=ALU.is_ge), reads=[lg, gmax], writes=[goh])
                P.op("dve", lambda e: e.tensor_scalar(out=gsum[:, 0:4], in0=lg[:, 0:4], scalar1=gmax[:, 0:1], scalar2=None,
                                                      op0=ALU.subtract), reads=[lg, gmax], writes=[gsum])
                P.op("act", lambda e: e.activation(out=gsum[:, 0:4], in_=gsum[:, 0:4], func=AF.Exp), reads=[gsum], writes=[gsum])
                P.op("dve", lambda e: e.tensor_reduce(out=gsum[:, 4:5], in_=gsum[:, 0:4], axis=AX.X, op=ALU.add),
                     reads=[gsum], writes=[gsum])
                P.op("dve", lambda e: e.reciprocal(out=gsum[:, 5:6], in_=gsum[:, 4:5]), reads=[gsum], writes=[gsum])
                P.op("dve", lambda e: e.tensor_scalar(out=goh[:, 4:8], in0=goh[:, 0:4], scalar1=-1.0, scalar2=1e30,
                                                      op0=ALU.add, op1=ALU.mult), reads=[goh], writes=[goh])
                for g in range(4):
                    P.op("dve", lambda e: e.tensor_scalar(out=m1[:, g * 8:(g + 1) * 8], in0=lg[:, 4 + g * 8:12 + g * 8],
                                                          scalar1=goh[:, 4 + g:5 + g], scalar2=None, op0=ALU.add),
                         reads=[lg, goh], writes=[m1])
                P.op("dve", lambda e: e.tensor_reduce(out=m1[:, 32:33], in_=m1[:, 0:32], axis=AX.X, op=ALU.max), reads=[m1],
                     writes=[m1])
                P.op("dve", lambda e: e.tensor_scalar(out=m2[:, 0:32], in0=m1[:, 0:32], scalar1=m1[:, 32:33], scalar2=None,
                                                      op0=ALU.is_ge), reads=[m1], writes=[m2])
                P.op("dve", lambda e: e.scalar_tensor_tensor(out=wk[:, 0:32], in0=m2[:, 0:32], scalar=-1e30, in1=m1[:, 0:32],
                                                             op0=ALU.mult, op1=ALU.add), reads=[m2, m1], writes=[wk])
                P.op("dve", lambda e: e.tensor_reduce(out=wk[:, 32:33], in_=wk[:, 0:32], axis=AX.X, op=ALU.max), reads=[wk],
                     writes=[wk])
                P.op("dve", lambda e: e.tensor_scalar(out=wk[:, 0:32], in0=wk[:, 0:32], scalar1=wk[:, 32:33], scalar2=None,
                                                      op0=ALU.is_ge), reads=[wk], writes=[wk])
                P.op("dve", lambda e: e.tensor_tensor(out=wk[:, 33:34], in0=wk[:, 32:33], in1=m1[:, 32:33], op=ALU.subtract),
                     reads=[wk, m1], writes=[wk])
                P.op("act", lambda e: e.activation(out=wk[:, 33:34], in_=wk[:, 33:34], func=AF.Exp), reads=[wk], writes=[wk])
                P.op("dve", lambda e: e.tensor_scalar(out=wk[:, 33:34], in0=wk[:, 33:34], scalar1=1.0, scalar2=None,
                                                      op0=ALU.add), reads=[wk], writes=[wk])
                P.op("dve", lambda e: e.reciprocal(out=wk[:, 34:35], in_=wk[:, 33:34]), reads=[wk], writes=[wk])
                P.op("dve", lambda e: e.tensor_tensor(out=wk[:, 34:35], in0=wk[:, 34:35], in1=gsum[:, 5:6], op=ALU.mult),
                     reads=[wk, gsum], writes=[wk])
                P.op("dve", lambda e: e.tensor_tensor(out=wk[:, 35:36], in0=gsum[:, 5:6], in1=wk[:, 34:35], op=ALU.subtract),
                     reads=[wk, gsum], writes=[wk])
                P.op("dve", lambda e: e.tensor_scalar(out=wg[:], in0=m2[:, 0:32], scalar1=wk[:, 34:35], scalar2=None,
                                                      op0=ALU.mult), reads=[m2, wk], writes=[wg])
                P.op("dve", lambda e: e.scalar_tensor_tensor(out=wg[:], in0=wk[:, 0:32], scalar=wk[:, 35:36], in1=wg[:],
                                                             op0=ALU.mult, op1=ALU.add), reads=[wk, wg], writes=[wg])
                P.op("pe", lambda e: e.transpose(out=pwt[0:32, j * 128:(j + 1) * 128], in_=wg[:], identity=identf[:]),
                     reads=[wg, identf], writes=[pwt])
                if h2tok is not None:
                    tk0 = t0 + j * 128
                    rts_, htk_ = rts[ntk % 2], htk[ntk % 2]
                    ntk += 1
                    P.op("dve", lambda e: e.tensor_copy(out=rts_[:, 0:32], in_=m2[:, 0:32]), reads=[m2], writes=[rts_])
                    P.op("dve", lambda e: e.tensor_copy(out=rts_[:, 32:64], in_=wk[:, 0:32]), reads=[wk], writes=[rts_])
                    P.op("dve", lambda e: e.tensor_copy(out=rts_[:, 64:66], in_=wk[:, 34:36]), reads=[wk], writes=[rts_])
                    P.op("dve", lambda e: e.tensor_copy(out=rts_[:, 66:68], in_=wk[:, 34:36]), reads=[wk], writes=[rts_])
                    P.dma("sp", rt[tk0:tk0 + 128, :], rts_[:], reads=[rts_], writes=[rt])
                    for hh in range(2):
                        for q in range(8):
                            fc = hh * 8 + q
                            P.op("pe", lambda e: e.transpose(out=ptb[:, q * 128:(q + 1) * 128], in_=h2b[:, fc, j * 128:(j + 1) * 128],
                                                             identity=identb[:]), reads=[h2b, identb], writes=[ptb])
                        P.op("act", lambda e: e.activation(out=htk_[:, hh * 1024:(hh + 1) * 1024], in_=ptb[:, :], func=AF.Copy),
                             reads=[ptb], writes=[htk_])
                    P.dma("sp", h2tok[tk0:tk0 + 128, :], htk_[:], reads=[htk_], writes=[h2tok])
            P.op("act", lambda e: e.activation(out=wgt_sb[:, 0:nt], in_=pwt[0:32, 0:nt], func=AF.Copy), reads=[pwt],
                 writes=[wgt_sb])
            P.dma("sp", wgT[:, t0:t1], wgt_sb[:, 0:nt], reads=[wgt_sb], writes=[wgT])


def phase_c2(P, C, x1T, h2T, wgT, mods_d, w1, w3, w2, lng, lnb, xoT, passes=None, ctxn=128):
    if passes is None:
        passes = [(0, 512), (512, 1024), (1024, 1536), (1536, 2176)]
    MAXT = max(b - a for a, b in passes)
    with P.scope() as st:
        mods = P.sb(st, "mods", [128, 96, 2], F32)
        P.dma("sp", mods[:], mods_d[:], reads=[mods_d], writes=[mods])
        g2a = P.sb(st, "g2a", [128, 16, 2], F32)
        P.op("dve", lambda e: e.tensor_scalar(out=g2a[:], in0=mods[:, 80:96, :], scalar1=1.0 / ALPHA, scalar2=None,
                                              op0=ALU.mult), reads=[mods], writes=[g2a])
        g_ = P.sb(st, "lng", [128, 16], F32)
        b_ = P.sb(st, "lnb", [128, 16], F32)
        P.dma("sp", g_[:], lng[:], reads=[lng], writes=[g_])
        P.dma("sp", b_[:], lnb[:], reads=[lnb], writes=[b_])
        id32 = P.sb(st, "id32", [32, 32], F32)
        P.op("pool", lambda e: e.memset(id32[:], 1.0), writes=[id32])
        P.op("pool", lambda e: e.affine_select(out=id32[:], in_=id32[:], pattern=[[-1, 32]], compare_op=ALU.is_equal,
                                               fill=0.0, base=0, channel_multiplier=1), reads=[id32], writes=[id32])
        W1 = [P.sb(st, "W1_%d" % i, [128, 16, 512], BF16) for i in range(2)]
        W3 = [P.sb(st, "W3_%d" % i, [128, 16, 512], BF16) for i in range(2)]
        W2 = [P.sb(st, "W2_%d" % i, [128, 4, D], BF16) for i in range(2)]
        acc = P.sb(st, "acc", [128, 16, MAXT], F32)
        h2b = [P.sb(st, "h2b%d" % i, [128, 16, MAXT], BF16) for i in range(1)]
        wgt = P.sb(st, "wgt", [32, MAXT], F32)
        wm = [P.sb(st, "wm%d" % i, [32, 512], F32) for i in range(2)]
        Wb = [P.sb(st, "Wb%d" % i, [128, 512], F32) for i in range(2)]
        sl = [P.sb(st, "sl%d" % i, [128, 512], F32) for i in range(2)]
        G = [P.sb(st, "G%d" % i, [128, 4, 512], BF16) for i in range(2)]
        x1c = [P.sb(st, "x1c%d" % i, [128, MAXT], F32) for i in range(2)]
        sq = [P.sb(st, "sq%d" % i, [128, 512], F32) for i in range(2)]
        tmp = [P.sb(st, "tmp%d" % i, [128, 512], F32) for i in range(2)]
        mean = P.sb(st, "mean", [128, 512], F32)
        rstd = P.sb(st, "rstd", [128, 512], F32)
        xo = [P.sb(st, "xo%d" % i, [128, 512], F32) for i in range(2)]
        pH1 = [P.ps(st, "pH1_%d" % i, [128, 512], F32) for i in range(2)]
        pH3 = [P.ps(st, "pH3_%d" % i, [128, 512], F32) for i in range(2)]
        pY = [P.ps(st, "pY%d" % i, [128, 512], F32) for i in range(3)]
        pW = P.ps(st, "pW", [128, 512], F32)
        h2v = h2T.t.rearrange("(kc p) t -> p kc t", p=128)
        w1v = w1.t.rearrange("e (kc p) n -> e p kc n", p=128)
        w3v = w3.t.rearrange("e (kc p) n -> e p kc n", p=128)
        w2v = w2.t.rearrange("e (kc p) n -> e p kc n", p=128)
        seq = [(pi, ex) for pi in range(len(passes)) for ex in range(32)]

        def load_w(k):
            ex = seq[k][1]
            P.dma("pool", W1[k % 2][:], w1v[ex], reads=[w1], writes=[W1[k % 2]])
            P.dma("pool", W3[k % 2][:], w3v[ex], reads=[w3], writes=[W3[k % 2]])
            P.dma("pool", W2[k % 2][:], w2v[ex], reads=[w2], writes=[W2[k % 2]])

        load_w(0)
        nG = 0
        nY = 0
        nH = 0
        for k, (pi, ex) in enumerate(seq):
            t0, t1 = passes[pi]
            ntp = t1 - t0
            subs = blocks(ntp, 512)
            h2b_ = h2b[0]
            if ex == 0:
                P.dma("sp", h2b_[:, :, 0:ntp], h2v[:, :, t0:t1], reads=[h2T], writes=[h2b_])
                P.dma("sp", wgt[:, 0:ntp], wgT[:, t0:t1], reads=[wgT], writes=[wgt])
            if k + 1 < len(seq):
                load_w(k + 1)
            W1_, W3_, W2_ = W1[k % 2], W3[k % 2], W2[k % 2]
            for (s0, s1) in subs:
                ns = s1 - s0
                G_ = G[nG % 2]
                Wb_ = Wb[nG % 2]
                wm_ = wm[nG % 2]
                nG += 1
                P.op("dve", lambda e: e.tensor_scalar(out=wm_[:, 0:ns], in0=wgt[:, s0:s1], scalar1=id32[:, ex:ex + 1],
                                                      scalar2=None, op0=ALU.mult), reads=[wgt, id32], writes=[wm_])
                P.op("pe", lambda e: e.matmul(out=pW[:, 0:ns], lhsT=C["ones_f"][0:32, :], rhs=wm_[:, 0:ns], start=True,
                                              stop=True), reads=[C["ones_f"], wm_], writes=[pW])
                P.op("act", lambda e: e.activation(out=Wb_[:, 0:ns], in_=pW[:, 0:ns], func=AF.Copy), reads=[pW],
                     writes=[Wb_])
                for ffc in range(4):
                    p1, p3 = pH1[nH % 2], pH3[nH % 2]
                    sl_ = sl[nH % 2]
                    nH += 1
                    for kc in range(16):
                        P.op("pe", lambda e: e.matmul(out=p1[:, 0:ns], lhsT=W1_[:, kc, ffc * 128:(ffc + 1) * 128],
                                                      rhs=h2b_[:, kc, s0:s1], start=(kc == 0), stop=(kc == 15)),
                             reads=[W1_, h2b_], writes=[p1])
                    for kc in range(16):
                        P.op("pe", lambda e: e.matmul(out=p3[:, 0:ns], lhsT=W3_[:, kc, ffc * 128:(ffc + 1) * 128],
                                                      rhs=h2b_[:, kc, s0:s1], start=(kc == 0), stop=(kc == 15)),
                             reads=[W3_, h2b_], writes=[p3])
                    P.op("act", lambda e: e.activation(out=sl_[:, 0:ns], in_=p1[:, 0:ns], func=AF.Silu), reads=[p1],
                         writes=[sl_])
                    P.op("dve", lambda e: e.tensor_tensor(out=sl_[:, 0:ns], in0=sl_[:, 0:ns], in1=Wb_[:, 0:ns], op=ALU.mult),
                         reads=[sl_, Wb_], writes=[sl_])
                    P.op("dve", lambda e: e.tensor_tensor(out=G_[:, ffc, 0:ns], in0=p3[:, 0:ns], in1=sl_[:, 0:ns], op=ALU.mult),
                         reads=[p3, sl_], writes=[G_])
                for fc in range(16):
                    pY_ = pY[nY % 3]
                    nY += 1
                    for ffc in range(4):
                        P.op("pe", lambda e: e.matmul(out=pY_[:, 0:ns], lhsT=W2_[:, ffc, fc * 128:(fc + 1) * 128],
                                                      rhs=G_[:, ffc, 0:ns], start=(ffc == 0), stop=(ffc == 3)),
                             reads=[W2_, G_], writes=[pY_])
                    if ex == 0:
                        P.op("act", lambda e: e.activation(out=acc[:, fc, s0:s1], in_=pY_[:, 0:ns], func=AF.Copy),
                             reads=[pY_], writes=[acc])
                    else:
                        P.op("dve", lambda e: e.tensor_tensor(out=acc[:, fc, s0:s1], in0=acc[:, fc, s0:s1], in1=pY_[:, 0:ns],
                                                              op=ALU.add), reads=[acc, pY_], writes=[acc])
            if ex != 31:
                continue
            sg = [(a - t0, b - t0, j) for (a, b, j) in segs_own(t0, t1, ctxn)]
            for fc in range(16):
                x1_ = x1c[fc % 2]
                P.dma("sp", x1_[:, 0:ntp], x1T[fc * 128:(fc + 1) * 128, t0:t1], reads=[x1T], writes=[x1_])
                for (a, b, j) in sg:
                    P.op("dve", lambda e: e.scalar_tensor_tensor(out=acc[:, fc, a:b], in0=acc[:, fc, a:b],
                                                                 scalar=g2a[:, fc, j:j + 1], in1=x1_[:, a:b], op0=ALU.mult,
                                                                 op1=ALU.add), reads=[acc, g2a, x1_], writes=[acc])
            for (s0, s1) in subs:
                ns = s1 - s0

                def outf(fc, t_):
                    xo_ = xo[fc % 2]
                    P.op("act", lambda e: e.activation(out=xo_[:, 0:ns], in_=t_[:, 0:ns], func=AF.Identity,
                                                       scale=g_[:, fc:fc + 1], bias=b_[:, fc:fc + 1]), reads=[t_, g_, b_],
                         writes=[xo_])
                    P.dma("sp", xoT[fc * 128:(fc + 1) * 128, t0 + s0:t0 + s1], xo_[:, 0:ns], reads=[xo_], writes=[xoT])

                ln_block(P, C, (sq, mean, rstd, tmp), acc, s0, ns, outf, (pH1[0], pH3[0]))


def phase_c2_sparse(P, C, x1T, h2tok, rt, mods_d, w1, w3, w2, lng, lnb, xoT, ysl, slots, N, ctxn, SB=256, layer=0):
    NT = N // 128
    NBLK = (2 * N) // SB + 32
    SUBS = SB // 128
    w1r, w3r, w2r = w1.t, w3.t, w2.t
    off1 = float(layer * 32 * 128)
    with P.scope() as st0:
        R = P.sb(st0, "R", [128, NT, 68], F32)
        P.dma("sp", R[:], rt.t.rearrange("(T p) c -> p T c", p=128), reads=[rt], writes=[R])
        DI = [P.sb(st0, "DI%d" % k, [128, NT], I32) for k in range(2)]
        g_ = P.sb(st0, "lng", [128, 16], F32)
        b_ = P.sb(st0, "lnb", [128, 16], F32)
        P.dma("sp", g_[:], lng[:], reads=[lng], writes=[g_])
        P.dma("sp", b_[:], lnb[:], reads=[lnb], writes=[b_])
        mods = P.sb(st0, "mods", [128, 96, 2], F32)
        P.dma("sp", mods[:], mods_d[:], reads=[mods_d], writes=[mods])
        g2a = P.sb(st0, "g2a", [128, 16, 2], F32)
        P.op("dve", lambda e: e.tensor_scalar(out=g2a[:], in0=mods[:, 80:96, :], scalar1=1.0 / ALPHA, scalar2=None,
                                              op0=ALU.mult), reads=[mods], writes=[g2a])
        identf = identity(P, st0, C, F32, "identf2")
        identb = identity(P, st0, C, BF16, "identb2")
        with P.scope() as st:
            Mx = P.sb(st, "Mx", [128, NT, 32], F32)
            P.op("dve", lambda e: e.tensor_tensor(out=Mx[:], in0=R[:, :, 0:32], in1=R[:, :, 32:64], op=ALU.add), reads=[R],
                 writes=[Mx])
            U = tri_mask(P, st, "U", F32, 1.0, 1, strict=True)
            RK = P.sb(st, "RK", [128, NT, 32], F32)
            carry = P.sb(st, "carry", [128, 32], F32)
            P.op("dve", lambda e: e.memset(carry[:], 0.0), writes=[carry])
            pr = P.ps(st, "pr", [128, 512], F32)
            pc = P.ps(st, "pc", [128, 512], F32)
            for T in range(NT):
                P.op("pe", lambda e: e.matmul(out=pr[:, 0:32], lhsT=U[:], rhs=Mx[:, T, :], start=True, stop=True),
                     reads=[U, Mx], writes=[pr])
                P.op("dve", lambda e: e.tensor_tensor(out=RK[:, T, :], in0=pr[:, 0:32], in1=carry[:], op=ALU.add),
                     reads=[pr, carry], writes=[RK])
                P.op("pe", lambda e: e.matmul(out=pc[:, 0:32], lhsT=C["ones_f"][:], rhs=Mx[:, T, :], start=True, stop=True),
                     reads=[C["ones_f"], Mx], writes=[pc])
                P.op("dve", lambda e: e.tensor_tensor(out=carry[:], in0=carry[:], in1=pc[:, 0:32], op=ALU.add),
                     reads=[pc, carry], writes=[carry])
            nb = P.sb(st, "nb", [128, 32], F32)
            P.op("dve", lambda e: e.memset(nb[:], 0.0), writes=[nb])
            for jj in range(N // SB + 1):
                P.op("dve", lambda e: e.scalar_tensor_tensor(out=nb[:], in0=carry[:], scalar=float(SB * jj), in1=nb[:],
                                                             op0=ALU.is_gt, op1=ALU.add), reads=[carry, nb], writes=[nb])
            cs = [P.sb(st, "cs%d" % i, [128, 32], F32) for i in range(2)]
            P.op("dve", lambda e: e.tensor_copy(out=cs[0][:], in_=nb[:]), reads=[nb], writes=[cs[0]])
            cur = 0
            for sft in (1, 2, 4, 8, 16):
                a_, b2 = cs[cur], cs[1 - cur]
                P.op("dve", lambda e: e.tensor_copy(out=b2[:, 0:sft], in_=a_[:, 0:sft]), reads=[a_], writes=[b2])
                P.op("dve", lambda e: e.tensor_tensor(out=b2[:, sft:32], in0=a_[:, sft:32], in1=a_[:, 0:32 - sft], op=ALU.add),
                     reads=[a_], writes=[b2])
                cur = 1 - cur
            pend = P.sb(st, "pend", [128, 32], F32)
            pstart = P.sb(st, "pstart", [128, 32], F32)
            P.op("dve", lambda e: e.tensor_scalar(out=pend[:], in0=cs[cur][:], scalar1=float(SB), scalar2=None, op0=ALU.mult),
                 reads=[cs[cur]], writes=[pend])
            P.op("dve", lambda e: e.scalar_tensor_tensor(out=pstart[:], in0=nb[:], scalar=-float(SB), in1=pend[:],
                                                         op0=ALU.mult, op1=ALU.add), reads=[nb, pend], writes=[pstart])
            Dk = [P.sb(st, "Dk%d" % k, [128, NT], F32) for k in range(2)]
            t32 = [P.sb(st, "t32_%d" % i, [128, 32], F32) for i in range(2)]
            for T in range(NT):
                P.op("dve", lambda e: e.tensor_tensor(out=t32[0][:], in0=RK[:, T, :], in1=pstart[:], op=ALU.add),
                     reads=[RK, pstart], writes=[t32[0]])
                for k in range(2):
                    P.op("dve", lambda e: e.tensor_tensor(out=t32[1][:], in0=t32[0][:], in1=R[:, T, 32 * k:32 * k + 32],
                                                          op=ALU.mult), reads=[t32[0], R], writes=[t32[1]])
                    P.op("dve", lambda e: e.tensor_reduce(out=Dk[k][:, T:T + 1], in_=t32[1][:], axis=AX.X, op=ALU.add),
                         reads=[t32[1]], writes=[Dk[k]])
            for k in range(2):
                P.op("dve", lambda e: e.tensor_copy(out=DI[k][:], in_=Dk[k][:]), reads=[Dk[k]], writes=[DI[k]])
            BE = P.sb(st, "BE", [128, NBLK], F32)
            for b in range(NBLK):
                P.op("dve", lambda e: e.tensor_scalar(out=t32[0][:], in0=pend[:], scalar1=float(SB * b), scalar2=None,
                                                      op0=ALU.is_le), reads=[pend], writes=[t32[0]])
                P.op("dve", lambda e: e.tensor_reduce(out=BE[:, b:b + 1], in_=t32[0][:], axis=AX.X, op=ALU.add),
                     reads=[t32[0]], writes=[BE])
            P.op("dve", lambda e: e.tensor_scalar(out=BE[:], in0=BE[:], scalar1=31.0, scalar2=None, op0=ALU.min), reads=[BE],
                 writes=[BE])
            tki = P.sb(st, "tki", [128, NT], I32)
            P.op("pool", lambda e: e.iota(tki[:], pattern=[[128, NT]], base=0, channel_multiplier=1), writes=[tki])
            tkf = P.sb(st, "tkf", [128, NT], F32)
            P.op("dve", lambda e: e.tensor_copy(out=tkf[:], in_=tki[:]), reads=[tki], writes=[tkf])
            TW = [P.sb(st, "TW%d" % k, [128, NT, 2], F32) for k in range(2)]
            for k in range(2):
                P.op("dve", lambda e: e.tensor_copy(out=TW[k][:, :, 0], in_=tkf[:]), reads=[tkf], writes=[TW[k]])
                P.op("dve", lambda e: e.tensor_copy(out=TW[k][:, :, 1], in_=R[:, :, 64 + k]), reads=[R], writes=[TW[k]])
            NSUB = NBLK * SUBS
            zt = P.sb(st, "zt", [128, NSUB, 2], F32)
            P.op("dve", lambda e: e.memset(zt[:], 0.0), writes=[zt])
            sl_v = slots.t.rearrange("(a p) c -> p a c", p=128)
            P.dma("sp", sl_v, zt[:], reads=[zt], writes=[slots])
            for T in range(NT):
                for k in range(2):
                    P.iscatter(slots[:], TW[k][:, T, :], DI[k][:, T:T + 1].bitcast(U32), reads=[TW[k], DI[k]], writes=[slots])
            STA = P.sb(st, "STA", [128, NSUB, 2], F32)
            P.dma("sp", STA[:], sl_v, reads=[slots], writes=[STA])
            STI = P.sb(st, "STI", [128, NSUB], I32)
            P.op("dve", lambda e: e.tensor_copy(out=STI[:], in_=STA[:, :, 0]), reads=[STA], writes=[STI])
            W1 = [P.sb(st, "W1_%d" % i, [128, 16, 512], BF16) for i in range(2)]
            W3 = [P.sb(st, "W3_%d" % i, [128, 16, 512], BF16) for i in range(2)]
            W2 = [P.sb(st, "W2_%d" % i, [128, 4, D], BF16) for i in range(2)]
            ix1 = [P.sb(st, "ix1_%d" % i, [128, 16], I32) for i in range(2)]
            ix2 = [P.sb(st, "ix2_%d" % i, [128, 4], I32) for i in range(2)]
            X = [P.sb(st, "X%d" % i, [128, D], BF16) for i in range(2)]
            XT = [P.sb(st, "XT%d" % i, [128, 16, 128], BF16) for i in range(2)]
            sl = [P.sb(st, "sl%d" % i, [128, 512], F32) for i in range(2)]
            G = [P.sb(st, "G%d" % i, [128, 512], BF16) for i in range(2)]
            GT = [P.sb(st, "GT%d" % i, [128, 4, 128], BF16) for i in range(2)]
            Ys = [P.sb(st, "Ys%d" % i, [128, D], F32) for i in range(2)]
            ptx = P.ps(st, "ptx", [128, 1024], BF16)
            pH1 = P.ps(st, "pH1", [128, 512], F32)
            pH3 = P.ps(st, "pH3", [128, 512], F32)
            pYs = [P.ps(st, "pYs%d" % i, [128, 512], F32) for i in range(2)]

            eo = [P.sb(st, "eo%d" % i, [128, 2], F32) for i in range(2)]

            def load_wb(b):
                i1, eo_ = ix1[b % 2], eo[b % 2]
                P.op("dve", lambda e: e.tensor_scalar(out=eo_[:, 0:1], in0=BE[:, b:b + 1], scalar1=128.0, scalar2=off1,
                                                      op0=ALU.mult, op1=ALU.add), reads=[BE], writes=[eo_])
                P.op("dve", lambda e: e.tensor_scalar(out=i1[:, 0:1], in0=tkf[:, 0:1], scalar1=eo_[:, 0:1], scalar2=None,
                                                      op0=ALU.add), reads=[tkf, eo_], writes=[i1])
                P.idma(W1[b % 2][:].rearrange("p a b -> p (a b)"), w1r[:], i1[:, 0:1].bitcast(U32), reads=[i1, w1],
                       writes=[W1[b % 2]])
                P.idma(W3[b % 2][:].rearrange("p a b -> p (a b)"), w3r[:], i1[:, 0:1].bitcast(U32), reads=[i1, w3],
                       writes=[W3[b % 2]])
                P.idma(W2[b % 2][:].rearrange("p a b -> p (a b)"), w2r[:], i1[:, 0:1].bitcast(U32), reads=[i1, w2],
                       writes=[W2[b % 2]])

            load_wb(0)
            nsb = 0
            for b in range(NBLK):
                if b + 1 < NBLK:
                    load_wb(b + 1)
                W1_, W3_, W2_ = W1[b % 2], W3[b % 2], W2[b % 2]
                for sub in range(SUBS):
                    i2_ = nsb % 2
                    nsb += 1
                    base = b * SB + sub * 128
                    sbi = b * SUBS + sub
                    X_, XT_ = X[i2_], XT[i2_]
                    P.idma(X_[:], h2tok[:], STI[:, sbi:sbi + 1].bitcast(U32), reads=[STI, h2tok], writes=[X_])
                    for hh in range(2):
                        for q in range(8):
                            fc = hh * 8 + q
                            P.op("pe", lambda e: e.transpose(out=ptx[:, q * 128:(q + 1) * 128], in_=X_[:, fc * 128:(fc + 1) * 128],
                                                             identity=identb[:]), reads=[X_, identb], writes=[ptx])
                        P.op("act", lambda e: e.activation(out=XT_[:, hh * 8:(hh + 1) * 8, :],
                                                           in_=ptx[:, :].rearrange("p (a b) -> p a b", b=128), func=AF.Copy),
                             reads=[ptx], writes=[XT_])
                    for kc in range(16):
                        P.op("pe", lambda e: e.matmul(out=pH1[:, :], lhsT=XT_[:, kc, :], rhs=W1_[:, kc, :], start=(kc == 0),
                                                      stop=(kc == 15)), reads=[XT_, W1_], writes=[pH1])
                    for kc in range(16):
                        P.op("pe", lambda e: e.matmul(out=pH3[:, :], lhsT=XT_[:, kc, :], rhs=W3_[:, kc, :], start=(kc == 0),
                                                      stop=(kc == 15)), reads=[XT_, W3_], writes=[pH3])
                    sl_, G_, GT_ = sl[i2_], G[i2_], GT[i2_]
                    P.op("act", lambda e: e.activation(out=sl_[:], in_=pH1[:, :], func=AF.Silu), reads=[pH1], writes=[sl_])
                    P.op("dve", lambda e: e.tensor_tensor(out=G_[:], in0=pH3[:, :], in1=sl_[:], op=ALU.mult), reads=[pH3, sl_],
                         writes=[G_])
                    for q in range(4):
                        P.op("pe", lambda e: e.transpose(out=ptx[:, q * 128:(q + 1) * 128], in_=G_[:, q * 128:(q + 1) * 128],
                                                         identity=identb[:]), reads=[G_, identb], writes=[ptx])
                    P.op("act", lambda e: e.activation(out=GT_[:], in_=ptx[:, 0:512].rearrange("p (a b) -> p a b", b=128),
                                                       func=AF.Copy), reads=[ptx], writes=[GT_])
                    Ys_ = Ys[i2_]
                    for nb4 in range(4):
                        pY_ = pYs[nb4 % 2]
                        for q in range(4):
                            P.op("pe", lambda e: e.matmul(out=pY_[:, :], lhsT=GT_[:, q, :], rhs=W2_[:, q, nb4 * 512:(nb4 + 1) * 512],
                                                          start=(q == 0), stop=(q == 3)), reads=[GT_, W2_], writes=[pY_])
                        P.op("act" if nb4 % 2 else "dve",
                             (lambda e: e.activation(out=Ys_[:, nb4 * 512:(nb4 + 1) * 512], in_=pY_[:, :], func=AF.Copy,
                                                     scale=STA[:, sbi, 1:2])) if nb4 % 2 else
                             (lambda e: e.tensor_scalar(out=Ys_[:, nb4 * 512:(nb4 + 1) * 512], in0=pY_[:, :], scalar1=STA[:, sbi, 1:2],
                                                        scalar2=None, op0=ALU.mult)),
                             reads=[pY_, STA], writes=[Ys_])
                    P.dma("sp", ysl[base:base + 128, :], Ys_[:], reads=[Ys_], writes=[ysl])
        with P.scope() as st:
            ya = [P.sb(st, "ya%d" % i, [128, D], F32) for i in range(2)]
            yb = [P.sb(st, "yb%d" % i, [128, D], F32) for i in range(2)]
            acc = P.sb(st, "acc", [128, 16, 512], F32)
            x1c = [P.sb(st, "x1c%d" % i, [128, 512], F32) for i in range(2)]
            sq = [P.sb(st, "sq%d" % i, [128, 512], F32) for i in range(2)]
            tmp = [P.sb(st, "tmp%d" % i, [128, 512], F32) for i in range(2)]
            mean = P.sb(st, "mean", [128, 512], F32)
            rstd = P.sb(st, "rstd", [128, 512], F32)
            xo = [P.sb(st, "xo%d" % i, [128, 512], F32) for i in range(2)]
            ptf = [P.ps(st, "ptf%d" % i, [128, 512], F32) for i in range(2)]
            ps_s = P.ps(st, "ps_s", [128, 512], F32)
            ps_q = P.ps(st, "ps_q", [128, 512], F32)
            ng = 0
            for (t0, t1) in blocks(N, 512):
                ntp = t1 - t0
                for jt in range(ntp // 128):
                    T = t0 // 128 + jt
                    ya_, yb_ = ya[ng % 2], yb[ng % 2]
                    ng += 1
                    P.idma(ya_[:], ysl[:], DI[0][:, T:T + 1].bitcast(U32), reads=[DI[0], ysl], writes=[ya_])
                    P.idma(yb_[:], ysl[:], DI[1][:, T:T + 1].bitcast(U32), reads=[DI[1], ysl], writes=[yb_])
                    P.op("dve", lambda e: e.tensor_tensor(out=ya_[:], in0=ya_[:], in1=yb_[:], op=ALU.add), reads=[ya_, yb_],
                         writes=[ya_])
                    for g4 in range(4):
                        pt_ = ptf[g4 % 2]
                        for q in range(4):
                            fc = g4 * 4 + q
                            P.op("pe", lambda e: e.transpose(out=pt_[:, q * 128:(q + 1) * 128], in_=ya_[:, fc * 128:(fc + 1) * 128],
                                                             identity=identf[:]), reads=[ya_, identf], writes=[pt_])
                        P.op("act", lambda e: e.activation(out=acc[:, g4 * 4:(g4 + 1) * 4, jt * 128:(jt + 1) * 128],
                                                           in_=pt_[:, :].rearrange("p (a b) -> p a b", b=128), func=AF.Copy),
                             reads=[pt_], writes=[acc])
                sg = [(a - t0, b - t0, j) for (a, b, j) in segs_own(t0, t1, ctxn)]
                for fc in range(16):
                    x1_ = x1c[fc % 2]
                    P.dma("sp", x1_[:, 0:ntp], x1T[fc * 128:(fc + 1) * 128, t0:t1], reads=[x1T], writes=[x1_])
                    for (a, b, j) in sg:
                        P.op("dve", lambda e: e.scalar_tensor_tensor(out=acc[:, fc, a:b], in0=acc[:, fc, a:b],
                                                                     scalar=g2a[:, fc, j:j + 1], in1=x1_[:, a:b], op0=ALU.mult,
                                                                     op1=ALU.add), reads=[acc, g2a, x1_], writes=[acc])

                def outf(fc, t_):
                    xo_ = xo[fc % 2]
                    P.op("act", lambda e: e.activation(out=xo_[:, 0:ntp], in_=t_[:, 0:ntp], func=AF.Identity,
                                                       scale=g_[:, fc:fc + 1], bias=b_[:, fc:fc + 1]), reads=[t_, g_, b_],
                         writes=[xo_])
                    P.dma("sp", xoT[fc * 128:(fc + 1) * 128, t0:t1], xo_[:, 0:ntp], reads=[xo_], writes=[xoT])

                ln_block(P, C, (sq, mean, rstd, tmp), acc, 0, ntp, outf, (ps_s, ps_q))


_CACHE = {}


def _run(key, build_fn, in_maps):
    if key not in _CACHE:
        _CACHE[key] = build_fn()
    nc = _CACHE[key]
    res = run_bass_kernel_spmd(nc, in_maps, core_ids=list(range(8)))
    return res.results


def _prog():
    nc = bass.Bass("TRN2", target_bir_lowering=False)
    return nc, Prog(nc)


def build_a():
    nc, P = _prog()
    cc = P.dram("cc", [128, 16, 2], F32, kind="ExternalInput")
    wmod = P.dram("wmod", [D, 6 * D], F32, kind="ExternalInput")
    bmod = P.dram("bmod", [128, 96], F32, kind="ExternalInput")
    xT = P.dram("xT", [D, NOWN], F32, kind="ExternalInput")
    mods_o = P.dram("mods_o", [128, 96, 2], F32, kind="ExternalOutput")
    hT = P.dram("hT", [D, NOWN], BF16, kind="ExternalOutput")
    with ExitStack() as st:
        C = consts(P, st)
        phase_a(P, C, cc, wmod, bmod, xT, mods_o, hT)
        P.finish([mods_o, hT])
    P.close()
    return nc


def build_mla(debug=False):
    nc, P = _prog()
    dbg = None
    if debug:
        dbg = dict(cqn=P.dram("d_cqn", [128, 6, NB], BF16, kind="ExternalOutput"),
                   ckvn=P.dram("d_ckvn", [128, 2, NB], BF16, kind="ExternalOutput"),
                   kr=P.dram("d_kr", [64, NB], BF16, kind="ExternalOutput"))
    hT = P.dram("hT", [D, NB], BF16, kind="ExternalInput")
    w_in = P.dram("w_in", [D, 1088], F32, kind="ExternalInput")
    qng = P.dram("qng", [128, 6], F32, kind="ExternalInput")
    w_uq = P.dram("w_uq", [768, 8 * 192], F32, kind="ExternalInput")
    kvng = P.dram("kvng", [128, 2], F32, kind="ExternalInput")
    w_ukv = P.dram("w_ukv", [256, 8 * 256], F32, kind="ExternalInput")
    cos2 = P.dram("cos2", [64, NB], F32, kind="ExternalInput")
    sin2 = P.dram("sin2", [64, NB], F32, kind="ExternalInput")
    oT = P.dram("oT", [1024, NB], BF16, kind="ExternalOutput")
    with ExitStack() as st:
        C = consts(P, st)
        phase_mla(P, C, hT, w_in, qng, w_uq, kvng, w_ukv, cos2, sin2, oT, dbg)
        P.finish([oT] + (list(dbg.values()) if dbg else []))
    P.close()
    return nc


def build_gla():
    nc, P = _prog()
    hT = P.dram("hT", [D, NB], BF16, kind="ExternalInput")
    w_in = P.dram("w_in", [D, 2 * 1536], F32, kind="ExternalInput")
    ga = P.dram("ga", [D, 32], F32, kind="ExternalInput")
    gb = P.dram("gb", [2, 16, 512], F32, kind="ExternalInput")
    gbias = P.dram("gbias", [2, 1, 512], F32, kind="ExternalInput")
    ng = P.dram("ng", [1, 512], F32, kind="ExternalInput")
    oT = P.dram("oT", [1024, NB], BF16, kind="ExternalOutput")
    oscr = P.dram("oscr", [NB, 512], F32, kind="Internal")
    with ExitStack() as st:
        C = consts(P, st)
        phase_gla(P, C, hT, w_in, ga, gb, gbias, ng, oT, oscr)
        P.finish([oT])
    P.close()
    return nc


def build_c():
    nc, P = _prog()
    xT = P.dram("xT", [D, NOWN], F32, kind="ExternalInput")
    mods_d = P.dram("mods", [128, 96, 2], F32, kind="ExternalInput")
    oTf = P.dram("oTf", [D, NOWN], BF16, kind="ExternalInput")
    w_o = P.dram("w_o", [D, D], F32, kind="ExternalInput")
    l1g = P.dram("l1g", [128, 16], F32, kind="ExternalInput")
    l1b = P.dram("l1b", [128, 16], F32, kind="ExternalInput")
    l2g = P.dram("l2g", [128, 16], F32, kind="ExternalInput")
    l2b = P.dram("l2b", [128, 16], F32, kind="ExternalInput")
    wr = P.dram("wr", [D, 36], F32, kind="ExternalInput")
    br = P.dram("br", [1, 36], F32, kind="ExternalInput")
    w1 = P.dram("w1", [32 * 128, 8192], F32, kind="ExternalInput")
    w3 = P.dram("w3", [32 * 128, 8192], F32, kind="ExternalInput")
    w2 = P.dram("w2", [32 * 128, 8192], F32, kind="ExternalInput")
    x1T = P.dram("x1T", [D, NOWN], F32, kind="Internal")
    h2T = P.dram("h2T", [D, NOWN], BF16, kind="Internal")
    wgT = P.dram("wgT", [32, NOWN], F32, kind="Internal")
    xoT = P.dram("xoT", [D, NOWN], F32, kind="ExternalOutput")
    h2tok = P.dram("h2tok", [NOWN, D], BF16)
    rt = P.dram("rt", [NOWN, 68], F32)
    SB = 256
    ysl = P.dram("ysl", [((2 * NOWN) // SB + 32) * SB, D], F32)
    with ExitStack() as st:
        C = consts(P, st)
        phase_c1(P, C, xT, mods_d, oTf, w_o, l1g, l1b, wr, br, x1T, h2T, wgT, h2tok=h2tok, rt=rt)
        slots = P.dram("slots", [((2 * NOWN) // SB + 32) * SB, 2], F32)
        phase_c2_sparse(P, C, x1T, h2tok, rt, mods_d, w1, w3, w2, l2g, l2b, xoT, ysl, slots, NOWN, 128, SB=SB)
        P.finish([xoT])
    P.close()
    return nc


def _lv(b, i, name):
    v = Buf("%s_%d" % (name, i), b.t[i])
    return v


def build_fused():
    nc, P = _prog()
    N, CT = NB, 256
    I = "ExternalInput"
    xT0 = P.dram("xT0", [D, N], F32, kind=I)
    cc = P.dram("cc", [128, 16, 2], F32, kind=I)
    w_mod = P.dram("w_mod", [DEPTH, D, 6 * D], F32, kind=I)
    b_mod = P.dram("b_mod", [DEPTH, 128, 96], F32, kind=I)
    l1g = P.dram("l1g", [DEPTH, 128, 16], F32, kind=I)
    l1b = P.dram("l1b", [DEPTH, 128, 16], F32, kind=I)
    l2g = P.dram("l2g", [DEPTH, 128, 16], F32, kind=I)
    l2b = P.dram("l2b", [DEPTH, 128, 16], F32, kind=I)
    m_w_in = P.dram("m_w_in", [2, D, 1088], F32, kind=I)
    m_qn = P.dram("m_qn", [2, 128, 6], F32, kind=I)
    m_w_uq = P.dram("m_w_uq", [2, 768, 3072], F32, kind=I)
    m_kvn = P.dram("m_kvn", [2, 128, 2], F32, kind=I)
    m_w_ukv = P.dram("m_w_ukv", [2, 256, 4096], F32, kind=I)
    m_w_o = P.dram("m_w_o", [2, D, D], F32, kind=I)
    g_w_in = P.dram("g_w_in", [2, D, 6144], F32, kind=I)
    g_ga = P.dram("g_ga", [2, D, 32], F32, kind=I)
    g_gb = P.dram("g_gb", [2, 2, 16, 1024], F32, kind=I)
    g_gbias = P.dram("g_gbias", [2, 2, 1, 1024], F32, kind=I)
    g_ng = P.dram("g_ng", [2, 1, 512], F32, kind=I)
    g_w_o = P.dram("g_w_o", [2, D, D], F32, kind=I)
    wr = P.dram("wr", [DEPTH, D, 36], F32, kind=I)
    br = P.dram("br", [DEPTH, 1, 36], F32, kind=I)
    w1 = P.dram("w1", [DEPTH * 32 * 128, 8192], F32, kind=I)
    w3 = P.dram("w3", [DEPTH * 32 * 128, 8192], F32, kind=I)
    w2 = P.dram("w2", [DEPTH * 32 * 128, 8192], F32, kind=I)
    cos2 = P.dram("cos2", [64, N], F32, kind=I)
    sin2 = P.dram("sin2", [64, N], F32, kind=I)
    xs = [P.dram("xA", [D, N], F32), P.dram("xB", [D, N], F32)]
    hT = P.dram("hT", [D, N], BF16)
    oT = P.dram("oT", [D, N], BF16)
    mods = P.dram("mods", [128, 96, 2], F32)
    x1T = P.dram("x1T", [D, N], F32)
    h2T = P.dram("h2T", [D, N], BF16)
    wgT = P.dram("wgT", [32, N], F32)
    oscr = P.dram("oscr", [N, 512], F32)
    xoT = P.dram("xoT", [D, N], F32, kind="ExternalOutput")
    h2tok = P.dram("h2tok", [N, D], BF16)
    rt = P.dram("rt", [N, 68], F32)
    SB = 256
    ysl = P.dram("ysl", [((2 * N) // SB + 32) * SB, D], F32)
    slots = P.dram("slots", [((2 * N) // SB + 32) * SB, 2], F32)
    wb = [P.dram("wb%d" % q, [32 * 128, 8192], BF16) for q in range(3)]
    passes = blocks(N, 512)
    with ExitStack() as st:
        C = consts(P, st)
        stage = [P.sb(st, "pfst%d" % q, [128, 2048], BF16) for q in range(2)]
        xin = xT0
        for i in range(DEPTH):
            j = i // 2
            jobs = []
            for ex in range(32):
                for q, wsrc in enumerate((w1, w3, w2)):
                    r0 = (i * 32 + ex) * 128
                    for hf2 in range(4):
                        jobs.append((wsrc.t[r0:r0 + 128, hf2 * 2048:(hf2 + 1) * 2048],
                                     wb[q].t[ex * 128:(ex + 1) * 128, hf2 * 2048:(hf2 + 1) * 2048], wsrc, wb[q]))
            pf = Prefetcher(P, stage, jobs)
            xout = xoT if i == DEPTH - 1 else xs[i % 2]
            phase_a(P, C, cc, _lv(w_mod, i, "wm"), _lv(b_mod, i, "bm"), xin, mods, hT, N=N, ctxn=CT)
            P.new_epoch()
            if i % 2 == 0:
                phase_mla(P, C, hT, _lv(m_w_in, j, "a"), _lv(m_qn, j, "b"), _lv(m_w_uq, j, "c"), _lv(m_kvn, j, "d"),
                          _lv(m_w_ukv, j, "e"), cos2, sin2, oT, NHT=16, tick=pf.tick)
                w_o = _lv(m_w_o, j, "f")
            else:
                phase_gla(P, C, hT, _lv(g_w_in, j, "g"), _lv(g_ga, j, "h"), _lv(g_gb, j, "i"), _lv(g_gbias, j, "j"),
                          _lv(g_ng, j, "k"), oT, oscr, NHL=4, tick=pf.tick)
                w_o = _lv(g_w_o, j, "l")
            pf.flush()
            P.new_epoch()
            phase_c1(P, C, xin, mods, oT, w_o, _lv(l1g, i, "m"), _lv(l1b, i, "n"), _lv(wr, i, "o"), _lv(br, i, "p"),
                     x1T, h2T, wgT, N=N, ctxn=CT, h2tok=h2tok, rt=rt)
            phase_c2_sparse(P, C, x1T, h2tok, rt, mods, wb[0], wb[1], wb[2], _lv(l2g, i, "t"),
                            _lv(l2b, i, "u"), xout, ysl, slots, N, CT, SB=SB, layer=0)
            P.new_epoch()
            xin = xout
        P.finish([xoT])
    P.close()
    print("fused program: ninst", P.ninst, "waits", P.nwaits, "nsem", P.nsem, "epochs", P.epoch)
    return nc


def relayout_w(w, kc):
    w = np.asarray(w, np.float32)
    L, E, K, n = w.shape
    return np.ascontiguousarray(w.reshape(L, E, kc, 128, n).transpose(0, 1, 3, 2, 4)).reshape(L * E * 128, kc * n)


def fm(v):
    return np.ascontiguousarray(np.asarray(v, np.float32).reshape(-1, 128).T)


def rope_tables():
    n_rows = 4096 // 64
    row = np.repeat(np.arange(n_rows, dtype=np.float32), 64)
    col = np.tile(np.arange(64, dtype=np.float32), n_rows)
    n_freq = 16
    inv_freq = np.power(np.float32(10000.0), -np.arange(n_freq, dtype=np.float32) / n_freq).astype(np.float32)
    ang = np.concatenate([row[:, None] * inv_freq, col[:, None] * inv_freq], axis=-1)
    cos, sin = np.cos(ang).astype(np.float32), np.sin(ang).astype(np.float32)
    cos2 = np.ones((64, NB), np.float32)
    sin2 = np.zeros((64, NB), np.float32)
    cos2[0:32, 256:] = cos.T
    cos2[32:64, 256:] = cos.T
    sin2[0:32, 256:] = sin.T
    sin2[32:64, 256:] = sin.T
    return cos2, sin2


def kernel(x, c, ctx, c_ctx, w_mod, b_mod, ln1_g, ln1_b, ln2_g, ln2_b,
           mla_w_in, mla_q_norm, mla_w_uq, mla_kv_norm, mla_w_ukv, mla_w_o,
           gla_w_in, gla_gate_a, gla_gate_b, gla_gate_bias, gla_norm, gla_w_o,
           moe_w_grp, moe_b_grp, moe_w_exp, moe_b_exp, moe_w1, moe_w3, moe_w2):
    f32 = np.float32
    A = lambda v: np.ascontiguousarray(np.asarray(v, f32))
    x = A(x); ctx = A(ctx); c = A(c); c_ctx = A(c_ctx)
    cos2, sin2 = rope_tables()
    gwi = np.asarray(gla_w_in, f32)
    cols = []
    for h in range(4):
        cols += [gwi[:, :, h * 256:(h + 1) * 256], gwi[:, :, 1024 + h * 256:1024 + (h + 1) * 256],
                 gwi[:, :, 2048 + h * 512:2048 + (h + 1) * 512], gwi[:, :, 4096 + h * 512:4096 + (h + 1) * 512]]
    shared = dict(
        w_mod=A(w_mod), b_mod=A(np.stack([fm(b_mod[i]) for i in range(DEPTH)])),
        l1g=A(np.stack([fm(ln1_g[i]) for i in range(DEPTH)])), l1b=A(np.stack([fm(ln1_b[i]) for i in range(DEPTH)])),
        l2g=A(np.stack([fm(ln2_g[i]) for i in range(DEPTH)])), l2b=A(np.stack([fm(ln2_b[i]) for i in range(DEPTH)])),
        m_w_in=A(mla_w_in), m_qn=A(np.stack([fm(mla_q_norm[j]) for j in range(2)])), m_w_uq=A(mla_w_uq),
        m_kvn=A(np.stack([fm(mla_kv_norm[j]) for j in range(2)])), m_w_ukv=A(mla_w_ukv), m_w_o=A(mla_w_o),
        g_w_in=A(np.concatenate(cols, axis=2)),
        g_ga=A(np.concatenate([np.asarray(gla_gate_a, f32)[:, 0], np.asarray(gla_gate_a, f32)[:, 1]], axis=2)),
        g_gb=A(gla_gate_b), g_gbias=A(np.asarray(gla_gate_bias, f32)[:, :, None, :]),
        g_ng=A(np.asarray(gla_norm, f32)[:, None, :]), g_w_o=A(gla_w_o),
        wr=A(np.concatenate([np.asarray(moe_w_grp, f32), np.asarray(moe_w_exp, f32)], axis=2)),
        br=A(np.concatenate([np.asarray(moe_b_grp, f32), np.asarray(moe_b_exp, f32)], axis=1)[:, None, :]),
        w1=relayout_w(moe_w1, 16), w3=relayout_w(moe_w3, 16), w2=relayout_w(moe_w2, 4), cos2=cos2, sin2=sin2)
    ins = []
    for core in range(8):
        b = core // 2
        d = dict(shared)
        d["xT0"] = np.ascontiguousarray(np.concatenate([ctx[b], x[b]], axis=0).T)
        d["cc"] = np.ascontiguousarray(np.stack([fm(c[b]), fm(c_ctx)], axis=-1))
        ins.append(d)
    res = _run("fused", build_fused, ins)
    out = np.empty((4, 4096, D), f32)
    for b in range(4):
        out[b] = res[2 * b]["xoT"][:, 256:].T
    return out
```

```python
import numpy as np
import ml_dtypes
from contextlib import ExitStack
import concourse.bass as bass
import concourse.mybir as mybir
from concourse.bass_utils import run_bass_kernel_spmd

F32 = mybir.dt.float32
BF16 = mybir.dt.bfloat16
I32 = mybir.dt.int32
U32 = mybir.dt.uint32
AF = mybir.ActivationFunctionType
ALU = mybir.AluOpType
AX = mybir.AxisListType

import os
SKIP = os.environ.get("SKIP", "")
STOPAT = float(os.environ.get("STOPAT", "99"))
D = 2048
DEPTH = 4
NOWN = 2176
NB = 4352
ALPHA = (2.0 * DEPTH) ** 0.25
EPS = 1e-6


class Buf:
    __slots__ = ("name", "t", "last_w", "reads", "dsem", "dcount", "psum", "nowaw")

    def __init__(self, name, t=None):
        self.psum = False
        self.nowaw = False
        self.name = name
        self.t = t
        self.last_w = None
        self.reads = []
        self.dsem = None
        self.dcount = 0

    def __getitem__(self, idx):
        return self.t[idx]


class Prog:
    def __init__(self, nc):
        self.nc = nc
        self.es = ExitStack()
        self.engs = {"pe": nc.tensor, "act": nc.scalar, "dve": nc.vector, "pool": nc.gpsimd, "sp": nc.sync}
        self.esem = {}
        self.ecount = {k: 0 for k in self.engs}
        self.waited = {k: {} for k in self.engs}
        self.sems = {}
        self.epoch = 0
        for k in self.engs:
            s = self.es.enter_context(nc.semaphore("es_" + k))
            self.esem[k] = s
            self.sems[("e", k, 0)] = s
        self.sem_pool = []
        self.scopes = []
        self.nsem = 0
        self.dbufs = []
        self.nwaits = 0
        self.ninst = 0
        self.uid = 0

    def sb(self, stack, name, shape, dt):
        self.uid += 1
        nm = "%s_%d" % (name, self.uid)
        t = stack.enter_context(self.nc.sbuf_tensor(nm, list(shape), dt))
        b = Buf(nm, t)
        if self.scopes:
            self.scopes[-1].append(b)
        return b

    def ps(self, stack, name, shape, dt):
        self.uid += 1
        nm = "%s_%d" % (name, self.uid)
        t = stack.enter_context(self.nc.psum_tensor(nm, list(shape), dt))
        b = Buf(nm, t)
        b.psum = True
        return b

    def dram(self, name, shape, dt, kind="Internal"):
        t = self.nc.dram_tensor(name, list(shape), dt, kind=kind)
        b = Buf(name, t.ap())
        b.nowaw = True
        return b

    def _dsem(self, buf):
        if buf.dsem is None:
            if self.sem_pool:
                buf.dsem, buf.dcount = self.sem_pool.pop()
            else:
                self.nsem += 1
                s = self.es.enter_context(self.nc.semaphore("ds%d" % self.nsem))
                buf.dsem = ("d", self.nsem)
                self.sems[buf.dsem] = s
            self.dbufs.append(buf)
        return buf.dsem

    def new_epoch(self):
        self.barrier()
        self.epoch += 1
        for k in self.engs:
            s = self.es.enter_context(self.nc.semaphore("es_%s_%d" % (k, self.epoch)))
            self.esem[k] = s
            self.sems[("e", k, self.epoch)] = s
            self.ecount[k] = 0

    def release_scope(self, bufs):
        for b in bufs:
            if b.dsem is not None:
                self.sem_pool.append((b.dsem, b.dcount))
                self.dbufs.remove(b)
                b.dsem = None

    def _wait(self, ek, ev):
        if ev is None:
            return
        key, val = ev
        if key == ("e", ek, self.epoch) and ek == "pe":
            return
        if self.waited[ek].get(key, 0) >= val:
            return
        self.waited[ek][key] = val
        self.engs[ek].wait_ge(self.sems[key], val)
        self.nwaits += 1

    def _deps(self, ek, reads, writes):
        for b in reads:
            self._wait(ek, b.last_w)
        for b in writes:
            if not (b.nowaw and b.last_w is not None and b.last_w[0] == b.dsem):
                self._wait(ek, b.last_w)
            for r in b.reads:
                self._wait(ek, r)

    def _commit(self, ev, reads, writes):
        for b in reads:
            b.reads.append(ev)
            if len(b.reads) > 24:
                d = {}
                for k, v in b.reads:
                    d[k] = max(d.get(k, 0), v)
                b.reads = list(d.items())
        for b in writes:
            b.last_w = ev
            b.reads = []

    def op(self, ek, fn, reads=(), writes=()):
        pr = [b for b in reads if b.psum]
        if pr:
            reads = [b for b in reads if not b.psum]
            writes = list(writes) + pr
        self._deps(ek, reads, writes)
        ins = fn(self.engs[ek])
        self.ecount[ek] += 1
        ins.then_inc(self.esem[ek], 1)
        self._commit((("e", ek, self.epoch), self.ecount[ek]), reads, writes)
        self.ninst += 1
        return ins

    def dma(self, q, out_ap, in_ap, reads=(), writes=(), sem_buf=None, **kw):
        self._deps(q, reads, writes)
        sb_ = sem_buf if sem_buf is not None else writes[0]
        key = self._dsem(sb_)
        ins = self.engs[q].dma_start(out=out_ap, in_=in_ap, **kw)
        sb_.dcount += 1
        ins.then_inc(self.sems[key], 16)
        self._commit((key, 16 * sb_.dcount), reads, writes)
        self.ninst += 1
        return ins

    def scope(self):
        return _Scope(self)

    def barrier(self):
        for ek in self.engs:
            for e2 in self.engs:
                if e2 != ek and self.ecount[e2] > 0:
                    self._wait(ek, (("e", e2, self.epoch), self.ecount[e2]))
            for b in self.dbufs:
                self._wait(ek, (b.dsem, 16 * b.dcount))

    def idma(self, out_ap, in_ap, idx_ap, reads=(), writes=()):
        self._deps("pool", reads, writes)
        sb_ = writes[0]
        key = self._dsem(sb_)
        ins = self.nc.gpsimd.indirect_dma_start(out=out_ap, out_offset=None, in_=in_ap,
                                                in_offset=bass.IndirectOffsetOnAxis(ap=idx_ap, axis=0))
        sb_.dcount += 1
        ins.then_inc(self.sems[key], 16)
        self._commit((key, 16 * sb_.dcount), reads, writes)
        self.ninst += 1
        return ins

    def iscatter(self, out_ap, in_ap, idx_ap, reads=(), writes=()):
        self._deps("pool", reads, writes)
        sb_ = writes[0]
        self._wait("pool", sb_.last_w)
        key = self._dsem(sb_)
        ins = self.nc.gpsimd.indirect_dma_start(out=out_ap, out_offset=bass.IndirectOffsetOnAxis(ap=idx_ap, axis=0),
                                                in_=in_ap, in_offset=None)
        sb_.dcount += 1
        ins.then_inc(self.sems[key], 16)
        self._commit((key, 16 * sb_.dcount), reads, writes)
        self.ninst += 1
        return ins

    def finish(self, bufs, ek="sp"):
        for b in bufs:
            self._wait(ek, b.last_w)

    def close(self):
        self.es.close()


class _Scope:
    def __init__(self, P):
        self.P = P
        self.st = ExitStack()

    def __enter__(self):
        self.st.__enter__()
        self.bufs = []
        self.P.scopes.append(self.bufs)
        return self.st

    def __exit__(self, *a):
        if a[0] is None:
            self.P.barrier()
            self.P.release_scope(self.bufs)
        self.P.scopes.pop()
        return self.st.__exit__(*a)


class Prefetcher:
    def __init__(self, P, stage, jobs):
        self.P, self.stage, self.jobs = P, stage, jobs
        self.n = 0
        self.m = 0

    def tick(self, k=1):
        P = self.P
        while self.m < self.n:
            src, dst, srcb, dstb = self.jobs[self.m]
            st_ = self.stage[self.m % len(self.stage)]
            P.dma("sp", dst, st_[:], reads=[st_], writes=[dstb])
            self.m += 1
        for _ in range(min(k, len(self.stage))):
            if self.n < len(self.jobs):
                src, dst, srcb, dstb = self.jobs[self.n]
                st_ = self.stage[self.n % len(self.stage)]
                P.dma("pool", st_[:], src, reads=[srcb], writes=[st_])
                self.n += 1

    def flush(self):
        while self.m < len(self.jobs):
            self.tick(len(self.stage))


def blocks(n, bs):
    out = []
    t = 0
    while t < n:
        out.append((t, min(n, t + bs)))
        t += bs
    return out


def segs_own(t0, t1, ctxn=128):
    out = []
    if t0 < ctxn:
        out.append((t0, min(t1, ctxn), 1))
    if t1 > ctxn:
        out.append((max(t0, ctxn), t1, 0))
    return out


def consts(P, st):
    C = {}
    C["ones_f"] = P.sb(st, "ones_f", [128, 128], F32)
    P.op("dve", lambda e: e.memset(C["ones_f"][:], 1.0), writes=[C["ones_f"]])
    C["ones_b"] = P.sb(st, "ones_b", [128, 128], BF16)
    P.op("dve", lambda e: e.memset(C["ones_b"][:], 1.0), writes=[C["ones_b"]])
    C["one"] = P.sb(st, "one", [128, 1], F32)
    P.op("dve", lambda e: e.memset(C["one"][:], 1.0), writes=[C["one"]])
    C["eps"] = P.sb(st, "eps", [128, 1], F32)
    P.op("dve", lambda e: e.memset(C["eps"][:], EPS), writes=[C["eps"]])
    C["epsa"] = P.sb(st, "epsa", [128, 1], F32)
    P.op("dve", lambda e: e.memset(C["epsa"][:], EPS / (ALPHA * ALPHA)), writes=[C["epsa"]])
    return C


def identity(P, st, C, dt, name):
    idf = P.sb(st, name, [128, 128], dt)
    P.op("pool", lambda e: e.memset(idf[:], 1.0), writes=[idf])
    P.op("pool", lambda e: e.affine_select(out=idf[:], in_=idf[:], pattern=[[-1, 128]], compare_op=ALU.is_equal,
                                           fill=0.0, base=0, channel_multiplier=1), reads=[idf], writes=[idf])
    return idf


def tri_mask(P, st, name, dt, val, sgn, strict=False):
    t = P.sb(st, name, [128, 128], dt)
    P.op("pool", lambda e: e.memset(t[:], val), writes=[t])
    P.op("pool", lambda e: e.affine_select(out=t[:], in_=t[:], pattern=[[sgn, 128]],
                                           compare_op=(ALU.is_gt if strict else ALU.is_ge),
                                           fill=0.0, base=0, channel_multiplier=-sgn), reads=[t], writes=[t])
    return t


def phase_a(P, C, cc, wmod, bmod, xT, mods_o, hT, N=NOWN, ctxn=128):
    with P.scope() as st:
        cs = P.sb(st, "cs", [128, 16, 2], F32)
        bm = P.sb(st, "bm", [128, 96], F32)
        mods = P.sb(st, "mods", [128, 96, 2], F32)
        slabs = [P.sb(st, "slab%d" % i, [128, 16, 512], F32) for i in range(2)]
        pm = P.ps(st, "pm", [128, 192], F32)
        P.dma("sp", cs[:], cc[:], reads=[cc], writes=[cs])
        P.dma("sp", bm[:], bmod[:], reads=[bmod], writes=[bm])
        P.op("act", lambda e: e.activation(out=cs[:], in_=cs[:], func=AF.Silu), reads=[cs], writes=[cs])
        wv = wmod.t.rearrange("(kc p) n -> p kc n", p=128)
        for s in range(24):
            sl = slabs[s % 2]
            P.dma("sp", sl[:], wv[:, :, s * 512:(s + 1) * 512], reads=[wmod], writes=[sl])
            for j in range(4):
                n = s * 4 + j
                for kc in range(16):
                    P.op("pe", lambda e: e.matmul(out=pm[:, 2 * n:2 * n + 2], lhsT=sl[:, kc, j * 128:(j + 1) * 128],
                                                  rhs=cs[:, kc, :], start=(kc == 0), stop=(kc == 15)),
                         reads=[sl, cs], writes=[pm])
        for j in range(2):
            P.op("dve", lambda e: e.tensor_tensor(out=mods[:, :, j],
                                                  in0=pm[:].rearrange("p (n j) -> p n j", j=2)[:, :, j],
                                                  in1=bm[:], op=ALU.add), reads=[pm, bm], writes=[mods])
        P.dma("sp", mods_o[:], mods[:], reads=[mods], writes=[mods_o])
        sc1p = P.sb(st, "sc1p", [128, 16, 2], F32)
        P.op("dve", lambda e: e.tensor_scalar(out=sc1p[:], in0=mods[:, 16:32, :], scalar1=1.0, scalar2=None,
                                              op0=ALU.add), reads=[mods], writes=[sc1p])
        xb = [P.sb(st, "xb%d" % i, [128, N], F32) for i in range(2)]
        hb = [P.sb(st, "hb%d" % i, [128, N], BF16) for i in range(2)]
        for c in range(16):
            x_ = xb[c % 2]
            h_ = hb[c % 2]
            P.dma("sp", x_[:], xT[c * 128:(c + 1) * 128, :], reads=[xT], writes=[x_])
            for (a, b, j) in segs_own(0, N, ctxn):
                P.op("act", lambda e: e.activation(out=h_[:, a:b], in_=x_[:, a:b], func=AF.Identity,
                                                   scale=sc1p[:, c, j:j + 1], bias=mods[:, c, j:j + 1]),
                     reads=[x_, sc1p, mods], writes=[h_])
            P.dma("sp", hT[c * 128:(c + 1) * 128, :], h_[:], reads=[h_], writes=[hT])


def phase_mla(P, C, hT, w_in, qng, w_uq, kvng, w_ukv, cos2, sin2, oT, dbg=None, NHT=8, tick=None):
    NH = 8
    scale = 192.0 ** -0.5
    with P.scope() as st0:
        cqn = P.sb(st0, "cqn", [128, 6, NB], BF16)
        ckvn = P.sb(st0, "ckvn", [128, 2, NB], BF16)
        kr = P.sb(st0, "kr", [64, NB], BF16)
        with P.scope() as st:
            win = P.sb(st, "win", [128, 16, 1088], BF16)
            P.dma("pool", win[:], w_in.t.rearrange("(kc p) n -> p kc n", p=128), reads=[w_in], writes=[win])
            wrot = P.sb(st, "wrot", [128, 16, 64], BF16)
            P.op("dve", lambda e: e.tensor_scalar(out=wrot[:, :, 0:32], in0=win[:, :, 1056:1088], scalar1=-1.0,
                                                  scalar2=None, op0=ALU.mult), reads=[win], writes=[wrot])
            P.op("dve", lambda e: e.tensor_copy(out=wrot[:, :, 32:64], in_=win[:, :, 1024:1056]), reads=[win],
                 writes=[wrot])
            qg = P.sb(st, "qg", [128, 6], F32)
            kg = P.sb(st, "kg", [128, 2], F32)
            P.dma("sp", qg[:], qng[:], reads=[qng], writes=[qg])
            P.dma("sp", kg[:], kvng[:], reads=[kvng], writes=[kg])
            hbs = [P.sb(st, "hblk%d" % i, [128, 16, 512], BF16) for i in range(2)]
            cf = P.sb(st, "cf", [128, 8, 512], F32)
            sq = [P.sb(st, "sq%d" % i, [128, 512], F32) for i in range(2)]
            rs = [P.sb(st, "rs%d" % i, [128, 512], F32) for i in range(2)]
            tb = [P.sb(st, "tb%d" % i, [64, 512], F32) for i in range(4)]
            pj = [P.ps(st, "pj%d" % i, [128, 512], F32) for i in range(3)]
            pss = [P.ps(st, "pss%d" % i, [128, 512], F32) for i in range(2)]
            hv = hT.t.rearrange("(kc p) t -> p kc t", p=128)
            npj = 0
            for bi, (t0, t1) in enumerate(blocks(NB, 512)):
                nt = t1 - t0
                hb_ = hbs[bi % 2]
                P.dma("sp", hb_[:, :, 0:nt], hv[:, :, t0:t1], reads=[hT], writes=[hb_])
                for oc in range(8):
                    pb = pj[npj % 3]
                    npj += 1
                    for kc in range(16):
                        P.op("pe", lambda e: e.matmul(out=pb[:, 0:nt], lhsT=win[:, kc, oc * 128:(oc + 1) * 128],
                                                      rhs=hb_[:, kc, 0:nt], start=(kc == 0), stop=(kc == 15)),
                             reads=[win, hb_], writes=[pb])
                    P.op("act", lambda e: e.activation(out=cf[:, oc, 0:nt], in_=pb[:, 0:nt], func=AF.Copy),
                         reads=[pb], writes=[cf])
                if "rope" not in SKIP:
                    pk = pj[npj % 3]
                    npj += 1
                    pr = pj[npj % 3]
                    npj += 1
                    for kc in range(16):
                        P.op("pe", lambda e: e.matmul(out=pk[0:64, 0:nt], lhsT=win[:, kc, 1024:1088], rhs=hb_[:, kc, 0:nt],
                                                      start=(kc == 0), stop=(kc == 15)), reads=[win, hb_], writes=[pk])
                    for kc in range(16):
                        P.op("pe", lambda e: e.matmul(out=pr[0:64, 0:nt], lhsT=wrot[:, kc, :], rhs=hb_[:, kc, 0:nt],
                                                      start=(kc == 0), stop=(kc == 15)), reads=[wrot, hb_], writes=[pr])
                    cb, sb_ = tb[0], tb[1]
                    P.dma("sp", cb[:, 0:nt], cos2[:, t0:t1], reads=[cos2], writes=[cb])
                    P.dma("sp", sb_[:, 0:nt], sin2[:, t0:t1], reads=[sin2], writes=[sb_])
                    P.op("dve", lambda e: e.tensor_tensor(out=tb[2][:, 0:nt], in0=pk[0:64, 0:nt], in1=cb[:, 0:nt], op=ALU.mult),
                         reads=[pk, cb], writes=[tb[2]])
                    P.op("dve", lambda e: e.tensor_tensor(out=tb[3][:, 0:nt], in0=pr[0:64, 0:nt], in1=sb_[:, 0:nt], op=ALU.mult),
                         reads=[pr, sb_], writes=[tb[3]])
                    P.op("dve", lambda e: e.tensor_tensor(out=kr[:, t0:t1], in0=tb[2][:, 0:nt], in1=tb[3][:, 0:nt], op=ALU.add),
                         reads=[tb[2], tb[3]], writes=[kr])
                for gi, (c0, c1, nf, gt, dst) in enumerate([(0, 6, 768.0, qg, cqn), (6, 8, 256.0, kg, ckvn)]):
                    pssb = pss[gi]
                    for ci in range(c0, c1):
                        s_ = sq[ci % 2]
                        P.op("act", lambda e: e.activation(out=s_[:, 0:nt], in_=cf[:, ci, 0:nt], func=AF.Square),
                             reads=[cf], writes=[s_])
                        P.op("pe", lambda e: e.matmul(out=pssb[:, 0:nt], lhsT=C["ones_f"][:], rhs=s_[:, 0:nt],
                                                      start=(ci == c0), stop=(ci == c1 - 1)),
                             reads=[C["ones_f"], s_], writes=[pssb])
                    r_ = rs[gi]
                    P.op("act", lambda e: e.activation(out=r_[:, 0:nt], in_=pssb[:, 0:nt], func=AF.Sqrt,
                                                       scale=1.0 / nf, bias=C["eps"][:]), reads=[pssb, C["eps"]], writes=[r_])
                    P.op("dve", lambda e: e.reciprocal(out=r_[:, 0:nt], in_=r_[:, 0:nt]), reads=[r_], writes=[r_])
                    for ci in range(c0, c1):
                        P.op("dve", lambda e: e.scalar_tensor_tensor(out=dst[:, ci - c0, t0:t1], in0=cf[:, ci, 0:nt],
                                                                     scalar=gt[:, ci - c0:ci - c0 + 1], in1=r_[:, 0:nt],
                                                                     op0=ALU.mult, op1=ALU.mult),
                             reads=[cf, gt, r_], writes=[dst])
        if dbg is not None:
            P.dma("sp", dbg["cqn"][:], cqn[:], reads=[cqn], writes=[dbg["cqn"]])
            P.dma("sp", dbg["ckvn"][:], ckvn[:], reads=[ckvn], writes=[dbg["ckvn"]])
            P.dma("sp", dbg["kr"][:], kr[:], reads=[kr], writes=[dbg["kr"]])
        for hg in range(NHT // 8):
            with P.scope() as st:
                wuq = P.sb(st, "wuq", [128, 6, NH * 192], BF16)
                P.dma("pool", wuq[:], w_uq.t.rearrange("(kc p) n -> p kc n", p=128)[:, :, hg * 1536:(hg + 1) * 1536], reads=[w_uq], writes=[wuq])
                wuqr = P.sb(st, "wuqr", [128, 6, NH * 64], BF16)
                wv4 = wuq[:].rearrange("p k (h d) -> p k h d", d=192)
                wr4 = wuqr[:].rearrange("p k (h d) -> p k h d", d=64)
                for kc in range(6):
                    P.op("dve", lambda e: e.tensor_scalar(out=wr4[:, kc, :, 0:32], in0=wv4[:, kc, :, 160:192], scalar1=-1.0,
                                                          scalar2=None, op0=ALU.mult), reads=[wuq], writes=[wuqr])
                    P.op("dve", lambda e: e.tensor_copy(out=wr4[:, kc, :, 32:64], in_=wv4[:, kc, :, 128:160]), reads=[wuq],
                         writes=[wuqr])
                wukv = P.sb(st, "wukv", [128, 2, NH * 256], BF16)
                P.dma("pool", wukv[:], w_ukv.t.rearrange("(kc p) n -> p kc n", p=128)[:, :, hg * 2048:(hg + 1) * 2048], reads=[w_ukv], writes=[wukv])
                Kh = [P.sb(st, "Kh%d" % i, [128, NB], BF16) for i in range(2)]
                Vh = [P.sb(st, "Vh%d" % i, [128, 34, 128], BF16) for i in range(2)]
                Qn = [P.sb(st, "Qn%d" % i, [128, 512], BF16) for i in range(2)]
                Qp = [P.sb(st, "Qp%d" % i, [64, 512], BF16) for i in range(2)]
                tq = [P.sb(st, "tq%d" % i, [64, 512], F32) for i in range(4)]
                Pt = [P.sb(st, "Pt%d" % i, [128, 512], BF16) for i in range(3)]
                rl = [P.sb(st, "rl%d" % i, [128, 512], F32) for i in range(2)]
                Lacc = [P.sb(st, "Lacc%d" % i, [128, 512], F32) for i in range(2)]
                ob = [P.sb(st, "ob%d" % i, [128, 512], BF16) for i in range(2)]
                pS = [P.ps(st, "pS%d" % i, [128, 512], F32) for i in range(3)]
                pO = [P.ps(st, "pO%d" % i, [128, 512], F32) for i in range(2)]
                pL = [P.ps(st, "pL%d" % i, [128, 512], F32) for i in range(2)]
                pX = P.ps(st, "pX", [128, 512], F32)
                nS = 0
                nq = 0
                qblocks = [(0, 256, 0, 2)] + [(256 + 512 * i, 256 + 512 * (i + 1), 0, 34) for i in range(8)]
                for h in range(NH):
                    K_ = Kh[h % 2]
                    V_ = Vh[h % 2]
                    for (t0, t1) in blocks(NB, 512):
                        nt = t1 - t0
                        for kc in range(2):
                            P.op("pe", lambda e: e.matmul(out=pX[:, 0:nt], lhsT=wukv[:, kc, h * 256:h * 256 + 128],
                                                          rhs=ckvn[:, kc, t0:t1], start=(kc == 0), stop=(kc == 1)),
                                 reads=[wukv, ckvn], writes=[pX])
                        P.op("act", lambda e: e.activation(out=K_[:, t0:t1], in_=pX[:, 0:nt], func=AF.Copy),
                             reads=[pX], writes=[K_])
                    for g in range(0, 34, 4):
                        ng = min(4, 34 - g)
                        for j in range(ng):
                            tt = g + j
                            for kc in range(2):
                                P.op("pe", lambda e: e.matmul(out=pX[:, j * 128:(j + 1) * 128],
                                                              lhsT=ckvn[:, kc, tt * 128:(tt + 1) * 128],
                                                              rhs=wukv[:, kc, h * 256 + 128:h * 256 + 256],
                                                              start=(kc == 0), stop=(kc == 1)),
                                     reads=[wukv, ckvn], writes=[pX])
                        P.op("dve", lambda e: e.tensor_copy(out=V_[:, g:g + ng, :],
                                                            in_=pX[:, 0:ng * 128].rearrange("p (g d) -> p g d", d=128)),
                             reads=[pX], writes=[V_])
                    for (q0, q1, k0, k1) in qblocks:
                        if tick is not None:
                            tick(3)
                        nt = q1 - q0
                        Qn_, Qp_ = Qn[nq % 2], Qp[nq % 2]
                        pO_, pL_ = pO[nq % 2], pL[nq % 2]
                        rl_, ob_ = rl[nq % 2], ob[nq % 2]
                        La_ = Lacc[nq % 2]
                        nq += 1
                        pa = pS[nS % 3]; nS += 1
                        for kc in range(6):
                            P.op("pe", lambda e: e.matmul(out=pa[:, 0:nt], lhsT=wuq[:, kc, h * 192:h * 192 + 128],
                                                          rhs=cqn[:, kc, q0:q1], start=(kc == 0), stop=(kc == 5)),
                                 reads=[wuq, cqn], writes=[pa])
                        P.op("act", lambda e: e.activation(out=Qn_[:, 0:nt], in_=pa[:, 0:nt], func=AF.Copy),
                             reads=[pa], writes=[Qn_])
                        pb = pS[nS % 3]; nS += 1
                        for kc in range(6):
                            P.op("pe", lambda e: e.matmul(out=pb[0:64, 0:nt], lhsT=wuq[:, kc, h * 192 + 128:h * 192 + 192],
                                                          rhs=cqn[:, kc, q0:q1], start=(kc == 0), stop=(kc == 5)),
                                 reads=[wuq, cqn], writes=[pb])
                        pc = pS[nS % 3]; nS += 1
                        for kc in range(6):
                            P.op("pe", lambda e: e.matmul(out=pc[0:64, 0:nt], lhsT=wuqr[:, kc, h * 64:(h + 1) * 64],
                                                          rhs=cqn[:, kc, q0:q1], start=(kc == 0), stop=(kc == 5)),
                                 reads=[wuqr, cqn], writes=[pc])
                        cb, sb_ = tq[0], tq[1]
                        P.dma("sp", cb[:, 0:nt], cos2[:, q0:q1], reads=[cos2], writes=[cb])
                        P.dma("sp", sb_[:, 0:nt], sin2[:, q0:q1], reads=[sin2], writes=[sb_])
                        P.op("dve", lambda e: e.tensor_tensor(out=tq[2][:, 0:nt], in0=pb[0:64, 0:nt], in1=cb[:, 0:nt], op=ALU.mult),
                             reads=[pb, cb], writes=[tq[2]])
                        P.op("dve", lambda e: e.tensor_tensor(out=tq[3][:, 0:nt], in0=pc[0:64, 0:nt], in1=sb_[:, 0:nt], op=ALU.mult),
                             reads=[pc, sb_], writes=[tq[3]])
                        P.op("dve", lambda e: e.tensor_tensor(out=Qp_[:, 0:nt], in0=tq[2][:, 0:nt], in1=tq[3][:, 0:nt], op=ALU.add),
                             reads=[tq[2], tq[3]], writes=[Qp_])
                        AHEAD = 2
                        slots_ = {}

                        def issue_s(kt):
                            nonlocal nS
                            pS_ = pS[nS % 3]
                            Pt_ = Pt[nS % 3]
                            nS += 1
                            P.op("pe", lambda e: e.matmul(out=pS_[:, 0:nt], lhsT=K_[:, kt * 128:(kt + 1) * 128], rhs=Qn_[:, 0:nt],
                                                          start=True, stop=False), reads=[K_, Qn_], writes=[pS_])
                            P.op("pe", lambda e: e.matmul(out=pS_[:, 0:nt], lhsT=kr[:, kt * 128:(kt + 1) * 128], rhs=Qp_[:, 0:nt],
                                                          start=False, stop=True), reads=[kr, Qp_], writes=[pS_])
                            slots_[kt] = (pS_, Pt_)

                        for kt in range(k0, min(k1, k0 + AHEAD)):
                            issue_s(kt)
                        for kt in range(k0, k1):
                            if kt + AHEAD < k1:
                                issue_s(kt + AHEAD)
                            pS_, Pt_ = slots_.pop(kt)
                            P.op("act", lambda e: e.activation(out=Pt_[:, 0:nt], in_=pS_[:, 0:nt], func=AF.Exp, scale=scale),
                                 reads=[pS_], writes=[Pt_])
                            P.op("pe", lambda e: e.matmul(out=pO_[:, 0:nt], lhsT=V_[:, kt, :], rhs=Pt_[:, 0:nt],
                                                          start=(kt == k0), stop=(kt == k1 - 1)), reads=[V_, Pt_], writes=[pO_])
                            if kt == k0:
                                P.op("dve", lambda e: e.tensor_copy(out=La_[:, 0:nt], in_=Pt_[:, 0:nt]), reads=[Pt_], writes=[La_])
                            else:
                                P.op("dve", lambda e: e.tensor_tensor(out=La_[:, 0:nt], in0=La_[:, 0:nt], in1=Pt_[:, 0:nt],
                                                                      op=ALU.add), reads=[La_, Pt_], writes=[La_])
                        P.op("pe", lambda e: e.matmul(out=pL_[:, 0:nt], lhsT=C["ones_f"][:], rhs=La_[:, 0:nt], start=True,
                                                      stop=True), reads=[C["ones_f"], La_], writes=[pL_])
                        P.op("dve", lambda e: e.reciprocal(out=rl_[:, 0:nt], in_=pL_[:, 0:nt]), reads=[pL_], writes=[rl_])
                        P.op("dve", lambda e: e.tensor_tensor(out=ob_[:, 0:nt], in0=pO_[:, 0:nt], in1=rl_[:, 0:nt], op=ALU.mult),
                             reads=[pO_, rl_], writes=[ob_])
                        P.dma("sp", oT[(hg * 8 + h) * 128:(hg * 8 + h + 1) * 128, q0:q1], ob_[:, 0:nt], reads=[ob_], writes=[oT])


def phase_gla(P, C, hT, w_in, ga, gb, gbias, ng, oT, oscr, NHL=2, tick=None):
    NT = 34
    with P.scope() as st0:
        ident = identity(P, st0, C, BF16, "identb")
        triF = tri_mask(P, st0, "triF", F32, -1.0 / 16.0, 1)
        triB = tri_mask(P, st0, "triB", F32, -1.0 / 16.0, -1)
        mskF = tri_mask(P, st0, "mskF", F32, 1.0, 1)
        mskB = tri_mask(P, st0, "mskB", F32, 1.0, -1)
        uT = [P.sb(st0, "uT%d" % d, [17, NB], BF16) for d in range(2)]
        gbw = [P.sb(st0, "gbw%d" % d, [17, NHL * 256], BF16) for d in range(2)]
        ngb = P.sb(st0, "ngb", [128, 512], F32)
        hv = hT.t.rearrange("(kc p) t -> p kc t", p=128)
        with P.scope() as st:
            gaw = P.sb(st, "gaw", [128, 16, 32], BF16)
            P.dma("pool", gaw[:], ga.t.rearrange("(kc p) n -> p kc n", p=128), reads=[ga], writes=[gaw])
            for d in range(2):
                P.dma("pool", gbw[d][0:16, :], gb[d], reads=[gb], writes=[gbw[d]])
                P.dma("pool", gbw[d][16:17, :], gbias[d], reads=[gbias], writes=[gbw[d]])
                P.op("dve", lambda e: e.memset(uT[d][:], 1.0), writes=[uT[d]])
            ngr = P.sb(st, "ngr", [1, 512], F32)
            P.dma("sp", ngr[:], ng[:], reads=[ng], writes=[ngr])
            pu = [P.ps(st, "pu%d" % i, [128, 512], F32) for i in range(2)]
            P.op("pe", lambda e: e.matmul(out=pu[0][:, :], lhsT=C["ones_f"][0:1, :], rhs=ngr[:], start=True, stop=True),
                 reads=[C["ones_f"], ngr], writes=[pu[0]])
            P.op("act", lambda e: e.activation(out=ngb[:], in_=pu[0][:, :], func=AF.Copy), reads=[pu[0]], writes=[ngb])
            hbs = [P.sb(st, "hblk%d" % i, [128, 16, 512], BF16) for i in range(2)]
            for bi, (t0, t1) in enumerate(blocks(NB, 512)):
                nt = t1 - t0
                hb_ = hbs[bi % 2]
                P.dma("sp", hb_[:, :, 0:nt], hv[:, :, t0:t1], reads=[hT], writes=[hb_])
                for d in range(2):
                    for kc in range(16):
                        P.op("pe", lambda e: e.matmul(out=pu[d][0:16, 0:nt], lhsT=gaw[:, kc, d * 16:(d + 1) * 16],
                                                      rhs=hb_[:, kc, 0:nt], start=(kc == 0), stop=(kc == 15)),
                             reads=[gaw, hb_], writes=[pu[d]])
                    P.op("act", lambda e: e.activation(out=uT[d][0:16, t0:t1], in_=pu[d][0:16, 0:nt], func=AF.Copy),
                         reads=[pu[d]], writes=[uT[d]])
        for hl in range(NHL):
            with P.scope() as st1:
                qT = P.sb(st1, "qT", [128, 2, NB], BF16)
                kT = P.sb(st1, "kT", [128, 2, NB], BF16)
                vv = P.sb(st1, "vv", [128, NT, 512], BF16)
                rr = P.sb(st1, "rr", [128, NT, 512], BF16)
                for sub in range(2):
                    if "proj" in SKIP:
                        continue
                    with P.scope() as st:
                        ncols = 512 if sub == 0 else 1024
                        c_off = hl * 1536 + (0 if sub == 0 else 512)
                        wh = P.sb(st, "wh", [128, 16, ncols], BF16)
                        P.dma("pool", wh[:], w_in.t.rearrange("(kc p) n -> p kc n", p=128)[:, :, c_off:c_off + ncols],
                              reads=[w_in], writes=[wh])
                        hbs = [P.sb(st, "hblk%d" % i, [128, 16, 256], BF16) for i in range(2)]
                        pp = [P.ps(st, "pp%d" % i, [128, 512], F32) for i in range(4)]
                        npp = 0
                        for bi, (t0, t1) in enumerate(blocks(NB, 256)):
                            nt = t1 - t0
                            hb_ = hbs[bi % 2]
                            P.dma("sp", hb_[:, :, 0:nt], hv[:, :, t0:t1], reads=[hT], writes=[hb_])
                            if sub == 0:
                                for oc in range(4):
                                    pb = pp[npp % 4]; npp += 1
                                    for kc in range(16):
                                        P.op("pe", lambda e: e.matmul(out=pb[:, 0:nt], lhsT=wh[:, kc, oc * 128:(oc + 1) * 128],
                                                                      rhs=hb_[:, kc, 0:nt], start=(kc == 0), stop=(kc == 15)),
                                             reads=[wh, hb_], writes=[pb])
                                    if oc < 2:
                                        P.op("act", lambda e: e.activation(out=qT[:, oc, t0:t1], in_=pb[:, 0:nt], func=AF.Copy,
                                                                           scale=1.0 / 16.0), reads=[pb], writes=[qT])
                                    else:
                                        P.op("dve", lambda e: e.tensor_copy(out=kT[:, oc - 2, t0:t1], in_=pb[:, 0:nt]),
                                             reads=[pb], writes=[kT])
                            else:
                                for j in range(nt // 128):
                                    tt = t0 // 128 + j
                                    pb = pp[npp % 4]; npp += 1
                                    for kc in range(16):
                                        P.op("pe", lambda e: e.matmul(out=pb[:, :], lhsT=hb_[:, kc, j * 128:(j + 1) * 128],
                                                                      rhs=wh[:, kc, 0:512], start=(kc == 0), stop=(kc == 15)),
                                             reads=[wh, hb_], writes=[pb])
                                    P.op("dve", lambda e: e.tensor_copy(out=vv[:, tt, :], in_=pb[:, :]), reads=[pb], writes=[vv])
                                    pb = pp[npp % 4]; npp += 1
                                    for kc in range(16):
                                        P.op("pe", lambda e: e.matmul(out=pb[:, :], lhsT=hb_[:, kc, j * 128:(j + 1) * 128],
                                                                      rhs=wh[:, kc, 512:1024], start=(kc == 0), stop=(kc == 15)),
                                             reads=[wh, hb_], writes=[pb])
                                    P.op("act", lambda e: e.activation(out=rr[:, tt, :], in_=pb[:, :], func=AF.Silu),
                                         reads=[pb], writes=[rr])
                with P.scope() as st:
                    S = P.sb(st, "S", [128, 2, 512], F32)
                    Sb = P.sb(st, "Sb", [128, 2, 512], BF16)
                    e1 = [P.sb(st, "e1_%d" % i, [128, 256], F32) for i in range(2)]
                    bl = [P.sb(st, "bl%d" % i, [128, 2], F32) for i in range(2)]
                    el = [P.sb(st, "el%d" % i, [128, 2], F32) for i in range(2)]
                    E1 = [P.sb(st, "E1_%d" % i, [128, 2, 128], F32) for i in range(2)]
                    E2 = [P.sb(st, "E2_%d" % i, [128, 2, 128], F32) for i in range(2)]
                    E3 = [P.sb(st, "E3_%d" % i, [128, 2, 128], F32) for i in range(2)]
                    qt = [P.sb(st, "qt%d" % i, [128, 2, 128], BF16) for i in range(2)]
                    kt_ = [P.sb(st, "kt%d" % i, [128, 2, 128], BF16) for i in range(2)]
                    kh = [P.sb(st, "kh%d" % i, [128, 2, 128], BF16) for i in range(2)]
                    khT = [P.sb(st, "khT%d" % i, [128, 256], BF16) for i in range(2)]
                    At = [P.sb(st, "At%d" % i, [128, 128], BF16) for i in range(2)]
                    of_ = [P.sb(st, "of%d" % i, [128, 512], F32) for i in range(2)]
                    osum = [P.sb(st, "osum%d" % i, [128, 512], F32) for i in range(2)]
                    junk = P.sb(st, "junk", [128, 512], F32)
                    ss = [P.sb(st, "ss%d" % i, [128, 1], F32) for i in range(2)]
                    on = [P.sb(st, "on%d" % i, [128, 512], BF16) for i in range(2)]
                    onT = [P.sb(st, "onT%d" % i, [128, 4, 128], BF16) for i in range(2)]
                    pg = P.ps(st, "pg", [128, 512], F32)
                    pbT = P.ps(st, "pbT", [128, 2, 128], F32)
                    pA = P.ps(st, "pA", [128, 512], F32)
                    po = [P.ps(st, "po%d" % i, [128, 512], F32) for i in range(2)]
                    pdS = [P.ps(st, "pdS%d" % i, [128, 512], F32) for i in range(2)]
                    ptr = P.ps(st, "ptr", [128, 1024], BF16)
                    for d in range(2):
                        if "scan" in SKIP:
                            continue
                        tri = triF if d == 0 else triB
                        msk = mskF if d == 0 else mskB
                        lastcol = 127 if d == 0 else 0
                        order = list(range(NT)) if d == 0 else [1, 0] + list(range(NT - 1, 1, -1))
                        P.op("dve", lambda e: e.memset(S[:], 0.0), writes=[S])
                        P.op("dve", lambda e: e.memset(Sb[:], 0.0), writes=[Sb])
                        def part1(ci, tt):
                                i2 = ci % 2
                                ts = slice(tt * 128, (tt + 1) * 128)
                                P.op("pe", lambda e: e.matmul(out=pg[:, 0:256], lhsT=uT[d][0:17, ts],
                                                              rhs=gbw[d][0:17, hl * 256:(hl + 1) * 256], start=True, stop=True),
                                     reads=[uT[d], gbw[d]], writes=[pg])
                                P.op("act", lambda e: e.activation(out=e1[i2][:], in_=pg[:, 0:256], func=AF.Exp, scale=-1.0),
                                     reads=[pg], writes=[e1[i2]])
                                P.op("act", lambda e: e.activation(out=e1[i2][:], in_=e1[i2][:], func=AF.Ln, bias=C["one"][:]),
                                     reads=[e1[i2], C["one"]], writes=[e1[i2]])
                                for dk in range(2):
                                    P.op("pe", lambda e: e.matmul(out=pbT[:, dk, :], lhsT=e1[i2][:, dk * 128:(dk + 1) * 128],
                                                                  rhs=tri[:], start=True, stop=True),
                                         reads=[e1[i2], tri], writes=[pbT])
                                P.op("dve", lambda e: e.tensor_copy(out=bl[i2][:], in_=pbT[:, :, lastcol]), reads=[pbT],
                                     writes=[bl[i2]])
                                for dk in range(2):
                                    P.op("act", lambda e: e.activation(out=E1[i2][:, dk, :], in_=pbT[:, dk, :], func=AF.Exp),
                                         reads=[pbT], writes=[E1[i2]])
                                    P.op("act", lambda e: e.activation(out=E2[i2][:, dk, :], in_=pbT[:, dk, :], func=AF.Exp,
                                                                       scale=-1.0), reads=[pbT], writes=[E2[i2]])
                                for dk in range(2):
                                    P.op("act", lambda e: e.activation(out=E3[i2][:, dk, :], in_=pbT[:, dk, :], func=AF.Exp,
                                                                       scale=-1.0, bias=bl[i2][:, dk:dk + 1]),
                                         reads=[pbT, bl[i2]], writes=[E3[i2]])
                                P.op("act", lambda e: e.activation(out=el[i2][:], in_=bl[i2][:], func=AF.Exp), reads=[bl[i2]],
                                     writes=[el[i2]])
                                P.op("dve", lambda e: e.tensor_tensor(out=qt[i2][:], in0=qT[:, :, ts], in1=E1[i2][:], op=ALU.mult),
                                     reads=[qT, E1[i2]], writes=[qt[i2]])
                                P.op("pool", lambda e: e.tensor_tensor(out=kt_[i2][:], in0=kT[:, :, ts], in1=E2[i2][:], op=ALU.mult),
                                     reads=[kT, E2[i2]], writes=[kt_[i2]])
                                P.op("pool", lambda e: e.tensor_tensor(out=kh[i2][:], in0=kT[:, :, ts], in1=E3[i2][:], op=ALU.mult),
                                     reads=[kT, E3[i2]], writes=[kh[i2]])
                                for dk in range(2):
                                    P.op("pe", lambda e: e.transpose(out=ptr[:, dk * 128:(dk + 1) * 128], in_=kh[i2][:, dk, :],
                                                                     identity=ident[:]), reads=[kh[i2], ident], writes=[ptr])
                                P.op("dve", lambda e: e.tensor_copy(out=khT[i2][:], in_=ptr[:, 0:256]), reads=[ptr],
                                     writes=[khT[i2]])
                                for dk in range(2):
                                    P.op("pe", lambda e: e.matmul(out=pA[:, 0:128], lhsT=kt_[i2][:, dk, :], rhs=qt[i2][:, dk, :],
                                                                  start=(dk == 0), stop=(dk == 1)),
                                         reads=[kt_[i2], qt[i2]], writes=[pA])
                                P.op("dve", lambda e: e.tensor_tensor(out=At[i2][:], in0=pA[:, 0:128], in1=msk[:], op=ALU.mult),
                                     reads=[pA, msk], writes=[At[i2]])

                        def part2(ci, tt):
                                i2 = ci % 2
                                ts = slice(tt * 128, (tt + 1) * 128)
                                po_ = po[i2]
                                for dk in range(2):
                                    P.op("pe", lambda e: e.matmul(out=po_[:, :], lhsT=qt[i2][:, dk, :], rhs=Sb[:, dk, :],
                                                                  start=(dk == 0), stop=False), reads=[qt[i2], Sb], writes=[po_])
                                P.op("pe", lambda e: e.matmul(out=po_[:, :], lhsT=At[i2][:], rhs=vv[:, tt, :], start=False,
                                                              stop=True), reads=[At[i2], vv], writes=[po_])
                                for dk in range(2):
                                    P.op("pe", lambda e: e.matmul(out=pdS[dk][:, :], lhsT=khT[i2][:, dk * 128:(dk + 1) * 128],
                                                                  rhs=vv[:, tt, :], start=True, stop=True),
                                         reads=[khT[i2], vv], writes=[pdS[dk]])
                                for dk in range(2):
                                    P.op("dve", lambda e: e.scalar_tensor_tensor(out=S[:, dk, :], in0=S[:, dk, :],
                                                                                 scalar=el[i2][:, dk:dk + 1], in1=pdS[dk][:, :],
                                                                                 op0=ALU.mult, op1=ALU.add),
                                         reads=[S, el[i2], pdS[dk]], writes=[S])
                                P.op("act", lambda e: e.activation(out=Sb[:], in_=S[:], func=AF.Copy), reads=[S], writes=[Sb])
                                if d == 0:
                                    P.op("act", lambda e: e.activation(out=of_[i2][:], in_=po_[:, :], func=AF.Copy),
                                         reads=[po_], writes=[of_[i2]])
                                    P.dma("sp", oscr[ts, :], of_[i2][:], reads=[of_[i2]], writes=[oscr])
                                else:
                                    P.dma("sp", of_[i2][:], oscr[ts, :], reads=[oscr], writes=[of_[i2]])
                                    P.op("dve", lambda e: e.tensor_tensor(out=osum[i2][:], in0=po_[:, :], in1=of_[i2][:],
                                                                          op=ALU.add), reads=[po_, of_[i2]], writes=[osum[i2]])
                                    P.op("dve", lambda e: e.tensor_tensor(out=junk[:], in0=osum[i2][:], in1=osum[i2][:], op=ALU.mult),
                                         reads=[osum[i2]], writes=[junk])
                                    P.op("dve", lambda e: e.tensor_reduce(out=ss[i2][:], in_=junk[:], axis=AX.X, op=ALU.add),
                                         reads=[junk], writes=[ss[i2]])
                                    P.op("act", lambda e: e.activation(out=ss[i2][:], in_=ss[i2][:], func=AF.Sqrt,
                                                                       scale=1.0 / 512.0, bias=C["eps"][:]),
                                         reads=[ss[i2], C["eps"]], writes=[ss[i2]])
                                    P.op("dve", lambda e: e.reciprocal(out=ss[i2][:], in_=ss[i2][:]), reads=[ss[i2]],
                                         writes=[ss[i2]])
                                    P.op("dve", lambda e: e.scalar_tensor_tensor(out=osum[i2][:], in0=osum[i2][:],
                                                                                 scalar=ss[i2][:, 0:1], in1=ngb[:],
                                                                                 op0=ALU.mult, op1=ALU.mult),
                                         reads=[osum[i2], ss[i2], ngb], writes=[osum[i2]])
                                    P.op("dve", lambda e: e.tensor_tensor(out=on[i2][:], in0=osum[i2][:], in1=rr[:, tt, :],
                                                                          op=ALU.mult), reads=[osum[i2], rr], writes=[on[i2]])
                                    for dv in range(4):
                                        P.op("pe", lambda e: e.transpose(out=ptr[:, 256 + dv * 128:256 + (dv + 1) * 128],
                                                                         in_=on[i2][:, dv * 128:(dv + 1) * 128],
                                                                         identity=ident[:]), reads=[on[i2], ident], writes=[ptr])
                                    P.op("act", lambda e: e.activation(out=onT[i2][:], in_=ptr[:, 256:768].rearrange(
                                        "p (a b) -> p a b", b=128), func=AF.Copy), reads=[ptr], writes=[onT[i2]])
                                    P.dma("sp", oT[hl * 512:(hl + 1) * 512, ts].rearrange("(a p) t -> p a t", p=128),
                                          onT[i2][:], reads=[onT[i2]], writes=[oT])


                        part1(0, order[0])
                        for ci, tt in enumerate(order):
                            if tick is not None:
                                tick(2)
                            if ci + 1 < len(order):
                                part1(ci + 1, order[ci + 1])
                            part2(ci, tt)


def ln_block(P, C, st_bufs, z, c0, nt, out_fn, pst):
    sq, mean, rstd, tmp = st_bufs
    ps_s, ps_q = pst
    for fc in range(16):
        s_ = sq[fc % 2]
        P.op("act", lambda e: e.activation(out=s_[:, 0:nt], in_=z[:, fc, c0:c0 + nt], func=AF.Square), reads=[z], writes=[s_])
        P.op("pe", lambda e: e.matmul(out=ps_s[:, 0:nt], lhsT=C["ones_f"][:], rhs=z[:, fc, c0:c0 + nt], start=(fc == 0),
                                      stop=(fc == 15)), reads=[C["ones_f"], z], writes=[ps_s])
        P.op("pe", lambda e: e.matmul(out=ps_q[:, 0:nt], lhsT=C["ones_f"][:], rhs=s_[:, 0:nt], start=(fc == 0),
                                      stop=(fc == 15)), reads=[C["ones_f"], s_], writes=[ps_q])
    P.op("act", lambda e: e.activation(out=mean[:, 0:nt], in_=ps_s[:, 0:nt], func=AF.Copy, scale=1.0 / D),
         reads=[ps_s], writes=[mean])
    P.op("dve", lambda e: e.tensor_tensor(out=rstd[:, 0:nt], in0=mean[:, 0:nt], in1=mean[:, 0:nt], op=ALU.mult),
         reads=[mean], writes=[rstd])
    P.op("dve", lambda e: e.scalar_tensor_tensor(out=rstd[:, 0:nt], in0=ps_q[:, 0:nt], scalar=1.0 / D, in1=rstd[:, 0:nt],
                                                 op0=ALU.mult, op1=ALU.subtract), reads=[ps_q, rstd], writes=[rstd])
    P.op("act", lambda e: e.activation(out=rstd[:, 0:nt], in_=rstd[:, 0:nt], func=AF.Sqrt, bias=C["epsa"][:]),
         reads=[rstd, C["epsa"]], writes=[rstd])
    P.op("dve", lambda e: e.reciprocal(out=rstd[:, 0:nt], in_=rstd[:, 0:nt]), reads=[rstd], writes=[rstd])
    for fc in range(16):
        t_ = tmp[fc % 2]
        P.op("dve", lambda e: e.tensor_tensor(out=t_[:, 0:nt], in0=z[:, fc, c0:c0 + nt], in1=mean[:, 0:nt], op=ALU.subtract),
             reads=[z, mean], writes=[t_])
        P.op("dve", lambda e: e.tensor_tensor(out=t_[:, 0:nt], in0=t_[:, 0:nt], in1=rstd[:, 0:nt], op=ALU.mult),
             reads=[t_, rstd], writes=[t_])
        out_fn(fc, t_)


def phase_c1(P, C, xT, mods_d, oTf, w_o, lng, lnb, wr, br, x1T, h2T, wgT, N=NOWN, ctxn=128, h2tok=None, rt=None):
    with P.scope() as st:
        wo = P.sb(st, "wo", [128, 16, D], BF16)
        P.dma("pool", wo[:], w_o.t.rearrange("(kc p) n -> p kc n", p=128), reads=[w_o], writes=[wo])
        mods = P.sb(st, "mods", [128, 96, 2], F32)
        P.dma("sp", mods[:], mods_d[:], reads=[mods_d], writes=[mods])
        g1a = P.sb(st, "g1a", [128, 16, 2], F32)
        P.op("dve", lambda e: e.tensor_scalar(out=g1a[:], in0=mods[:, 32:48, :], scalar1=1.0 / ALPHA, scalar2=None,
                                              op0=ALU.mult), reads=[mods], writes=[g1a])
        sc2p = P.sb(st, "sc2p", [128, 16, 2], F32)
        P.op("dve", lambda e: e.tensor_scalar(out=sc2p[:], in0=mods[:, 64:80, :], scalar1=1.0, scalar2=None, op0=ALU.add),
             reads=[mods], writes=[sc2p])
        g_ = P.sb(st, "lng", [128, 16], F32)
        b_ = P.sb(st, "lnb", [128, 16], F32)
        P.dma("sp", g_[:], lng[:], reads=[lng], writes=[g_])
        P.dma("sp", b_[:], lnb[:], reads=[lnb], writes=[b_])
        wrt = P.sb(st, "wrt", [128, 16, 36], F32)
        P.dma("sp", wrt[:], wr.t.rearrange("(kc p) n -> p kc n", p=128), reads=[wr], writes=[wrt])
        brr = P.sb(st, "brr", [1, 36], F32)
        P.dma("sp", brr[:], br[:], reads=[br], writes=[brr])
        brb = P.sb(st, "brb", [128, 36], F32)
        identf = identity(P, st, C, F32, "identf")
        if h2tok is not None:
            identb = identity(P, st, C, BF16, "identb1")
            ptb = P.ps(st, "ptb", [128, 1024], BF16)
            htk = [P.sb(st, "htk%d" % i, [128, D], BF16) for i in range(2)]
            rts = [P.sb(st, "rts%d" % i, [128, 68], F32) for i in range(2)]
            ntk = 0
        BS = 256
        ob = [P.sb(st, "ob%d" % i, [128, 16, BS], BF16) for i in range(2)]
        xb = [P.sb(st, "xb%d" % i, [128, 16, BS], F32) for i in range(2)]
        z = P.sb(st, "z", [128, 16, BS], F32)
        x1 = P.sb(st, "x1", [128, 16, BS], F32)
        h2f = P.sb(st, "h2f", [128, 16, BS], F32)
        h2b = P.sb(st, "h2b", [128, 16, BS], BF16)
        sq = [P.sb(st, "sq%d" % i, [128, BS], F32) for i in range(2)]
        tmp = [P.sb(st, "tmp%d" % i, [128, BS], F32) for i in range(2)]
        mean = P.sb(st, "mean", [128, BS], F32)
        rstd = P.sb(st, "rstd", [128, BS], F32)
        lg = P.sb(st, "lg", [128, 36], F32)
        sm = [P.sb(st, "sm%d" % i, [128, 40], F32) for i in range(6)]
        wg = P.sb(st, "wg", [128, 32], F32)
        wgt_sb = P.sb(st, "wgt_sb", [32, BS], F32)
        py = [P.ps(st, "py%d" % i, [128, 512], F32) for i in range(2)]
        ps_s = P.ps(st, "ps_s", [128, 512], F32)
        ps_q = P.ps(st, "ps_q", [128, 512], F32)
        plg = P.ps(st, "plg", [128, 512], F32)
        pwt = P.ps(st, "pwt", [128, 512], F32)
        P.op("pe", lambda e: e.matmul(out=plg[:, 0:36], lhsT=C["ones_f"][0:1, :], rhs=brr[:], start=True, stop=True),
             reads=[C["ones_f"], brr], writes=[plg])
        P.op("act", lambda e: e.activation(out=brb[:], in_=plg[:, 0:36], func=AF.Copy), reads=[plg], writes=[brb])
        ov = oTf.t.rearrange("(kc p) t -> p kc t", p=128)
        xv = xT.t.rearrange("(kc p) t -> p kc t", p=128)
        x1v = x1T.t.rearrange("(kc p) t -> p kc t", p=128)
        h2v = h2T.t.rearrange("(kc p) t -> p kc t", p=128)
        for bi, (t0, t1) in enumerate(blocks(N, BS)):
            nt = t1 - t0
            sg = [(a - t0, b - t0, j) for (a, b, j) in segs_own(t0, t1, ctxn)]
            ob_, xb_ = ob[bi % 2], xb[bi % 2]
            P.dma("sp", ob_[:, :, 0:nt], ov[:, :, t0:t1], reads=[oTf], writes=[ob_])
            P.dma("sp", xb_[:, :, 0:nt], xv[:, :, t0:t1], reads=[xT], writes=[xb_])
            for fc in range(16):
                pb = py[fc % 2]
                for kc in range(16):
                    P.op("pe", lambda e: e.matmul(out=pb[:, 0:nt], lhsT=wo[:, kc, fc * 128:(fc + 1) * 128], rhs=ob_[:, kc, 0:nt],
                                                  start=(kc == 0), stop=(kc == 15)), reads=[wo, ob_], writes=[pb])
                for (a, b, j) in sg:
                    P.op("dve", lambda e: e.scalar_tensor_tensor(out=z[:, fc, a:b], in0=pb[:, a:b], scalar=g1a[:, fc, j:j + 1],
                                                                 in1=xb_[:, fc, a:b], op0=ALU.mult, op1=ALU.add),
                         reads=[pb, g1a, xb_], writes=[z])

            def outf(fc, t_):
                P.op("act", lambda e: e.activation(out=x1[:, fc, 0:nt], in_=t_[:, 0:nt], func=AF.Identity,
                                                   scale=g_[:, fc:fc + 1], bias=b_[:, fc:fc + 1]), reads=[t_, g_, b_], writes=[x1])
                for (a, b, j) in sg:
                    P.op("act", lambda e: e.activation(out=h2f[:, fc, a:b], in_=x1[:, fc, a:b], func=AF.Identity,
                                                       scale=sc2p[:, fc, j:j + 1], bias=mods[:, 48 + fc, j:j + 1]),
                         reads=[x1, sc2p, mods], writes=[h2f])
                P.op("pool", lambda e: e.tensor_copy(out=h2b[:, fc, 0:nt], in_=h2f[:, fc, 0:nt]), reads=[h2f], writes=[h2b])

            ln_block(P, C, (sq, mean, rstd, tmp), z, 0, nt, outf, (ps_s, ps_q))
            P.dma("sp", x1v[:, :, t0:t1], x1[:, :, 0:nt], reads=[x1], writes=[x1T])
            P.dma("sp", h2v[:, :, t0:t1], h2b[:, :, 0:nt], reads=[h2b], writes=[h2T])
            for j in range(nt // 128):
                for kc in range(16):
                    P.op("pe", lambda e: e.matmul(out=plg[:, 0:36], lhsT=h2f[:, kc, j * 128:(j + 1) * 128], rhs=wrt[:, kc, :],
                                                  start=(kc == 0), stop=(kc == 15)), reads=[h2f, wrt], writes=[plg])
                P.op("dve", lambda e: e.tensor_tensor(out=lg[:], in0=plg[:, 0:36], in1=brb[:], op=ALU.add), reads=[plg, brb],
                     writes=[lg])
                gmax, gsum, goh, m1, m2, wk = sm
                P.op("dve", lambda e: e.tensor_reduce(out=gmax[:, 0:1], in_=lg[:, 0:4], axis=AX.X, op=ALU.max), reads=[lg],
                     writes=[gmax])
                P.op("dve", lambda e: e.tensor_scalar(out=goh[:, 0:4], in0=lg[:, 0:4], scalar1=gmax[:, 0:1], scalar2=None,
                                                      op0=ALU.is_ge), reads=[lg, gmax], writes=[goh])
                P.op("dve", lambda e: e.tensor_scalar(out=gsum[:, 0:4], in0=lg[:, 0:4], scalar1=gmax[:, 0:1], scalar2=None,
                                                      op0=ALU.subtract), reads=[lg, gmax], writes=[gsum])
                P.op("act", lambda e: e.activation(out=gsum[:, 0:4], in_=gsum[:, 0:4], func=AF.Exp), reads=[gsum], writes=[gsum])
                P.op("dve", lambda e: e.tensor_reduce(out=gsum[:, 4:5], in_=gsum[:, 0:4], axis=AX.X, op=ALU.add),
                     reads=[gsum], writes=[gsum])
                P.op("dve", lambda e: e.reciprocal(out=gsum[:, 5:6], in_=gsum[:, 4:5]), reads=[gsum], writes=[gsum])
                P.op("dve", lambda e: e.tensor_scalar(out=goh[:, 4:8], in0=goh[:, 0:4], scalar1=-1.0, scalar2=1e30,
                                                      op0=ALU.add, op1=ALU.mult), reads=[goh], writes=[goh])
                for g in range(4):
                    P.op("dve", lambda e: e.tensor_scalar(out=m1[:, g * 8:(g + 1) * 8], in0=lg[:, 4 + g * 8:12 + g * 8],
                                                          scalar1=goh[:, 4 + g:5 + g], scalar2=None, op0=ALU.add),
                         reads=[lg, goh], writes=[m1])
                P.op("dve", lambda e: e.tensor_reduce(out=m1[:, 32:33], in_=m1[:, 0:32], axis=AX.X, op=ALU.max), reads=[m1],
                     writes=[m1])
                P.op("dve", lambda e: e.tensor_scalar(out=m2[:, 0:32], in0=m1[:, 0:32], scalar1=m1[:, 32:33], scalar2=None,
                                                      op0=ALU.is_ge), reads=[m1], writes=[m2])
                P.op("dve", lambda e: e.scalar_tensor_tensor(out=wk[:, 0:32], in0=m2[:, 0:32], scalar=-1e30, in1=m1[:, 0:32],
                                                             op0=ALU.mult, op1=ALU.add), reads=[m2, m1], writes=[wk])
                P.op("dve", lambda e: e.tensor_reduce(out=wk[:, 32:33], in_=wk[:, 0:32], axis=AX.X, op=ALU.max), reads=[wk],
                     writes=[wk])
                P.op("dve", lambda e: e.tensor_scalar(out=wk[:, 0:32], in0=wk[:, 0:32], scalar1=wk[:, 32:33], scalar2=None,
                                                      op0=ALU.is_ge), reads=[wk], writes=[wk])
                P.op("dve", lambda e: e.tensor_tensor(out=wk[:, 33:34], in0=wk[:, 32:33], in1=m1[:, 32:33], op=ALU.subtract),
                     reads=[wk, m1], writes=[wk])
                P.op("act", lambda e: e.activation(out=wk[:, 33:34], in_=wk[:, 33:34], func=AF.Exp), reads=[wk], writes=[wk])
                P.op("dve", lambda e: e.tensor_scalar(out=wk[:, 33:34], in0=wk[:, 33:34], scalar1=1.0, scalar2=None,
                                                      op0=ALU.add), reads=[wk], writes=[wk])
                P.op("dve", lambda e: e.reciprocal(out=wk[:, 34:35], in_=wk[:, 33:34]), reads=[wk], writes=[wk])
                P.op("dve", lambda e: e.tensor_tensor(out=wk[:, 34:35], in0=wk[:, 34:35], in1=gsum[:, 5:6], op=ALU.mult),
                     reads=[wk, gsum], writes=[wk])
                P.op("dve", lambda e: e.tensor_tensor(out=wk[:, 35:36], in0=gsum[:, 5:6], in1=wk[:, 34:35], op=ALU.subtract),
                     reads=[wk, gsum], writes=[wk])
                P.op("dve", lambda e: e.tensor_scalar(out=wg[:], in0=m2[:, 0:32], scalar1=wk[:, 34:35], scalar2=None,
                                                      op0=ALU.mult), reads=[m2, wk], writes=[wg])
                P.op("dve", lambda e: e.scalar_tensor_tensor(out=wg[:], in0=wk[:, 0:32], scalar=wk[:, 35:36], in1=wg[:],
                                                             op0=ALU.mult, op1=ALU.add), reads=[wk, wg], writes=[wg])
                P.op("pe", lambda e: e.transpose(out=pwt[0:32, j * 128:(j + 1) * 128], in_=wg[:], identity=identf[:]),
                     reads=[wg, identf], writes=[pwt])
                if h2tok is not None:
                    tk0 = t0 + j * 128
                    rts_, htk_ = rts[ntk % 2], htk[ntk % 2]
                    ntk += 1
                    P.op("dve", lambda e: e.tensor_copy(out=rts_[:, 0:32], in_=m2[:, 0:32]), reads=[m2], writes=[rts_])
                    P.op("dve", lambda e: e.tensor_copy(out=rts_[:, 32:64], in_=wk[:, 0:32]), reads=[wk], writes=[rts_])
                    P.op("dve", lambda e: e.tensor_copy(out=rts_[:, 64:66], in_=wk[:, 34:36]), reads=[wk], writes=[rts_])
                    P.op("dve", lambda e: e.tensor_copy(out=rts_[:, 66:68], in_=wk[:, 34:36]), reads=[wk], writes=[rts_])
                    P.dma("sp", rt[tk0:tk0 + 128, :], rts_[:], reads=[rts_], writes=[rt])
                    for hh in range(2):
                        for q in range(8):
                            fc = hh * 8 + q
                            P.op("pe", lambda e: e.transpose(out=ptb[:, q * 128:(q + 1) * 128], in_=h2b[:, fc, j * 128:(j + 1) * 128],
                                                             identity=identb[:]), reads=[h2b, identb], writes=[ptb])
                        P.op("act", lambda e: e.activation(out=htk_[:, hh * 1024:(hh + 1) * 1024], in_=ptb[:, :], func=AF.Copy),
                             reads=[ptb], writes=[htk_])
                    P.dma("sp", h2tok[tk0:tk0 + 128, :], htk_[:], reads=[htk_], writes=[h2tok])
            P.op("act", lambda e: e.activation(out=wgt_sb[:, 0:nt], in_=pwt[0:32, 0:nt], func=AF.Copy), reads=[pwt],
                 writes=[wgt_sb])
            P.dma("sp", wgT[:, t0:t1], wgt_sb[:, 0:nt], reads=[wgt_sb], writes=[wgT])


def phase_c2(P, C, x1T, h2T, wgT, mods_d, w1, w3, w2, lng, lnb, xoT, passes=None, ctxn=128):
    if passes is None:
        passes = [(0, 512), (512, 1024), (1024, 1536), (1536, 2176)]
    MAXT = max(b - a for a, b in passes)
    with P.scope() as st:
        mods = P.sb(st, "mods", [128, 96, 2], F32)
        P.dma("sp", mods[:], mods_d[:], reads=[mods_d], writes=[mods])
        g2a = P.sb(st, "g2a", [128, 16, 2], F32)
        P.op("dve", lambda e: e.tensor_scalar(out=g2a[:], in0=mods[:, 80:96, :], scalar1=1.0 / ALPHA, scalar2=None,
                                              op0=ALU.mult), reads=[mods], writes=[g2a])
        g_ = P.sb(st, "lng", [128, 16], F32)
        b_ = P.sb(st, "lnb", [128, 16], F32)
        P.dma("sp", g_[:], lng[:], reads=[lng], writes=[g_])
        P.dma("sp", b_[:], lnb[:], reads=[lnb], writes=[b_])
        id32 = P.sb(st, "id32", [32, 32], F32)
        P.op("pool", lambda e: e.memset(id32[:], 1.0), writes=[id32])
        P.op("pool", lambda e: e.affine_select(out=id32[:], in_=id32[:], pattern=[[-1, 32]], compare_op=ALU.is_equal,
                                               fill=0.0, base=0, channel_multiplier=1), reads=[id32], writes=[id32])
        W1 = [P.sb(st, "W1_%d" % i, [128, 16, 512], BF16) for i in range(2)]
        W3 = [P.sb(st, "W3_%d" % i, [128, 16, 512], BF16) for i in range(2)]
        W2 = [P.sb(st, "W2_%d" % i, [128, 4, D], BF16) for i in range(2)]
        acc = P.sb(st, "acc", [128, 16, MAXT], F32)
        h2b = [P.sb(st, "h2b%d" % i, [128, 16, MAXT], BF16) for i in range(1)]
        wgt = P.sb(st, "wgt", [32, MAXT], F32)
        wm = [P.sb(st, "wm%d" % i, [32, 512], F32) for i in range(2)]
        Wb = [P.sb(st, "Wb%d" % i, [128, 512], F32) for i in range(2)]
        sl = [P.sb(st, "sl%d" % i, [128, 512], F32) for i in range(2)]
        G = [P.sb(st, "G%d" % i, [128, 4, 512], BF16) for i in range(2)]
        x1c = [P.sb(st, "x1c%d" % i, [128, MAXT], F32) for i in range(2)]
        sq = [P.sb(st, "sq%d" % i, [128, 512], F32) for i in range(2)]
        tmp = [P.sb(st, "tmp%d" % i, [128, 512], F32) for i in range(2)]
        mean = P.sb(st, "mean", [128, 512], F32)
        rstd = P.sb(st, "rstd", [128, 512], F32)
        xo = [P.sb(st, "xo%d" % i, [128, 512], F32) for i in range(2)]
        pH1 = [P.ps(st, "pH1_%d" % i, [128, 512], F32) for i in range(2)]
        pH3 = [P.ps(st, "pH3_%d" % i, [128, 512], F32) for i in range(2)]
        pY = [P.ps(st, "pY%d" % i, [128, 512], F32) for i in range(3)]
        pW = P.ps(st, "pW", [128, 512], F32)
        h2v = h2T.t.rearrange("(kc p) t -> p kc t", p=128)
        w1v = w1.t.rearrange("e (kc p) n -> e p kc n", p=128)
        w3v = w3.t.rearrange("e (kc p) n -> e p kc n", p=128)
        w2v = w2.t.rearrange("e (kc p) n -> e p kc n", p=128)
        seq = [(pi, ex) for pi in range(len(passes)) for ex in range(32)]

        def load_w(k):
            ex = seq[k][1]
            P.dma("pool", W1[k % 2][:], w1v[ex], reads=[w1], writes=[W1[k % 2]])
            P.dma("pool", W3[k % 2][:], w3v[ex], reads=[w3], writes=[W3[k % 2]])
            P.dma("pool", W2[k % 2][:], w2v[ex], reads=[w2], writes=[W2[k % 2]])

        load_w(0)
        nG = 0
        nY = 0
        nH = 0
        for k, (pi, ex) in enumerate(seq):
            t0, t1 = passes[pi]
            ntp = t1 - t0
            subs = blocks(ntp, 512)
            h2b_ = h2b[0]
            if ex == 0:
                P.dma("sp", h2b_[:, :, 0:ntp], h2v[:, :, t0:t1], reads=[h2T], writes=[h2b_])
                P.dma("sp", wgt[:, 0:ntp], wgT[:, t0:t1], reads=[wgT], writes=[wgt])
            if k + 1 < len(seq):
                load_w(k + 1)
            W1_, W3_, W2_ = W1[k % 2], W3[k % 2], W2[k % 2]
            for (s0, s1) in subs:
                ns = s1 - s0
                G_ = G[nG % 2]
                Wb_ = Wb[nG % 2]
                wm_ = wm[nG % 2]
                nG += 1
                P.op("dve", lambda e: e.tensor_scalar(out=wm_[:, 0:ns], in0=wgt[:, s0:s1], scalar1=id32[:, ex:ex + 1],
                                                      scalar2=None, op0=ALU.mult), reads=[wgt, id32], writes=[wm_])
                P.op("pe", lambda e: e.matmul(out=pW[:, 0:ns], lhsT=C["ones_f"][0:32, :], rhs=wm_[:, 0:ns], start=True,
                                              stop=True), reads=[C["ones_f"], wm_], writes=[pW])
                P.op("act", lambda e: e.activation(out=Wb_[:, 0:ns], in_=pW[:, 0:ns], func=AF.Copy), reads=[pW],
                     writes=[Wb_])
                for ffc in range(4):
                    p1, p3 = pH1[nH % 2], pH3[nH % 2]
                    sl_ = sl[nH % 2]
                    nH += 1
                    for kc in range(16):
                        P.op("pe", lambda e: e.matmul(out=p1[:, 0:ns], lhsT=W1_[:, kc, ffc * 128:(ffc + 1) * 128],
                                                      rhs=h2b_[:, kc, s0:s1], start=(kc == 0), stop=(kc == 15)),
                             reads=[W1_, h2b_], writes=[p1])
                    for kc in range(16):
                        P.op("pe", lambda e: e.matmul(out=p3[:, 0:ns], lhsT=W3_[:, kc, ffc * 128:(ffc + 1) * 128],
                                                      rhs=h2b_[:, kc, s0:s1], start=(kc == 0), stop=(kc == 15)),
                             reads=[W3_, h2b_], writes=[p3])
                    P.op("act", lambda e: e.activation(out=sl_[:, 0:ns], in_=p1[:, 0:ns], func=AF.Silu), reads=[p1],
                         writes=[sl_])
                    P.op("dve", lambda e: e.tensor_tensor(out=sl_[:, 0:ns], in0=sl_[:, 0:ns], in1=Wb_[:, 0:ns], op=ALU.mult),
                         reads=[sl_, Wb_], writes=[sl_])
                    P.op("dve", lambda e: e.tensor_tensor(out=G_[:, ffc, 0:ns], in0=p3[:, 0:ns], in1=sl_[:, 0:ns], op=ALU.mult),
                         reads=[p3, sl_], writes=[G_])
                for fc in range(16):
                    pY_ = pY[nY % 3]
                    nY += 1
                    for ffc in range(4):
                        P.op("pe", lambda e: e.matmul(out=pY_[:, 0:ns], lhsT=W2_[:, ffc, fc * 128:(fc + 1) * 128],
                                                      rhs=G_[:, ffc, 0:ns], start=(ffc == 0), stop=(ffc == 3)),
                             reads=[W2_, G_], writes=[pY_])
                    if ex == 0:
                        P.op("act", lambda e: e.activation(out=acc[:, fc, s0:s1], in_=pY_[:, 0:ns], func=AF.Copy),
                             reads=[pY_], writes=[acc])
                    else:
                        P.op("dve", lambda e: e.tensor_tensor(out=acc[:, fc, s0:s1], in0=acc[:, fc, s0:s1], in1=pY_[:, 0:ns],
                                                              op=ALU.add), reads=[acc, pY_], writes=[acc])
            if ex != 31:
                continue
            sg = [(a - t0, b - t0, j) for (a, b, j) in segs_own(t0, t1, ctxn)]
            for fc in range(16):
                x1_ = x1c[fc % 2]
                P.dma("sp", x1_[:, 0:ntp], x1T[fc * 128:(fc + 1) * 128, t0:t1], reads=[x1T], writes=[x1_])
                for (a, b, j) in sg:
                    P.op("dve", lambda e: e.scalar_tensor_tensor(out=acc[:, fc, a:b], in0=acc[:, fc, a:b],
                                                                 scalar=g2a[:, fc, j:j + 1], in1=x1_[:, a:b], op0=ALU.mult,
                                                                 op1=ALU.add), reads=[acc, g2a, x1_], writes=[acc])
            for (s0, s1) in subs:
                ns = s1 - s0

                def outf(fc, t_):
                    xo_ = xo[fc % 2]
                    P.op("act", lambda e: e.activation(out=xo_[:, 0:ns], in_=t_[:, 0:ns], func=AF.Identity,
                                                       scale=g_[:, fc:fc + 1], bias=b_[:, fc:fc + 1]), reads=[t_, g_, b_],
                         writes=[xo_])
                    P.dma("sp", xoT[fc * 128:(fc + 1) * 128, t0 + s0:t0 + s1], xo_[:, 0:ns], reads=[xo_], writes=[xoT])

                ln_block(P, C, (sq, mean, rstd, tmp), acc, s0, ns, outf, (pH1[0], pH3[0]))


def phase_c2_sparse(P, C, x1T, h2tok, rt, mods_d, w1, w3, w2, lng, lnb, xoT, ysl, slots, N, ctxn, SB=256, layer=0):
    NT = N // 128
    NBLK = (2 * N) // SB + 32
    SUBS = SB // 128
    w1r, w3r, w2r = w1.t, w3.t, w2.t
    off1 = float(layer * 32 * 128)
    with P.scope() as st0:
        R = P.sb(st0, "R", [128, NT, 68], F32)
        P.dma("sp", R[:], rt.t.rearrange("(T p) c -> p T c", p=128), reads=[rt], writes=[R])
        DI = [P.sb(st0, "DI%d" % k, [128, NT], I32) for k in range(2)]
        g_ = P.sb(st0, "lng", [128, 16], F32)
        b_ = P.sb(st0, "lnb", [128, 16], F32)
        P.dma("sp", g_[:], lng[:], reads=[lng], writes=[g_])
        P.dma("sp", b_[:], lnb[:], reads=[lnb], writes=[b_])
        mods = P.sb(st0, "mods", [128, 96, 2], F32)
        P.dma("sp", mods[:], mods_d[:], reads=[mods_d], writes=[mods])
        g2a = P.sb(st0, "g2a", [128, 16, 2], F32)
        P.op("dve", lambda e: e.tensor_scalar(out=g2a[:], in0=mods[:, 80:96, :], scalar1=1.0 / ALPHA, scalar2=None,
                                              op0=ALU.mult), reads=[mods], writes=[g2a])
        identf = identity(P, st0, C, F32, "identf2")
        identb = identity(P, st0, C, BF16, "identb2")
        with P.scope() as st:
            Mx = P.sb(st, "Mx", [128, NT, 32], F32)
            P.op("dve", lambda e: e.tensor_tensor(out=Mx[:], in0=R[:, :, 0:32], in1=R[:, :, 32:64], op=ALU.add), reads=[R],
                 writes=[Mx])
            U = tri_mask(P, st, "U", F32, 1.0, 1, strict=True)
            RK = P.sb(st, "RK", [128, NT, 32], F32)
            carry = P.sb(st, "carry", [128, 32], F32)
            P.op("dve", lambda e: e.memset(carry[:], 0.0), writes=[carry])
            pr = P.ps(st, "pr", [128, 512], F32)
            pc = P.ps(st, "pc", [128, 512], F32)
            for T in range(NT):
                P.op("pe", lambda e: e.matmul(out=pr[:, 0:32], lhsT=U[:], rhs=Mx[:, T, :], start=True, stop=True),
                     reads=[U, Mx], writes=[pr])
                P.op("dve", lambda e: e.tensor_tensor(out=RK[:, T, :], in0=pr[:, 0:32], in1=carry[:], op=ALU.add),
                     reads=[pr, carry], writes=[RK])
                P.op("pe", lambda e: e.matmul(out=pc[:, 0:32], lhsT=C["ones_f"][:], rhs=Mx[:, T, :], start=True, stop=True),
                     reads=[C["ones_f"], Mx], writes=[pc])
                P.op("dve", lambda e: e.tensor_tensor(out=carry[:], in0=carry[:], in1=pc[:, 0:32], op=ALU.add),
                     reads=[pc, carry], writes=[carry])
            nb = P.sb(st, "nb", [128, 32], F32)
            P.op("dve", lambda e: e.memset(nb[:], 0.0), writes=[nb])
            for jj in range(N // SB + 1):
                P.op("dve", lambda e: e.scalar_tensor_tensor(out=nb[:], in0=carry[:], scalar=float(SB * jj), in1=nb[:],
                                                             op0=ALU.is_gt, op1=ALU.add), reads=[carry, nb], writes=[nb])
            cs = [P.sb(st, "cs%d" % i, [128, 32], F32) for i in range(2)]
            P.op("dve", lambda e: e.tensor_copy(out=cs[0][:], in_=nb[:]), reads=[nb], writes=[cs[0]])
            cur = 0
            for sft in (1, 2, 4, 8, 16):
                a_, b2 = cs[cur], cs[1 - cur]
                P.op("dve", lambda e: e.tensor_copy(out=b2[:, 0:sft], in_=a_[:, 0:sft]), reads=[a_], writes=[b2])
                P.op("dve", lambda e: e.tensor_tensor(out=b2[:, sft:32], in0=a_[:, sft:32], in1=a_[:, 0:32 - sft], op=ALU.add),
                     reads=[a_], writes=[b2])
                cur = 1 - cur
            pend = P.sb(st, "pend", [128, 32], F32)
            pstart = P.sb(st, "pstart", [128, 32], F32)
            P.op("dve", lambda e: e.tensor_scalar(out=pend[:], in0=cs[cur][:], scalar1=float(SB), scalar2=None, op0=ALU.mult),
                 reads=[cs[cur]], writes=[pend])
            P.op("dve", lambda e: e.scalar_tensor_tensor(out=pstart[:], in0=nb[:], scalar=-float(SB), in1=pend[:],
                                                         op0=ALU.mult, op1=ALU.add), reads=[nb, pend], writes=[pstart])
            Dk = [P.sb(st, "Dk%d" % k, [128, NT], F32) for k in range(2)]
            t32 = [P.sb(st, "t32_%d" % i, [128, 32], F32) for i in range(2)]
            for T in range(NT):
                P.op("dve", lambda e: e.tensor_tensor(out=t32[0][:], in0=RK[:, T, :], in1=pstart[:], op=ALU.add),
                     reads=[RK, pstart], writes=[t32[0]])
                for k in range(2):
                    P.op("dve", lambda e: e.tensor_tensor(out=t32[1][:], in0=t32[0][:], in1=R[:, T, 32 * k:32 * k + 32],
                                                          op=ALU.mult), reads=[t32[0], R], writes=[t32[1]])
                    P.op("dve", lambda e: e.tensor_reduce(out=Dk[k][:, T:T + 1], in_=t32[1][:], axis=AX.X, op=ALU.add),
                         reads=[t32[1]], writes=[Dk[k]])
            for k in range(2):
                P.op("dve", lambda e: e.tensor_copy(out=DI[k][:], in_=Dk[k][:]), reads=[Dk[k]], writes=[DI[k]])
            BE = P.sb(st, "BE", [128, NBLK], F32)
            for b in range(NBLK):
                P.op("dve", lambda e: e.tensor_scalar(out=t32[0][:], in0=pend[:], scalar1=float(SB * b), scalar2=None,
                                                      op0=ALU.is_le), reads=[pend], writes=[t32[0]])
                P.op("dve", lambda e: e.tensor_reduce(out=BE[:, b:b + 1], in_=t32[0][:], axis=AX.X, op=ALU.add),
                     reads=[t32[0]], writes=[BE])
            P.op("dve", lambda e: e.tensor_scalar(out=BE[:], in0=BE[:], scalar1=31.0, scalar2=None, op0=ALU.min), reads=[BE],
                 writes=[BE])
            tki = P.sb(st, "tki", [128, NT], I32)
            P.op("pool", lambda e: e.iota(tki[:], pattern=[[128, NT]], base=0, channel_multiplier=1), writes=[tki])
            tkf = P.sb(st, "tkf", [128, NT], F32)
            P.op("dve", lambda e: e.tensor_copy(out=tkf[:], in_=tki[:]), reads=[tki], writes=[tkf])
            TW = [P.sb(st, "TW%d" % k, [128, NT, 2], F32) for k in range(2)]
            for k in range(2):
                P.op("dve", lambda e: e.tensor_copy(out=TW[k][:, :, 0], in_=tkf[:]), reads=[tkf], writes=[TW[k]])
                P.op("dve", lambda e: e.tensor_copy(out=TW[k][:, :, 1], in_=R[:, :, 64 + k]), reads=[R], writes=[TW[k]])
            NSUB = NBLK * SUBS
            zt = P.sb(st, "zt", [128, NSUB, 2], F32)
            P.op("dve", lambda e: e.memset(zt[:], 0.0), writes=[zt])
            sl_v = slots.t.rearrange("(a p) c -> p a c", p=128)
            P.dma("sp", sl_v, zt[:], reads=[zt], writes=[slots])
            for T in range(NT):
                for k in range(2):
                    P.iscatter(slots[:], TW[k][:, T, :], DI[k][:, T:T + 1].bitcast(U32), reads=[TW[k], DI[k]], writes=[slots])
            STA = P.sb(st, "STA", [128, NSUB, 2], F32)
            P.dma("sp", STA[:], sl_v, reads=[slots], writes=[STA])
            STI = P.sb(st, "STI", [128, NSUB], I32)
            P.op("dve", lambda e: e.tensor_copy(out=STI[:], in_=STA[:, :, 0]), reads=[STA], writes=[STI])
            W1 = [P.sb(st, "W1_%d" % i, [128, 16, 512], BF16) for i in range(2)]
            W3 = [P.sb(st, "W3_%d" % i, [128, 16, 512], BF16) for i in range(2)]
            W2 = [P.sb(st, "W2_%d" % i, [128, 4, D], BF16) for i in range(2)]
            ix1 = [P.sb(st, "ix1_%d" % i, [128, 16], I32) for i in range(2)]
            ix2 = [P.sb(st, "ix2_%d" % i, [128, 4], I32) for i in range(2)]
            X = [P.sb(st, "X%d" % i, [128, D], BF16) for i in range(2)]
            XT = [P.sb(st, "XT%d" % i, [128, 16, 128], BF16) for i in range(2)]
            sl = [P.sb(st, "sl%d" % i, [128, 512], F32) for i in range(2)]
            G = [P.sb(st, "G%d" % i, [128, 512], BF16) for i in range(2)]
            GT = [P.sb(st, "GT%d" % i, [128, 4, 128], BF16) for i in range(2)]
            Ys = [P.sb(st, "Ys%d" % i, [128, D], F32) for i in range(2)]
            ptx = P.ps(st, "ptx", [128, 1024], BF16)
            pH1 = P.ps(st, "pH1", [128, 512], F32)
            pH3 = P.ps(st, "pH3", [128, 512], F32)
            pYs = [P.ps(st, "pYs%d" % i, [128, 512], F32) for i in range(2)]

            eo = [P.sb(st, "eo%d" % i, [128, 2], F32) for i in range(2)]

            def load_wb(b):
                i1, eo_ = ix1[b % 2], eo[b % 2]
                P.op("dve", lambda e: e.tensor_scalar(out=eo_[:, 0:1], in0=BE[:, b:b + 1], scalar1=128.0, scalar2=off1,
                                                      op0=ALU.mult, op1=ALU.add), reads=[BE], writes=[eo_])
                P.op("dve", lambda e: e.tensor_scalar(out=i1[:, 0:1], in0=tkf[:, 0:1], scalar1=eo_[:, 0:1], scalar2=None,
                                                      op0=ALU.add), reads=[tkf, eo_], writes=[i1])
                P.idma(W1[b % 2][:].rearrange("p a b -> p (a b)"), w1r[:], i1[:, 0:1].bitcast(U32), reads=[i1, w1],
                       writes=[W1[b % 2]])
                P.idma(W3[b % 2][:].rearrange("p a b -> p (a b)"), w3r[:], i1[:, 0:1].bitcast(U32), reads=[i1, w3],
                       writes=[W3[b % 2]])
                P.idma(W2[b % 2][:].rearrange("p a b -> p (a b)"), w2r[:], i1[:, 0:1].bitcast(U32), reads=[i1, w2],
                       writes=[W2[b % 2]])

            load_wb(0)
            nsb = 0
            for b in range(NBLK):
                if b + 1 < NBLK:
                    load_wb(b + 1)
                W1_, W3_, W2_ = W1[b % 2], W3[b % 2], W2[b % 2]
                for sub in range(SUBS):
                    i2_ = nsb % 2
                    nsb += 1
                    base = b * SB + sub * 128
                    sbi = b * SUBS + sub
                    X_, XT_ = X[i2_], XT[i2_]
                    P.idma(X_[:], h2tok[:], STI[:, sbi:sbi + 1].bitcast(U32), reads=[STI, h2tok], writes=[X_])
                    for hh in range(2):
                        for q in range(8):
                            fc = hh * 8 + q
                            P.op("pe", lambda e: e.transpose(out=ptx[:, q * 128:(q + 1) * 128], in_=X_[:, fc * 128:(fc + 1) * 128],
                                                             identity=identb[:]), reads=[X_, identb], writes=[ptx])
                        P.op("act", lambda e: e.activation(out=XT_[:, hh * 8:(hh + 1) * 8, :],
                                                           in_=ptx[:, :].rearrange("p (a b) -> p a b", b=128), func=AF.Copy),
                             reads=[ptx], writes=[XT_])
                    for kc in range(16):
                        P.op("pe", lambda e: e.matmul(out=pH1[:, :], lhsT=XT_[:, kc, :], rhs=W1_[:, kc, :], start=(kc == 0),
                                                      stop=(kc == 15)), reads=[XT_, W1_], writes=[pH1])
                    for kc in range(16):
                        P.op("pe", lambda e: e.matmul(out=pH3[:, :], lhsT=XT_[:, kc, :], rhs=W3_[:, kc, :], start=(kc == 0),
                                                      stop=(kc == 15)), reads=[XT_, W3_], writes=[pH3])
                    sl_, G_, GT_ = sl[i2_], G[i2_], GT[i2_]
                    P.op("act", lambda e: e.activation(out=sl_[:], in_=pH1[:, :], func=AF.Silu), reads=[pH1], writes=[sl_])
                    P.op("dve", lambda e: e.tensor_tensor(out=G_[:], in0=pH3[:, :], in1=sl_[:], op=ALU.mult), reads=[pH3, sl_],
                         writes=[G_])
                    for q in range(4):
                        P.op("pe", lambda e: e.transpose(out=ptx[:, q * 128:(q + 1) * 128], in_=G_[:, q * 128:(q + 1) * 128],
                                                         identity=identb[:]), reads=[G_, identb], writes=[ptx])
                    P.op("act", lambda e: e.activation(out=GT_[:], in_=ptx[:, 0:512].rearrange("p (a b) -> p a b", b=128),
                                                       func=AF.Copy), reads=[ptx], writes=[GT_])
                    Ys_ = Ys[i2_]
                    for nb4 in range(4):
                        pY_ = pYs[nb4 % 2]
                        for q in range(4):
                            P.op("pe", lambda e: e.matmul(out=pY_[:, :], lhsT=GT_[:, q, :], rhs=W2_[:, q, nb4 * 512:(nb4 + 1) * 512],
                                                          start=(q == 0), stop=(q == 3)), reads=[GT_, W2_], writes=[pY_])
                        P.op("act" if nb4 % 2 else "dve",
                             (lambda e: e.activation(out=Ys_[:, nb4 * 512:(nb4 + 1) * 512], in_=pY_[:, :], func=AF.Copy,
                                                     scale=STA[:, sbi, 1:2])) if nb4 % 2 else
                             (lambda e: e.tensor_scalar(out=Ys_[:, nb4 * 512:(nb4 + 1) * 512], in0=pY_[:, :], scalar1=STA[:, sbi, 1:2],
                                                        scalar2=None, op0=ALU.mult)),
                             reads=[pY_, STA], writes=[Ys_])
                    P.dma("sp", ysl[base:base + 128, :], Ys_[:], reads=[Ys_], writes=[ysl])
        with P.scope() as st:
            ya = [P.sb(st, "ya%d" % i, [128, D], F32) for i in range(2)]
            yb = [P.sb(st, "yb%d" % i, [128, D], F32) for i in range(2)]
            acc = P.sb(st, "acc", [128, 16, 512], F32)
            x1c = [P.sb(st, "x1c%d" % i, [128, 512], F32) for i in range(2)]
            sq = [P.sb(st, "sq%d" % i, [128, 512], F32) for i in range(2)]
            tmp = [P.sb(st, "tmp%d" % i, [128, 512], F32) for i in range(2)]
            mean = P.sb(st, "mean", [128, 512], F32)
            rstd = P.sb(st, "rstd", [128, 512], F32)
            xo = [P.sb(st, "xo%d" % i, [128, 512], F32) for i in range(2)]
            ptf = [P.ps(st, "ptf%d" % i, [128, 512], F32) for i in range(2)]
            ps_s = P.ps(st, "ps_s", [128, 512], F32)
            ps_q = P.ps(st, "ps_q", [128, 512], F32)
            ng = 0
            for (t0, t1) in blocks(N, 512):
                ntp = t1 - t0
                for jt in range(ntp // 128):
                    T = t0 // 128 + jt
                    ya_, yb_ = ya[ng % 2], yb[ng % 2]
                    ng += 1
                    P.idma(ya_[:], ysl[:], DI[0][:, T:T + 1].bitcast(U32), reads=[DI[0], ysl], writes=[ya_])
                    P.idma(yb_[:], ysl[:], DI[1][:, T:T + 1].bitcast(U32), reads=[DI[1], ysl], writes=[yb_])
                    P.op("dve", lambda e: e.tensor_tensor(out=ya_[:], in0=ya_[:], in1=yb_[:], op=ALU.add), reads=[ya_, yb_],
                         writes=[ya_])
                    for g4 in range(4):
                        pt_ = ptf[g4 % 2]
                        for q in range(4):
                            fc = g4 * 4 + q
                            P.op("pe", lambda e: e.transpose(out=pt_[:, q * 128:(q + 1) * 128], in_=ya_[:, fc * 128:(fc + 1) * 128],
                                                             identity=identf[:]), reads=[ya_, identf], writes=[pt_])
                        P.op("act", lambda e: e.activation(out=acc[:, g4 * 4:(g4 + 1) * 4, jt * 128:(jt + 1) * 128],
                                                           in_=pt_[:, :].rearrange("p (a b) -> p a b", b=128), func=AF.Copy),
                             reads=[pt_], writes=[acc])
                sg = [(a - t0, b - t0, j) for (a, b, j) in segs_own(t0, t1, ctxn)]
                for fc in range(16):
                    x1_ = x1c[fc % 2]
                    P.dma("sp", x1_[:, 0:ntp], x1T[fc * 128:(fc + 1) * 128, t0:t1], reads=[x1T], writes=[x1_])
                    for (a, b, j) in sg:
                        P.op("dve", lambda e: e.scalar_tensor_tensor(out=acc[:, fc, a:b], in0=acc[:, fc, a:b],
                                                                     scalar=g2a[:, fc, j:j + 1], in1=x1_[:, a:b], op0=ALU.mult,
                                                                     op1=ALU.add), reads=[acc, g2a, x1_], writes=[acc])

                def outf(fc, t_):
                    xo_ = xo[fc % 2]
                    P.op("act", lambda e: e.activation(out=xo_[:, 0:ntp], in_=t_[:, 0:ntp], func=AF.Identity,
                                                       scale=g_[:, fc:fc + 1], bias=b_[:, fc:fc + 1]), reads=[t_, g_, b_],
                         writes=[xo_])
                    P.dma("sp", xoT[fc * 128:(fc + 1) * 128, t0:t1], xo_[:, 0:ntp], reads=[xo_], writes=[xoT])

                ln_block(P, C, (sq, mean, rstd, tmp), acc, 0, ntp, outf, (ps_s, ps_q))


_CACHE = {}


def _run(key, build_fn, in_maps):
    if key not in _CACHE:
        _CACHE[key] = build_fn()
    nc = _CACHE[key]
    res = run_bass_kernel_spmd(nc, in_maps, core_ids=list(range(8)))
    return res.results


def _prog():
    nc = bass.Bass("TRN2", target_bir_lowering=False)
    return nc, Prog(nc)


def build_a():
    nc, P = _prog()
    cc = P.dram("cc", [128, 16, 2], F32, kind="ExternalInput")
    wmod = P.dram("wmod", [D, 6 * D], F32, kind="ExternalInput")
    bmod = P.dram("bmod", [128, 96], F32, kind="ExternalInput")
    xT = P.dram("xT", [D, NOWN], F32, kind="ExternalInput")
    mods_o = P.dram("mods_o", [128, 96, 2], F32, kind="ExternalOutput")
    hT = P.dram("hT", [D, NOWN], BF16, kind="ExternalOutput")
    with ExitStack() as st:
        C = consts(P, st)
        phase_a(P, C, cc, wmod, bmod, xT, mods_o, hT)
        P.finish([mods_o, hT])
    P.close()
    return nc


def build_mla(debug=False):
    nc, P = _prog()
    dbg = None
    if debug:
        dbg = dict(cqn=P.dram("d_cqn", [128, 6, NB], BF16, kind="ExternalOutput"),
                   ckvn=P.dram("d_ckvn", [128, 2, NB], BF16, kind="ExternalOutput"),
                   kr=P.dram("d_kr", [64, NB], BF16, kind="ExternalOutput"))
    hT = P.dram("hT", [D, NB], BF16, kind="ExternalInput")
    w_in = P.dram("w_in", [D, 1088], F32, kind="ExternalInput")
    qng = P.dram("qng", [128, 6], F32, kind="ExternalInput")
    w_uq = P.dram("w_uq", [768, 8 * 192], F32, kind="ExternalInput")
    kvng = P.dram("kvng", [128, 2], F32, kind="ExternalInput")
    w_ukv = P.dram("w_ukv", [256, 8 * 256], F32, kind="ExternalInput")
    cos2 = P.dram("cos2", [64, NB], F32, kind="ExternalInput")
    sin2 = P.dram("sin2", [64, NB], F32, kind="ExternalInput")
    oT = P.dram("oT", [1024, NB], BF16, kind="ExternalOutput")
    with ExitStack() as st:
        C = consts(P, st)
        phase_mla(P, C, hT, w_in, qng, w_uq, kvng, w_ukv, cos2, sin2, oT, dbg)
        P.finish([oT] + (list(dbg.values()) if dbg else []))
    P.close()
    return nc


def build_gla():
    nc, P = _prog()
    hT = P.dram("hT", [D, NB], BF16, kind="ExternalInput")
    w_in = P.dram("w_in", [D, 2 * 1536], F32, kind="ExternalInput")
    ga = P.dram("ga", [D, 32], F32, kind="ExternalInput")
    gb = P.dram("gb", [2, 16, 512], F32, kind="ExternalInput")
    gbias = P.dram("gbias", [2, 1, 512], F32, kind="ExternalInput")
    ng = P.dram("ng", [1, 512], F32, kind="ExternalInput")
    oT = P.dram("oT", [1024, NB], BF16, kind="ExternalOutput")
    oscr = P.dram("oscr", [NB, 512], F32, kind="Internal")
    with ExitStack() as st:
        C = consts(P, st)
        phase_gla(P, C, hT, w_in, ga, gb, gbias, ng, oT, oscr)
        P.finish([oT])
    P.close()
    return nc


def build_c():
    nc, P = _prog()
    xT = P.dram("xT", [D, NOWN], F32, kind="ExternalInput")
    mods_d = P.dram("mods", [128, 96, 2], F32, kind="ExternalInput")
    oTf = P.dram("oTf", [D, NOWN], BF16, kind="ExternalInput")
    w_o = P.dram("w_o", [D, D], F32, kind="ExternalInput")
    l1g = P.dram("l1g", [128, 16], F32, kind="ExternalInput")
    l1b = P.dram("l1b", [128, 16], F32, kind="ExternalInput")
    l2g = P.dram("l2g", [128, 16], F32, kind="ExternalInput")
    l2b = P.dram("l2b", [128, 16], F32, kind="ExternalInput")
    wr = P.dram("wr", [D, 36], F32, kind="ExternalInput")
    br = P.dram("br", [1, 36], F32, kind="ExternalInput")
    w1 = P.dram("w1", [32 * 128, 8192], F32, kind="ExternalInput")
    w3 = P.dram("w3", [32 * 128, 8192], F32, kind="ExternalInput")
    w2 = P.dram("w2", [32 * 128, 8192], F32, kind="ExternalInput")
    x1T = P.dram("x1T", [D, NOWN], F32, kind="Internal")
    h2T = P.dram("h2T", [D, NOWN], BF16, kind="Internal")
    wgT = P.dram("wgT", [32, NOWN], F32, kind="Internal")
    xoT = P.dram("xoT", [D, NOWN], F32, kind="ExternalOutput")
    h2tok = P.dram("h2tok", [NOWN, D], BF16)
    rt = P.dram("rt", [NOWN, 68], F32)
    SB = 256
    ysl = P.dram("ysl", [((2 * NOWN) // SB + 32) * SB, D], F32)
    with ExitStack() as st:
        C = consts(P, st)
        phase_c1(P, C, xT, mods_d, oTf, w_o, l1g, l1b, wr, br, x1T, h2T, wgT, h2tok=h2tok, rt=rt)
        slots = P.dram("slots", [((2 * NOWN) // SB + 32) * SB, 2], F32)
        phase_c2_sparse(P, C, x1T, h2tok, rt, mods_d, w1, w3, w2, l2g, l2b, xoT, ysl, slots, NOWN, 128, SB=SB)
        P.finish([xoT])
    P.close()
    return nc


def _lv(b, i, name):
    v = Buf("%s_%d" % (name, i), b.t[i])
    return v


def build_fused():
    nc, P = _prog()
    N, CT = NB, 256
    I = "ExternalInput"
    xT0 = P.dram("xT0", [D, N], F32, kind=I)
    cc = P.dram("cc", [128, 16, 2], F32, kind=I)
    w_mod = P.dram("w_mod", [DEPTH, D, 6 * D], F32, kind=I)
    b_mod = P.dram("b_mod", [DEPTH, 128, 96], F32, kind=I)
    l1g = P.dram("l1g", [DEPTH, 128, 16], F32, kind=I)
    l1b = P.dram("l1b", [DEPTH, 128, 16], F32, kind=I)
    l2g = P.dram("l2g", [DEPTH, 128, 16], F32, kind=I)
    l2b = P.dram("l2b", [DEPTH, 128, 16], F32, kind=I)
    m_w_in = P.dram("m_w_in", [2, D, 1088], F32, kind=I)
    m_qn = P.dram("m_qn", [2, 128, 6], F32, kind=I)
    m_w_uq = P.dram("m_w_uq", [2, 768, 3072], F32, kind=I)
    m_kvn = P.dram("m_kvn", [2, 128, 2], F32, kind=I)
    m_w_ukv = P.dram("m_w_ukv", [2, 256, 4096], F32, kind=I)
    m_w_o = P.dram("m_w_o", [2, D, D], F32, kind=I)
    g_w_in = P.dram("g_w_in", [2, D, 6144], F32, kind=I)
    g_ga = P.dram("g_ga", [2, D, 32], F32, kind=I)
    g_gb = P.dram("g_gb", [2, 2, 16, 1024], F32, kind=I)
    g_gbias = P.dram("g_gbias", [2, 2, 1, 1024], F32, kind=I)
    g_ng = P.dram("g_ng", [2, 1, 512], F32, kind=I)
    g_w_o = P.dram("g_w_o", [2, D, D], F32, kind=I)
    wr = P.dram("wr", [DEPTH, D, 36], F32, kind=I)
    br = P.dram("br", [DEPTH, 1, 36], F32, kind=I)
    w1 = P.dram("w1", [DEPTH * 32 * 128, 8192], F32, kind=I)
    w3 = P.dram("w3", [DEPTH * 32 * 128, 8192], F32, kind=I)
    w2 = P.dram("w2", [DEPTH * 32 * 128, 8192], F32, kind=I)
    cos2 = P.dram("cos2", [64, N], F32, kind=I)
    sin2 = P.dram("sin2", [64, N], F32, kind=I)
    xs = [P.dram("xA", [D, N], F32), P.dram("xB", [D, N], F32)]
    hT = P.dram("hT", [D, N], BF16)
    oT = P.dram("oT", [D, N], BF16)
    mods = P.dram("mods", [128, 96, 2], F32)
    x1T = P.dram("x1T", [D, N], F32)
    h2T = P.dram("h2T", [D, N], BF16)
    wgT = P.dram("wgT", [32, N], F32)
    oscr = P.dram("oscr", [N, 512], F32)
    xoT = P.dram("xoT", [D, N], F32, kind="ExternalOutput")
    h2tok = P.dram("h2tok", [N, D], BF16)
    rt = P.dram("rt", [N, 68], F32)
    SB = 256
    ysl = P.dram("ysl", [((2 * N) // SB + 32) * SB, D], F32)
    slots = P.dram("slots", [((2 * N) // SB + 32) * SB, 2], F32)
    wb = [P.dram("wb%d" % q, [32 * 128, 8192], BF16) for q in range(3)]
    passes = blocks(N, 512)
    with ExitStack() as st:
        C = consts(P, st)
        stage = [P.sb(st, "pfst%d" % q, [128, 2048], BF16) for q in range(3)]
        xin = xT0
        for i in range(DEPTH):
            j = i // 2
            jobs = []
            for ex in range(32):
                for q, wsrc in enumerate((w1, w3, w2)):
                    r0 = (i * 32 + ex) * 128
                    for hf2 in range(4):
                        jobs.append((wsrc.t[r0:r0 + 128, hf2 * 2048:(hf2 + 1) * 2048],
                                     wb[q].t[ex * 128:(ex + 1) * 128, hf2 * 2048:(hf2 + 1) * 2048], wsrc, wb[q]))
            pf = Prefetcher(P, stage, jobs)
            xout = xoT if i == DEPTH - 1 else xs[i % 2]
            phase_a(P, C, cc, _lv(w_mod, i, "wm"), _lv(b_mod, i, "bm"), xin, mods, hT, N=N, ctxn=CT)
            P.new_epoch()
            if i % 2 == 0:
                phase_mla(P, C, hT, _lv(m_w_in, j, "a"), _lv(m_qn, j, "b"), _lv(m_w_uq, j, "c"), _lv(m_kvn, j, "d"),
                          _lv(m_w_ukv, j, "e"), cos2, sin2, oT, NHT=16, tick=pf.tick)
                w_o = _lv(m_w_o, j, "f")
            else:
                phase_gla(P, C, hT, _lv(g_w_in, j, "g"), _lv(g_ga, j, "h"), _lv(g_gb, j, "i"), _lv(g_gbias, j, "j"),
                          _lv(g_ng, j, "k"), oT, oscr, NHL=4, tick=pf.tick)
                w_o = _lv(g_w_o, j, "l")
            pf.flush()
            P.new_epoch()
            phase_c1(P, C, xin, mods, oT, w_o, _lv(l1g, i, "m"), _lv(l1b, i, "n"), _lv(wr, i, "o"), _lv(br, i, "p"),
                     x1T, h2T, wgT, N=N, ctxn=CT, h2tok=h2tok, rt=rt)
            phase_c2_sparse(P, C, x1T, h2tok, rt, mods, wb[0], wb[1], wb[2], _lv(l2g, i, "t"),
                            _lv(l2b, i, "u"), xout, ysl, slots, N, CT, SB=SB, layer=0)
            P.new_epoch()
            xin = xout
        P.finish([xoT])
    P.close()
    print("fused program: ninst", P.ninst, "waits", P.nwaits, "nsem", P.nsem, "epochs", P.epoch)
    return nc


def relayout_w(w, kc):
    w = np.asarray(w, np.float32)
    L, E, K, n = w.shape
    return np.ascontiguousarray(w.reshape(L, E, kc, 128, n).transpose(0, 1, 3, 2, 4)).reshape(L * E * 128, kc * n)


def fm(v):
    return np.ascontiguousarray(np.asarray(v, np.float32).reshape(-1, 128).T)


def rope_tables():
    n_rows = 4096 // 64
    row = np.repeat(np.arange(n_rows, dtype=np.float32), 64)
    col = np.tile(np.arange(64, dtype=np.float32), n_rows)
    n_freq = 16
    inv_freq = np.power(np.float32(10000.0), -np.arange(n_freq, dtype=np.float32) / n_freq).astype(np.float32)
    ang = np.concatenate([row[:, None] * inv_freq, col[:, None] * inv_freq], axis=-1)
    cos, sin = np.cos(ang).astype(np.float32), np.sin(ang).astype(np.float32)
    cos2 = np.ones((64, NB), np.float32)
    sin2 = np.zeros((64, NB), np.float32)
    cos2[0:32, 256:] = cos.T
    cos2[32:64, 256:] = cos.T
    sin2[0:32, 256:] = sin.T
    sin2[32:64, 256:] = sin.T
    return cos2, sin2


def kernel(x, c, ctx, c_ctx, w_mod, b_mod, ln1_g, ln1_b, ln2_g, ln2_b,
           mla_w_in, mla_q_norm, mla_w_uq, mla_kv_norm, mla_w_ukv, mla_w_o,
           gla_w_in, gla_gate_a, gla_gate_b, gla_gate_bias, gla_norm, gla_w_o,
           moe_w_grp, moe_b_grp, moe_w_exp, moe_b_exp, moe_w1, moe_w3, moe_w2):
    f32 = np.float32
    A = lambda v: np.ascontiguousarray(np.asarray(v, f32))
    x = A(x); ctx = A(ctx); c = A(c); c_ctx = A(c_ctx)
    cos2, sin2 = rope_tables()
    gwi = np.asarray(gla_w_in, f32)
    cols = []
    for h in range(4):
        cols += [gwi[:, :, h * 256:(h + 1) * 256], gwi[:, :, 1024 + h * 256:1024 + (h + 1) * 256],
                 gwi[:, :, 2048 + h * 512:2048 + (h + 1) * 512], gwi[:, :, 4096 + h * 512:4096 + (h + 1) * 512]]
    shared = dict(
        w_mod=A(w_mod), b_mod=A(np.stack([fm(b_mod[i]) for i in range(DEPTH)])),
        l1g=A(np.stack([fm(ln1_g[i]) for i in range(DEPTH)])), l1b=A(np.stack([fm(ln1_b[i]) for i in range(DEPTH)])),
        l2g=A(np.stack([fm(ln2_g[i]) for i in range(DEPTH)])), l2b=A(np.stack([fm(ln2_b[i]) for i in range(DEPTH)])),
        m_w_in=A(mla_w_in), m_qn=A(np.stack([fm(mla_q_norm[j]) for j in range(2)])), m_w_uq=A(mla_w_uq),
        m_kvn=A(np.stack([fm(mla_kv_norm[j]) for j in range(2)])), m_w_ukv=A(mla_w_ukv), m_w_o=A(mla_w_o),
        g_w_in=A(np.concatenate(cols, axis=2)),
        g_ga=A(np.concatenate([np.asarray(gla_gate_a, f32)[:, 0], np.asarray(gla_gate_a, f32)[:, 1]], axis=2)),
        g_gb=A(gla_gate_b), g_gbias=A(np.asarray(gla_gate_bias, f32)[:, :, None, :]),
        g_ng=A(np.asarray(gla_norm, f32)[:, None, :]), g_w_o=A(gla_w_o),
        wr=A(np.concatenate([np.asarray(moe_w_grp, f32), np.asarray(moe_w_exp, f32)], axis=2)),
        br=A(np.concatenate([np.asarray(moe_b_grp, f32), np.asarray(moe_b_exp, f32)], axis=1)[:, None, :]),
        w1=relayout_w(moe_w1, 16), w3=relayout_w(moe_w3, 16), w2=relayout_w(moe_w2, 4), cos2=cos2, sin2=sin2)
    ins = []
    for core in range(8):
        b = core // 2
        d = dict(shared)
        d["xT0"] = np.ascontiguousarray(np.concatenate([ctx[b], x[b]], axis=0).T)
        d["cc"] = np.ascontiguousarray(np.stack([fm(c[b]), fm(c_ctx)], axis=-1))
        ins.append(d)
    res = _run("fused", build_fused, ins)
    out = np.empty((4, 4096, D), f32)
    for b in range(4):
        out[b] = res[2 * b]["xoT"][:, 256:].T
    return out
```
